# Optimizing a Trainium2 kernel written in Bass

```python
import jax, jax.numpy as jnp
from jax import lax
import numpy as np

D_MODEL = 1024
BATCH = 2
SEQ = 16384
DEPTH = 2

GRID_W = 64
CTX_LEN = 256
MIX_W = D_MODEL
NA_HEADS = 8
NA_HEAD_DIM = 64
NA_W = NA_HEADS * NA_HEAD_DIM
NA_KH = 8
NA_KW = 16
SGU_GROUPS = 8
SGU_W = MIX_W - NA_W
SGU_GROUP_DIM = SGU_W // SGU_GROUPS
SGU_CHUNK = 128
IN_COLS = 3 * NA_W + 2 * SGU_W
N_GROUPS = 4
EXPERTS_PER_GROUP = 4
N_EXPERTS = N_GROUPS * EXPERTS_PER_GROUP
TOP_K = 2
D_EXPERT = D_MODEL // 2
N_MOD = 6
EPS = 1e-6

kernel_name = "hybrid_na_sgu_hmoe_dit"


def _rms(x):
    x32 = x.astype(jnp.float32)
    return (x32 * lax.rsqrt(jnp.mean(x32 * x32, -1, keepdims=True) + EPS)).astype(x.dtype)


def _layernorm(x, g):
    x32 = x.astype(jnp.float32)
    mu = jnp.mean(x32, -1, keepdims=True)
    var = jnp.mean(jnp.square(x32 - mu), -1, keepdims=True)
    return ((x32 - mu) * lax.rsqrt(var + EPS)).astype(x.dtype) * g


def _adaln(cond, w, b):
    return jnp.split(jax.nn.silu(cond) @ w + b, N_MOD, axis=-1)


def _modulate(x, shift, scale):
    return _rms(x) * (1 + scale) + shift


def _heads(t):
    return t.reshape(t.shape[:-1] + (NA_HEADS, NA_HEAD_DIM))


def _project(h, w_in, q_gain, k_gain):
    p = h @ w_in
    q = _rms(_heads(p[..., :NA_W])) * q_gain
    k = _rms(_heads(p[..., NA_W:2 * NA_W])) * k_gain
    v = _heads(p[..., 2 * NA_W:3 * NA_W])
    u = p[..., 3 * NA_W:3 * NA_W + SGU_W]
    vs = p[..., 3 * NA_W + SGU_W:]
    return q, k, v, u, vs


def _project_kv(h, w_in, k_gain):
    p = h @ w_in[:, NA_W:3 * NA_W]
    return _rms(_heads(p[..., :NA_W])) * k_gain, _heads(p[..., NA_W:])


def _neighborhood_attention(q, k, v, k_ctx, v_ctx, rpb):
    bsz, seq, heads, dh = q.shape
    rows = seq // GRID_W
    kh = min(NA_KH, rows)
    kw = NA_KW
    scale = dh ** -0.5
    q_rows = jnp.moveaxis(q.reshape(bsz, rows, GRID_W, heads, dh), 1, 0)
    k_grid = k.reshape(bsz, rows, GRID_W, heads, dh)
    v_grid = v.reshape(bsz, rows, GRID_W, heads, dh)
    col = jnp.arange(GRID_W)
    col_start = jnp.clip(col - kw // 2, 0, GRID_W - kw)
    col_idx = col_start[:, None] + jnp.arange(kw)[None, :]
    rpb_cols = rpb[:, :, col_idx - col[:, None] + (NA_KW - 1)].astype(jnp.float32)

    def row_block(args):
        q_r, r = args
        r_start = jnp.clip(r - kh // 2, 0, rows - kh)
        k_win = lax.dynamic_slice_in_dim(k_grid, r_start, kh, axis=1)[:, :, col_idx]
        v_win = lax.dynamic_slice_in_dim(v_grid, r_start, kh, axis=1)[:, :, col_idx]
        bias = rpb_cols[:, r_start + jnp.arange(kh) - r + (NA_KH - 1)]
        s_win = jnp.einsum('bqhd,bkqwhd->bhqkw', q_r, k_win).astype(jnp.float32) * scale
        s_win = s_win + jnp.transpose(bias, (0, 2, 1, 3))[None]
        s_ctx = jnp.einsum('bqhd,bchd->bhqc', q_r, k_ctx).astype(jnp.float32) * scale
        s = jnp.concatenate([s_win.reshape(bsz, heads, GRID_W, kh * kw), s_ctx], axis=-1)
        p = jax.nn.softmax(s, axis=-1).astype(v.dtype)
        p_win = p[..., :kh * kw].reshape(bsz, heads, GRID_W, kh, kw)
        p_ctx = p[..., kh * kw:]
        return (jnp.einsum('bhqkw,bkqwhd->bqhd', p_win, v_win)
                + jnp.einsum('bhqc,bchd->bqhd', p_ctx, v_ctx))

    out = lax.map(row_block, (q_rows, jnp.arange(rows)))
    return jnp.moveaxis(out, 0, 1).reshape(bsz, seq, heads * dh)


def _context_attention(q, k, v):
    bsz, clen, heads, dh = q.shape
    s = jnp.einsum('bqhd,bkhd->bhqk', q, k).astype(jnp.float32) * dh ** -0.5
    p = jax.nn.softmax(s, axis=-1).astype(v.dtype)
    return jnp.einsum('bhqk,bkhd->bqhd', p, v).reshape(bsz, clen, heads * dh)


def _sgu(u, v, ln_g, w_s, b_s):
    bsz, length, _ = u.shape
    u = jax.nn.gelu(u)
    v = _layernorm(jax.nn.gelu(v), ln_g)
    v = v.reshape(bsz, length // SGU_CHUNK, SGU_CHUNK, SGU_GROUPS, SGU_GROUP_DIM)
    mixed = jnp.einsum('hpq,bnqhd->bnphd', w_s, v) + b_s.T[:, :, None]
    return u * mixed.reshape(bsz, length, SGU_W)


def _merge(o_na, o_sgu, out_gain, w_out):
    y = jnp.concatenate([_rms(o_na), _rms(o_sgu)], axis=-1) * out_gain
    return y @ w_out


def _hier_moe(h, rg_w, rg_b, re_w, re_b, w1, w3, w2):
    shp = h.shape
    t = h.reshape(-1, D_MODEL)
    p_g = jax.nn.softmax((t @ rg_w).astype(jnp.float32) + rg_b, axis=-1)
    p_top, g_idx = lax.top_k(p_g, 1)
    e_all = jnp.einsum('nd,gde->nge', t, re_w).astype(jnp.float32) + re_b
    e_logits = jnp.take_along_axis(e_all, g_idx[:, :, None], axis=1)[:, 0]
    e_top, e_idx = lax.top_k(jax.nn.softmax(e_logits, axis=-1), TOP_K)
    w = e_top / jnp.sum(e_top, -1, keepdims=True) * p_top
    within = jnp.sum(jax.nn.one_hot(e_idx, EXPERTS_PER_GROUP, dtype=jnp.float32) * w[..., None], axis=1)
    gate = (jax.nn.one_hot(g_idx[:, 0], N_GROUPS, dtype=jnp.float32)[:, :, None]
            * within[:, None, :]).reshape(-1, N_EXPERTS).astype(t.dtype)
    out = jnp.zeros_like(t)
    for e in range(N_EXPERTS):
        hid = jax.nn.silu(t @ w1[e]) * (t @ w3[e])
        out = out + gate[:, e:e + 1] * (hid @ w2[e])
    return out.reshape(shp)


def setup_inputs(seed: int = 0) -> dict:
    key = jax.random.key(seed)
    ks = jax.random.split(key, 22)

    def nrm(k, shape, s):
        return jax.random.normal(k, shape, jnp.float32) * s

    return {
        'x': nrm(ks[0], (BATCH, SEQ, D_MODEL), 1.0),
        'c': nrm(ks[1], (BATCH, D_MODEL), 1.0),
        'ctx': nrm(ks[2], (BATCH, CTX_LEN, D_MODEL), 1.0),
        'c_ctx': nrm(ks[3], (D_MODEL,), 1.0),
        'w_ada': nrm(ks[4], (DEPTH, D_MODEL, N_MOD * D_MODEL), 0.5 * D_MODEL ** -0.5),
        'b_ada': nrm(ks[5], (DEPTH, N_MOD * D_MODEL), 0.02),
        'w_in': nrm(ks[6], (DEPTH, D_MODEL, IN_COLS), D_MODEL ** -0.5),
        'q_gain': 1.0 + nrm(ks[7], (DEPTH, NA_HEAD_DIM), 0.02),
        'k_gain': 1.0 + nrm(ks[8], (DEPTH, NA_HEAD_DIM), 0.02),
        'rpb': nrm(ks[9], (DEPTH, NA_HEADS, 2 * NA_KH - 1, 2 * NA_KW - 1), 0.1),
        'sgu_ln': 1.0 + nrm(ks[10], (DEPTH, SGU_W), 0.02),
        'sgu_w': nrm(ks[11], (DEPTH, SGU_GROUPS, SGU_CHUNK, SGU_CHUNK), SGU_CHUNK ** -0.5),
        'sgu_b': 1.0 + nrm(ks[12], (DEPTH, SGU_GROUPS, SGU_CHUNK), 0.02),
        'out_gain': 1.0 + nrm(ks[13], (DEPTH, MIX_W), 0.02),
        'w_out': nrm(ks[14], (DEPTH, MIX_W, D_MODEL), MIX_W ** -0.5),
        'rg_w': nrm(ks[15], (DEPTH, D_MODEL, N_GROUPS), D_MODEL ** -0.5),
        'rg_b': nrm(ks[16], (DEPTH, N_GROUPS), 0.01),
        're_w': nrm(ks[17], (DEPTH, N_GROUPS, D_MODEL, EXPERTS_PER_GROUP), D_MODEL ** -0.5),
        're_b': nrm(ks[18], (DEPTH, N_GROUPS, EXPERTS_PER_GROUP), 0.01),
        'w1': nrm(ks[19], (DEPTH, N_EXPERTS, D_MODEL, D_EXPERT), D_MODEL ** -0.5),
        'w3': nrm(ks[20], (DEPTH, N_EXPERTS, D_MODEL, D_EXPERT), D_MODEL ** -0.5),
        'w2': nrm(ks[21], (DEPTH, N_EXPERTS, D_EXPERT, D_MODEL), D_EXPERT ** -0.5),
    }


def reference(x, c, ctx, c_ctx, w_ada, b_ada, w_in, q_gain, k_gain, rpb, sgu_ln, sgu_w, sgu_b,
              out_gain, w_out, rg_w, rg_b, re_w, re_b, w1, w3, w2):
    for i in range(DEPTH):
        last = i == DEPTH - 1
        sh_a, sc_a, g_a, sh_f, sc_f, g_f = _adaln(c, w_ada[i], b_ada[i])
        csh_a, csc_a, cg_a, csh_f, csc_f, cg_f = _adaln(c_ctx, w_ada[i], b_ada[i])

        h = _modulate(x, sh_a[:, None], sc_a[:, None])
        hc = _modulate(ctx, csh_a, csc_a)
        q, k, v, u, vs = _project(h, w_in[i], q_gain[i], k_gain[i])
        if last:
            kc, vc = _project_kv(hc, w_in[i], k_gain[i])
        else:
            qc, kc, vc, uc, vsc = _project(hc, w_in[i], q_gain[i], k_gain[i])
        o_na = _neighborhood_attention(q, k, v, kc, vc, rpb[i])
        o_sg = _sgu(u, vs, sgu_ln[i], sgu_w[i], sgu_b[i])
        x = x + g_a[:, None] * _merge(o_na, o_sg, out_gain[i], w_out[i])

        hf = _modulate(x, sh_f[:, None], sc_f[:, None])
        x = x + g_f[:, None] * _hier_moe(hf, rg_w[i], rg_b[i], re_w[i], re_b[i], w1[i], w3[i], w2[i])

        if not last:
            oc_na = _context_attention(qc, kc, vc)
            oc_sg = _sgu(uc, vsc, sgu_ln[i], sgu_w[i], sgu_b[i])
            ctx = ctx + cg_a * _merge(oc_na, oc_sg, out_gain[i], w_out[i])
            hcf = _modulate(ctx, csh_f, csc_f)
            ctx = ctx + cg_f * _hier_moe(hcf, rg_w[i], rg_b[i], re_w[i], re_b[i], w1[i], w3[i], w2[i])
    return x
```

```python
import os as _os
import numpy as np
import ml_dtypes
from contextlib import ExitStack
import concourse.bass as bass
import concourse.mybir as mybir
from concourse.bass_utils import run_bass_kernel_spmd

F32, BF16 = mybir.dt.float32, mybir.dt.bfloat16
AF = mybir.ActivationFunctionType
ALU = mybir.AluOpType
AX = mybir.AxisListType

D = 1024
NEG = -30000.0
EPS = 1e-6
ENGS = ("sp", "pe", "act", "dve", "pool")


class Buf:
    __slots__ = ("name", "w", "r")

    def __init__(self, name):
        self.name, self.w, self.r = name, None, []


class Op:
    __slots__ = ("eng", "fn", "dma", "key", "deps", "signal", "val")


class Sched:
    def __init__(self, nc):
        self.nc = nc
        self.ops = []
        self.bufs = []
        self.dma_out = []
        self.last = {}

    def buf(self, name):
        b = Buf(name)
        self.bufs.append(b)
        return b

    def _dep(self, op, d, raw):
        if d is op:
            return
        if (not d.dma) and (not op.dma) and d.eng == op.eng and not raw:
            return
        op.deps[d] = True
        d.signal = True

    def add(self, eng, fn, reads=(), writes=(), slot=None):
        op = Op()
        op.eng, op.fn, op.dma = eng, fn, slot is not None
        op.key = ("slot", slot.name, eng) if slot is not None else eng
        op.deps, op.signal, op.val = {}, op.dma, 0
        for b in reads:
            if b.w is not None:
                self._dep(op, b.w, True)
        for b in writes:
            if b.w is not None:
                self._dep(op, b.w, False)
            for r in b.r:
                self._dep(op, r, False)
        for b in reads:
            b.r.append(op)
        for b in writes:
            b.w = op
            b.r = []
        self.ops.append(op)
        if op.dma:
            self.dma_out.append(op)
        else:
            self.last[eng] = op
        return op

    def barrier(self):
        a = self.add("sp", lambda e: e.nop())
        for d in self.dma_out:
            a.deps[d] = True
        for eng, o in self.last.items():
            if eng != "sp":
                a.deps[o] = True
                o.signal = True
        a.signal = True
        self.dma_out = []
        for eng in ENGS:
            if eng != "sp":
                o = self.add(eng, lambda e: e.nop())
                o.deps[a] = True
        for b in self.bufs:
            b.w, b.r = None, []

    def emit(self, es):
        nc = self.nc
        cnt = {}
        for op in self.ops:
            if op.signal:
                cnt[op.key] = cnt.get(op.key, 0) + (16 if op.dma else 1)
                op.val = cnt[op.key]
        sems = {}
        for i, k in enumerate(cnt):
            sems[k] = es.enter_context(nc.semaphore("s%d" % i))
        per = {e: [o for o in self.ops if o.eng == e] for e in ENGS}
        block = es.enter_context(nc.Block())

        def body(eng):
            def f(e):
                seen = {}
                for op in per[eng]:
                    need = {}
                    for d in op.deps:
                        if need.get(d.key, 0) < d.val:
                            need[d.key] = d.val
                    for k, v in need.items():
                        if seen.get(k, 0) < v:
                            e.wait_ge(sems[k], v)
                            seen[k] = v
                    ins = op.fn(e)
                    if op.signal:
                        ins.then_inc(sems[op.key], 16 if op.dma else 1)
            return f

        block.sync(body("sp"))
        block.tensor(body("pe"))
        block.scalar(body("act"))
        block.vector(body("dve"))
        block.gpsimd(body("pool"))
        return len(sems)


class T:
    def __init__(self, S, es, nc, name, shape, dt, psum=False):
        self.t = es.enter_context((nc.psum_tensor if psum else nc.sbuf_tensor)("t_" + name, list(shape), dt))
        self.b = S.buf(name.split("_L")[0] if name[-3:-1] == "_L" else name)

    def __getitem__(self, k):
        return self.t[k]


def build(n_layers=2, do_f=True, stages=("W", "M", "PA", "F"), dbg=False, tile_limit=None):
    nc = bass.Bass("TRN2", target_bir_lowering=False)
    S = Sched(nc)

    def din(name, shape, dt=F32):
        return nc.dram_tensor(name, list(shape), dt, kind="ExternalInput").ap()

    def dscr(name, shape, dt=F32):
        return nc.dram_tensor(name, list(shape), dt, kind="ExternalOutput" if dbg else "Internal").ap()

    xw = din("xw", [40 * 128, D])
    ctx0 = din("ctx", [256, D])
    cvec = din("cvec", [128, 16])
    ident_d = din("ident", [128, 128], BF16)
    identf_d = din("identf", [128, 128])
    w_ada = din("w_ada", [2, D, 6 * D])
    b_ada = din("b_ada", [2, 6 * D])
    b_adaT = din("b_adaT", [2, 128, 48])
    FW = {"w_in": 2 * D * 2560 // 128, "w_out": 2 * D * D // 128, "w1": 2 * 16 * D * 512 // 128,
          "w3": 2 * 16 * D * 512 // 128, "w2": 2 * 16 * 512 * D // 128}
    wf = {k: din(k, [128, v]) for k, v in FW.items() if k in ("w_in", "w_out")}
    wb = {k: dscr(k + "_b", [128, v], BF16) for k, v in FW.items() if k in ("w_in", "w_out")}
    wf["w1"] = din("w1", [32, D, 512]); wf["w3"] = din("w3", [32, D, 512]); wf["w2"] = din("w2", [32, 512, D])
    for k_ in ("w1", "w3", "w2"):
        wb[k_] = dscr(k_ + "_b", [32 * 128, 4096], BF16)
    rw = din("rw", [2, D, 20])
    rb = din("rb", [2, 20])
    qg = din("qg", [2, 128, 1])
    kg = din("kg", [2, 128, 1])
    bias = din("bias", [2, 5, 6, 128, 1024])
    sln = din("sln", [2, 512])
    swT = din("swT", [2, 128, 1024])
    sbT = din("sbT", [2, 128, 8])
    ogT = din("ogT", [2, 128, 8])
    out = nc.dram_tensor("out", [32 * 128, D], F32, kind="ExternalOutput").ap()
    x1 = dscr("x1", [40 * 128, D])
    xm = dscr("xm", [40 * 128, D])
    ctx1 = dscr("ctx1", [256, D])
    ctxm = dscr("ctxm", [256, D])
    if dbg:
        dbg_modf = nc.dram_tensor("dbg_modf", [2, 128, 64], F32, kind="ExternalOutput").ap()
        dbg_gates = nc.dram_tensor("dbg_gates", [2, 4, 128, D], F32, kind="ExternalOutput").ap()
    NTMAX = 35
    hfd = dscr("hfd", [40 * 128, D], BF16)
    srt_d = dscr("srt_d", [NTMAX * 512, D], BF16)
    sout = dscr("sout", [NTMAX * 512, D])
    cU_d = din("cU", [128, 128], BF16)
    cOnes_d = din("cOnes", [128, 128], BF16)
    itc_d = din("itc", [128, NTMAX * 16])
    pidx_d = din("pidx", [128, 2])
    fbc_d = dscr("fbc_d", [4, 128, D])
    dbufs = {}

    def dbuf(name):
        if name not in dbufs:
            dbufs[name] = S.buf("dram_" + name)
        return dbufs[name]

    def wview(k, l):
        flat = wb[k].rearrange("p f -> (p f)")
        if k == "w_in":
            return flat.rearrange("(l kc p n) -> l p kc n", l=2, kc=8, p=128, n=2560)[l]
        if k == "w_out":
            return flat.rearrange("(l kc p n) -> l p kc n", l=2, kc=8, p=128, n=1024)[l]
        if k in ("w1", "w3"):
            return flat.rearrange("(l e kc p n) -> l e p kc n", l=2, e=16, kc=8, p=128, n=512)[l]
        return flat.rearrange("(l e kc p n) -> l e p kc n", l=2, e=16, kc=4, p=128, n=1024)[l]

    with ExitStack() as es:
        mk = lambda name, shape, dt, psum=False: T(S, es, nc, name, shape, dt, psum)
        ident = mk("ident", [128, 128], BF16)
        identf = mk("identf", [128, 128], F32)
        modf = mk("modf", [128, 4, 8, 2], F32)
        zc = mk("zc", [128, 4], F32)
        S.add("pool", lambda e: e.memset(zc[:], 0.0), writes=[zc.b])
        epsc = mk("epsc", [128, 2], F32)
        S.add("pool", lambda e: e.memset(epsc[:], EPS), writes=[epsc.b])
        gates = [[mk("gate%d%d" % (s, k), [128, D], F32) for k in range(2)] for s in range(2)]
        S.add("sp", lambda e: e.dma_start(out=ident[:], in_=ident_d[:, :]), writes=[ident.b], slot=ident.b)
        S.add("sp", lambda e: e.dma_start(out=identf[:], in_=identf_d[:, :]), writes=[identf.b], slot=identf.b)

        def rstd_ops(src, dst, tmp, n, pfx=""):
            S.add("act", lambda e: e.activation(out=tmp[0], in_=src[0], func=AF.Ln, bias=epsc[:, 0:1], scale=1.0 / n), reads=[src[1], epsc.b], writes=[tmp[1]])
            S.add("act", lambda e: e.activation(out=dst[0], in_=tmp[0], func=AF.Exp, scale=-0.5), reads=[tmp[1]], writes=[dst[1]])

        with ExitStack() as es2:
          if "W" in stages:
            mk2 = lambda name, shape, dt, psum=False: T(S, es2, nc, name, shape, dt, psum)
            CH = 4096
            st32 = [mk2("st32_%d" % i, [128, CH], F32) for i in range(2)]
            st16 = [mk2("st16_%d" % i, [128, CH], BF16) for i in range(2)]
            i = 0
            jobs = []
            for k in ("w_in", "w_out"):
                for off in range(0, FW[k], CH):
                    n = min(CH, FW[k] - off)
                    jobs.append((n, wf[k][:, off:off + n], wb[k][:, off:off + n], None))
            for n, src, dst, c3 in jobs:
                a, b = st32[i % 2], st16[i % 2]
                if c3 is None:
                    S.add("sp", lambda e, a=a, src=src, n=n: e.dma_start(out=a[:, 0:n], in_=src), writes=[a.b], slot=a.b)
                else:
                    S.add("sp", lambda e, a=a, src=src, c3=c3: e.dma_start(out=a[:].rearrange("p (c n) -> p c n", c=c3), in_=src), writes=[a.b], slot=a.b)
                if i % 2 == 0:
                    S.add("dve", lambda e, a=a, b=b, n=n: e.tensor_copy(out=b[:, 0:n], in_=a[:, 0:n]), reads=[a.b], writes=[b.b])
                else:
                    S.add("act", lambda e, a=a, b=b, n=n: e.activation(out=b[:, 0:n], in_=a[:, 0:n], func=AF.Copy), reads=[a.b], writes=[b.b])
                S.add("pool", lambda e, b=b, dst=dst, n=n: e.dma_start(out=dst, in_=b[:, 0:n]), reads=[b.b], writes=[S.buf('wtmp')], slot=b.b)
                i += 1
            S.barrier()

        def layer(l):
            with ExitStack() as es2:
              if "M" in stages:
                mk2 = lambda name, shape, dt, psum=False: T(S, es2, nc, name + "_L%d" % l, shape, dt, psum)
                cv = mk2("cv", [128, 16], F32)
                sil = mk2("sil", [128, 16], F32)
                silrep = mk2("silrep", [128, 16, 128], F32)
                wa = [mk2("wa%d" % i, [128, 8, 512], F32) for i in range(2)]
                badT = mk2("badT", [128, 48], F32)
                bbc = [mk2("bbc%d" % i, [128, 512], F32) for i in range(2)]
                pg = [mk2("pg%d" % i, [128, 512], F32, True) for i in range(2)]
                pf = [mk2("pf%d" % i, [128, 8], F32, True) for i in range(2)]
                fbc = [[mk2("fbc%d%d" % (s_, k_), [128, D], F32) for k_ in range(2)] for s_ in range(2)]
                S.add("sp", lambda e: e.dma_start(out=cv[:], in_=cvec[:, :]), writes=[cv.b], slot=cv.b)
                S.add("sp", lambda e: e.dma_start(out=badT[:], in_=b_adaT[l]), writes=[badT.b], slot=badT.b)
                S.add("act", lambda e: e.activation(out=sil[:], in_=cv[:], func=AF.Silu), reads=[cv.b], writes=[sil.b])
                S.add("dve", lambda e: e.tensor_copy(out=silrep[:], in_=sil[:].unsqueeze(2).to_broadcast([128, 16, 128])),
                      reads=[sil.b], writes=[silrep.b])
                wav = w_ada[l].rearrange("(kc p) n -> p kc n", p=128)
                for j in range(12):
                    mod, half = j // 2, j % 2
                    w_ = wa[j % 2]
                    S.add("sp", lambda e, w_=w_, j=j: e.dma_start(out=w_[:], in_=wav[:, :, j * 512:(j + 1) * 512]),
                          writes=[w_.b], slot=w_.b)
                    if mod in (2, 3, 4, 5):
                        bb = bbc[(j // 2) % 2]
                        S.add("sp", lambda e, bb=bb, j=j: e.dma_start(out=bb[:], in_=b_ada[l, j * 512:(j + 1) * 512].partition_broadcast(128)),
                              writes=[bb.b], slot=bb.b)
                        for s in range(2):
                            p_ = pg[s]
                            for kc in range(8):
                                S.add("pe", lambda e, p_=p_, w_=w_, kc=kc, s=s: e.matmul(p_[:], lhsT=silrep[:, kc * 2 + s, :], rhs=w_[:, kc, :],
                                                                                      start=(kc == 0), stop=(kc == 7)),
                                      reads=[silrep.b, w_.b], writes=[p_.b])
                            g_ = {2: gates[s][0], 5: gates[s][1], 3: fbc[s][0], 4: fbc[s][1]}[mod]
                            S.add("dve", lambda e, p_=p_, g_=g_, bb=bb, half=half, mod=mod: e.scalar_tensor_tensor(out=g_[:, half * 512:(half + 1) * 512], in0=p_[:], scalar=(1.0 if mod == 4 else 0.0), in1=bb[:], op0=ALU.add, op1=ALU.add),
                                  reads=[p_.b, bb.b], writes=[g_.b])
                    if mod in (0, 1, 3, 4):
                        kind = {0: 0, 1: 1, 3: 2, 4: 3}[mod]
                        p_ = pf[j % 2]
                        for blk in range(4):
                            for kc in range(8):
                                S.add("pe", lambda e, p_=p_, w_=w_, kc=kc, blk=blk: e.matmul(p_[:, blk * 2:blk * 2 + 2], lhsT=w_[:, kc, blk * 128:(blk + 1) * 128],
                                                                                          rhs=sil[:, kc * 2:kc * 2 + 2], start=(kc == 0), stop=(kc == 7)),
                                      reads=[sil.b, w_.b], writes=[p_.b])
                        for blk in range(4):
                            ch = half * 4 + blk
                            S.add("dve", lambda e, p_=p_, blk=blk, ch=ch, kind=kind, mod=mod: e.tensor_scalar(
                                out=modf[:, kind, ch, :], in0=p_[:, blk * 2:blk * 2 + 2], scalar1=badT[:, mod * 8 + ch:mod * 8 + ch + 1],
                                scalar2=(1.0 if kind in (1, 3) else 0.0), op0=ALU.add, op1=ALU.add),
                                reads=[p_.b, badT.b], writes=[modf.b])
                for s_ in range(2):
                    for k_ in range(2):
                        f_ = fbc[s_][k_]
                        S.add("sp", lambda e, f_=f_, s_=s_, k_=k_: e.dma_start(out=fbc_d[s_ * 2 + k_], in_=f_[:]), reads=[f_.b], writes=[S.buf("fbcd")], slot=f_.b)
                S.barrier()

            kv_lo, kv_hi = (0, 40) if l == 0 else (2, 38)
            q_lo, q_hi = (2, 38) if l == 0 else (4, 36)
            if tile_limit is not None:
                kv_hi = min(kv_hi, tile_limit)
                q_hi = min(q_hi, tile_limit - 3)
            xin = xw if l == 0 else x1
            cin = ctx0 if l == 0 else ctx1
            xin_b = dbuf("xin%d" % l) if l == 0 else dbuf("x1")
            cin_b = dbuf("cin%d" % l) if l == 0 else dbuf("ctx1")

            if dbg and "M" in stages:
                S.add("sp", lambda e: e.dma_start(out=dbg_modf[l], in_=modf[:].rearrange("p a b c -> p (a b c)")), reads=[modf.b], writes=[S.buf("dbgm")], slot=modf.b)
                for s_i in range(2):
                    for k_i in range(2):
                        g_ = gates[s_i][k_i]
                        S.add("sp", lambda e, g_=g_, s_i=s_i, k_i=k_i: e.dma_start(out=dbg_gates[l, s_i * 2 + k_i], in_=g_[:]), reads=[g_.b], writes=[S.buf("dbgg")], slot=g_.b)
                S.barrier()
            with ExitStack() as es2:
              if "PA" in stages:
                mk2 = lambda name, shape, dt, psum=False: T(S, es2, nc, name + "_L%d" % l, shape, dt, psum)
                Win = mk2("Win", [128, 8, 2560], BF16)
                Wout = mk2("Wout", [128, 8, 1024], BF16)
                Eint = mk2("Eint", [128, 6, 1024], BF16)
                Esp = mk2("Esp", [128, 6, 1024], BF16)
                bst = [mk2("bst%d" % i, [128, 1024], F32) for i in range(1)]
                g2 = mk2("g2", [128, 4], F32)
                slnb = mk2("slnb", [128, 512], F32)
                swTb = mk2("swTb", [128, 1024], BF16)
                sb = mk2("sb", [128, 8], F32)
                og = mk2("og", [128, 8], F32)
                xt = [mk2("xt%d" % i, [128, D], F32) for i in range(2)]
                junk = mk2("junk", [128, D], BF16)
                xn = [mk2("xn%d" % i, [128, D], BF16) for i in range(2)]
                hT = [mk2("hT%d" % i, [128, 8, 128], BF16) for i in range(2)]
                qk = mk2("qk", [128, D], F32)
                guv = mk2("guv", [128, D], F32)
                sqb = mk2("sqb", [128, D], F32)
                qkn = mk2("qkn", [128, D], BF16)
                st = [mk2("stat%d" % i, [128, 64], F32) for i in range(2)]
                KT = [mk2("KT%d" % i, [128, 4, 128], BF16) for i in range(8)]
                QT = [mk2("QT%d" % i, [128, 4, 128], BF16) for i in range(4)]
                V = [mk2("V%d" % i, [128, 520], BF16) for i in range(8)]
                cKT = [mk2("cKT%d" % i, [128, 4, 128], BF16) for i in range(2)]
                cQT = [mk2("cQT%d" % i, [128, 4, 128], BF16) for i in range(2)]
                cV = [mk2("cV%d" % i, [128, 520], BF16) for i in range(2)]
                vsn = mk2("vsn", [128, 512], BF16)
                t1 = mk2("t1", [128, 512], F32)
                osg = [mk2("osg%d" % i, [128, 512], F32) for i in range(4)]
                cosg = [mk2("cosg%d" % i, [128, 512], F32) for i in range(2)]
                rsg = [mk2("rsg%d" % i, [128, 4], F32) for i in range(4)]
                crsg = [mk2("crsg%d" % i, [128, 4], F32) for i in range(2)]
                pT = [mk2("pT%d" % i, [128, 1024], BF16) for i in range(2)]
                ona = mk2("ona", [128, 512], F32)
                ast = mk2("ast", [128, 16], F32)
                yn = mk2("yn", [128, D], BF16)
                yT = mk2("yT", [128, 8, 128], BF16)
                xa = mk2("xa", [128, D], F32)
                xo = [mk2("xo%d" % i, [128, D], F32) for i in range(1)]
                wjobs = []
                if l == 0:
                    wst32 = [mk2("wst32_%d" % i, [128, 1024], F32) for i in range(2)]
                    wst16 = [mk2("wst16_%d" % i, [128, 1024], BF16) for i in range(1)]
                    for le in range(32):
                        for k_ in ("w1", "w3", "w2"):
                            for q_ in range(4):
                                v_ = wf[k_][le].rearrange("(c p) n -> p c n", p=128)
                                src = v_[:, 2 * q_:2 * q_ + 2, :] if k_ != "w2" else v_[:, q_:q_ + 1, :]
                                wjobs.append((src, wb[k_][le * 128:(le + 1) * 128, q_ * 1024:(q_ + 1) * 1024]))
                wcnt = {"i": 0}

                def emit_wjobs(n):
                    for _ in range(n):
                        if wcnt["i"] >= len(wjobs):
                            return
                        src, dst = wjobs[wcnt["i"]]
                        a, b = wst32[wcnt["i"] % 2], wst16[0]
                        wcnt["i"] += 1
                        c3 = src.shape[1]
                        S.add("sp", lambda e, a=a, src=src, c3=c3: e.dma_start(out=a[:].rearrange("p (c n) -> p c n", c=c3), in_=src), writes=[a.b], slot=a.b)
                        S.add("pool", lambda e, a=a, b=b: e.tensor_copy(out=b[:], in_=a[:]), reads=[a.b], writes=[b.b])
                        S.add("pool", lambda e, b=b, dst=dst: e.dma_start(out=dst, in_=b[:]), reads=[b.b], writes=[S.buf("wtmp")], slot=b.b)

                P = [mk2("P%d" % i, [128, 512], F32, True) for i in range(1)]
                T0 = mk2("T0", [128, 1024], BF16, True)
                T1 = mk2("T1", [128, 1024], BF16, True)
                Sb = [mk2("S%d" % i, [128, 512], F32, True) for i in range(4)]
                O0 = mk2("O0", [128, 512], F32, True)

                S.add("sp", lambda e: e.dma_start(out=Win[:], in_=wview("w_in", l)), reads=[dbuf("w_in")], writes=[Win.b], slot=Win.b)
                S.add("sp", lambda e: e.dma_start(out=Wout[:], in_=wview("w_out", l)), reads=[dbuf("w_out")], writes=[Wout.b], slot=Wout.b)

                def load_E(dst, sidx):
                    for o in range(6):
                        b_ = bst[0]
                        S.add("sp", lambda e, b_=b_, o=o: e.dma_start(out=b_[:], in_=bias[l, sidx, o]), writes=[b_.b], slot=b_.b)
                        S.add("act", lambda e, b_=b_, o=o: e.activation(out=dst[:, o, :], in_=b_[:], func=AF.Exp), reads=[b_.b], writes=[dst.b])

                load_E(Eint, 0)
                S.add("sp", lambda e: e.dma_start(out=g2[:, 0:1], in_=qg[l]), writes=[g2.b], slot=g2.b)
                S.add("sp", lambda e: e.dma_start(out=g2[:, 1:2], in_=kg[l]), writes=[g2.b], slot=g2.b)
                S.add("dve", lambda e: e.scalar_tensor_tensor(out=g2[:, 2:3], in0=g2[:, 0:1], scalar=0.125, in1=g2[:, 1:2], op0=ALU.mult, op1=ALU.mult),
                      reads=[g2.b], writes=[g2.b])
                S.add("sp", lambda e: e.dma_start(out=slnb[:], in_=sln[l].partition_broadcast(128)), writes=[slnb.b], slot=slnb.b)
                S.add("sp", lambda e: e.dma_start(out=bst[0][:], in_=swT[l]), writes=[bst[0].b], slot=bst[0].b)
                S.add("dve", lambda e: e.tensor_copy(out=swTb[:], in_=bst[0][:]), reads=[bst[0].b], writes=[swTb.b])
                S.add("sp", lambda e: e.dma_start(out=sb[:], in_=sbT[l]), writes=[sb.b], slot=sb.b)
                S.add("sp", lambda e: e.dma_start(out=og[:], in_=ogT[l]), writes=[og.b], slot=og.b)
                for v_ in V + cV:
                    S.add("pool", lambda e, v_=v_: e.memset(v_[:].rearrange("p (h c) -> p h c", c=65)[:, :, 64:65], 1.0), writes=[v_.b])

                cnt = {"p": 0}

                def proj(src_ap, src_b, stream, kt, vt, qt, osg_t, rsg_t, need_q):
                    i = cnt["p"] % 2
                    cnt["p"] += 1
                    x_, xn_, h_, s_ = xt[i], xn[i], hT[i], st[i]
                    S.add("sp", lambda e: e.dma_start(out=x_[:], in_=src_ap), reads=[src_b], writes=[x_.b], slot=x_.b)
                    S.add("act", lambda e: e.activation(out=junk[:], in_=x_[:], func=AF.Square, accum_out=s_[:, 0:1]), reads=[x_.b], writes=[junk.b, s_.b])
                    rstd_ops((s_[:, 0:1], s_.b), (s_[:, 2:3], s_.b), (s_[:, 1:2], s_.b), D)
                    S.add("dve", lambda e: e.tensor_scalar(out=xn_[:], in0=x_[:], scalar1=s_[:, 2:3], scalar2=None, op0=ALU.mult), reads=[x_.b, s_.b], writes=[xn_.b])
                    yield
                    for kc in range(8):
                        S.add("pe", lambda e, kc=kc: e.transpose(out=T0[:, kc * 128:(kc + 1) * 128], in_=xn_[:, kc * 128:(kc + 1) * 128], identity=ident[:]),
                              reads=[xn_.b, ident.b], writes=[T0.b])
                    yield
                    for kc in range(8):
                        if kc % 2 == 0:
                            S.add("act", lambda e, kc=kc: e.activation(out=h_[:, kc, :], in_=T0[:, kc * 128:(kc + 1) * 128], func=AF.Identity,
                                                                      bias=modf[:, 0, kc, stream:stream + 1], scale=modf[:, 1, kc, stream:stream + 1]),
                                  reads=[T0.b, modf.b], writes=[h_.b])
                        else:
                            S.add("dve", lambda e, kc=kc: e.tensor_scalar(out=h_[:, kc, :], in0=T0[:, kc * 128:(kc + 1) * 128], scalar1=modf[:, 1, kc, stream:stream + 1],
                                                                         scalar2=modf[:, 0, kc, stream:stream + 1], op0=ALU.mult, op1=ALU.add),
                                  reads=[T0.b, modf.b], writes=[h_.b])
                    yield
                    plevel = float(_os.environ.get("DBG_P", "9"))
                    if plevel < 2:
                        return
                    chunks = [0, 1, 2, 3, 4] if need_q else [1, 2]
                    for nch in chunks:
                        yield
                        p_ = P[0]
                        for kc in range(8):
                            S.add("pe", lambda e, p_=p_, kc=kc, nch=nch: e.matmul(p_[:], lhsT=h_[:, kc, :], rhs=Win[:, kc, nch * 512:(nch + 1) * 512], start=(kc == 0), stop=(kc == 7)),
                                  reads=[h_.b, Win.b], writes=[p_.b])
                        if nch < 2:
                            S.add("act", lambda e, p_=p_, nch=nch: e.activation(out=qk[:, nch * 512:(nch + 1) * 512], in_=p_[:], func=AF.Copy), reads=[p_.b], writes=[qk.b])
                        elif nch == 2:
                            S.add("dve", lambda e, p_=p_: e.tensor_copy(out=vt[:].rearrange("p (h c) -> p h c", c=65)[:, :, 0:64], in_=p_[:].rearrange("p (h c) -> p h c", c=64)),
                                  reads=[p_.b], writes=[vt.b])
                        elif nch == 3:
                            S.add("act", lambda e, p_=p_: e.activation(out=guv[:, 0:512], in_=p_[:], func=AF.Gelu_apprx_tanh), reads=[p_.b], writes=[guv.b])
                        else:
                            S.add("act", lambda e, p_=p_: e.activation(out=guv[:, 512:1024], in_=p_[:], func=AF.Gelu_apprx_tanh, accum_out=s_[:, 4:5]), reads=[p_.b], writes=[guv.b, s_.b])
                    yield
                    if plevel < 2.2:
                        return
                    c0, nh = (0, 16) if need_q else (512, 8)
                    S.add("dve", lambda e: e.tensor_tensor(out=sqb[:, c0:1024], in0=qk[:, c0:1024], in1=qk[:, c0:1024], op=ALU.mult), reads=[qk.b], writes=[sqb.b])
                    S.add("dve", lambda e: e.tensor_reduce(out=s_[:, 16:16 + nh], in_=sqb[:, c0:1024].rearrange("p (h c) -> p h c", c=64), axis=AX.X, op=ALU.add),
                          reads=[sqb.b], writes=[s_.b])
                    rstd_ops((s_[:, 16:16 + nh], s_.b), (s_[:, 48:48 + nh], s_.b), (s_[:, 32:32 + nh], s_.b), 64)
                    yield
                    if plevel < 2.5:
                        return
                    S.add("dve", lambda e: e.tensor_tensor(out=qkn[:, c0:1024].rearrange("p (h c) -> p h c", c=64), in0=qk[:, c0:1024].rearrange("p (h c) -> p h c", c=64),
                                                          in1=s_[:, 48:48 + nh].unsqueeze(2).to_broadcast([128, nh, 64]), op=ALU.mult), reads=[qk.b, s_.b], writes=[qkn.b])
                    yield
                    for j in range(0 if need_q else 4, 8):
                        S.add("pe", lambda e, j=j: e.transpose(out=T0[:, j * 128:(j + 1) * 128], in_=qkn[:, j * 128:(j + 1) * 128], identity=ident[:]),
                              reads=[qkn.b, ident.b], writes=[T0.b])
                    yield
                    if plevel < 2.8:
                        return
                    ev = _os.environ.get("DBG_EV", "both")
                    if need_q and ev in ("q", "both"):
                        S.add("act", lambda e: e.activation(out=qt[:].rearrange("p a b -> p (a b)"), in_=T0[:, 0:512], func=AF.Identity, scale=g2[:, 2:3]),
                              reads=[T0.b, g2.b], writes=[qt.b])
                    if ev in ("k", "both"):
                        S.add("act", lambda e: e.activation(out=kt[:].rearrange("p a b -> p (a b)"), in_=T0[:, 512:1024], func=AF.Copy), reads=[T0.b], writes=[kt.b])
                    yield
                    if not need_q or plevel < 4:
                        return
                    S.add("dve", lambda e: e.tensor_scalar(out=s_[:, 5:6], in0=s_[:, 4:5], scalar1=-1.0 / 512, scalar2=None, op0=ALU.mult), reads=[s_.b], writes=[s_.b])
                    S.add("act", lambda e: e.activation(out=junk[:, 0:512], in_=guv[:, 512:1024], func=AF.Square, bias=s_[:, 5:6], accum_out=s_[:, 6:7]),
                          reads=[guv.b, s_.b], writes=[junk.b, s_.b])
                    rstd_ops((s_[:, 6:7], s_.b), (s_[:, 8:9], s_.b), (s_[:, 7:8], s_.b), 512)
                    S.add("dve", lambda e: e.tensor_scalar(out=vsn[:], in0=guv[:, 512:1024], scalar1=s_[:, 5:6], scalar2=s_[:, 8:9], op0=ALU.add, op1=ALU.mult),
                          reads=[guv.b, s_.b], writes=[vsn.b])
                    yield
                    p_ = P[0]
                    for h in range(8):
                        S.add("pe", lambda e, h=h: e.matmul(p_[:, h * 64:(h + 1) * 64], lhsT=swTb[:, h * 128:(h + 1) * 128], rhs=vsn[:, h * 64:(h + 1) * 64], start=True, stop=True),
                              reads=[swTb.b, vsn.b], writes=[p_.b])
                    yield
                    S.add("dve", lambda e: e.tensor_tensor(out=t1[:], in0=p_[:], in1=slnb[:], op=ALU.mult), reads=[p_.b, slnb.b], writes=[t1.b])
                    S.add("dve", lambda e: e.tensor_tensor(out=t1[:].rearrange("p (h c) -> p h c", c=64), in0=t1[:].rearrange("p (h c) -> p h c", c=64),
                                                          in1=sb[:].unsqueeze(2).to_broadcast([128, 8, 64]), op=ALU.add), reads=[t1.b, sb.b], writes=[t1.b])
                    S.add("dve", lambda e: e.tensor_tensor(out=osg_t[:], in0=t1[:], in1=guv[:, 0:512], op=ALU.mult), reads=[t1.b, guv.b], writes=[osg_t.b])
                    yield
                    S.add("act", lambda e: e.activation(out=junk[:, 512:1024], in_=osg_t[:], func=AF.Square, accum_out=rsg_t[:, 0:1]), reads=[osg_t.b], writes=[junk.b, rsg_t.b])
                    rstd_ops((rsg_t[:, 0:1], rsg_t.b), (rsg_t[:, 2:3], rsg_t.b), (rsg_t[:, 1:2], rsg_t.b), 512)

                acnt = {"a": 0}

                def attn(src_ap, src_b, dst_ap, dst_b, stream, qt, keys, Eset, osg_t, rsg_t):
                    nk = len(keys)
                    nw = sum(1 for k_ in keys if k_[2] is not None)
                    def emit_qk(h):
                        hp, po = h // 2, (h % 2) * 64
                        SA, SB2 = Sb[(h % 2) * 2], Sb[(h % 2) * 2 + 1]
                        for i, (kt, vt, eo) in enumerate(keys):
                            bank = SA if i < 4 else SB2
                            col = (i % 4) * 128
                            S.add("pe", lambda e, kt=kt, bank=bank, col=col, po=po, hp=hp: e.matmul(bank[:, col:col + 128], lhsT=kt[po:po + 64, hp, :], rhs=qt[po:po + 64, hp, :], start=True, stop=True),
                                  reads=[kt.b, qt.b], writes=[bank.b])

                    emit_qk(0)
                    for h in range(8):
                        SA, SB2 = Sb[(h % 2) * 2], Sb[(h % 2) * 2 + 1]
                        p_ = pT[h % 2]
                        if h + 1 < 8:
                            emit_qk(h + 1)
                        yield
                        na = min(nk, 4)
                        S.add("act", lambda e, SA=SA, p_=p_, na=na: e.activation(out=p_[:, 0:na * 128], in_=SA[:, 0:na * 128], func=AF.Exp), reads=[SA.b], writes=[p_.b])
                        if nk > 4:
                            S.add("act", lambda e, SB2=SB2, p_=p_: e.activation(out=p_[:, 512:nk * 128], in_=SB2[:, 0:(nk - 4) * 128], func=AF.Exp), reads=[SB2.b], writes=[p_.b])
                        yield
                        if nw > 0:
                            S.add("dve", lambda e, p_=p_, h=h: e.tensor_tensor(out=p_[:, 0:nw * 128].rearrange("p (a b) -> p a b", b=128), in0=p_[:, 0:nw * 128].rearrange("p (a b) -> p a b", b=128),
                                                                              in1=Eset[:, 0:nw, h * 128:(h + 1) * 128], op=ALU.mult), reads=[p_.b, Eset.b], writes=[p_.b])
                        yield
                        oc = (h % 4) * 65
                        for i, (kt, vt, eo) in enumerate(keys):
                            S.add("pe", lambda e, vt=vt, i=i, p_=p_, oc=oc, h=h: e.matmul(O0[:, oc:oc + 65], lhsT=p_[:, i * 128:(i + 1) * 128], rhs=vt[:, h * 65:(h + 1) * 65], start=(i == 0), stop=(i == nk - 1)),
                                  reads=[p_.b, vt.b], writes=[O0.b])
                        yield
                        if h % 4 == 3:
                            hb = h - 3
                            S.add("dve", lambda e, hb=hb: e.reciprocal(out=ast[:, hb:hb + 4], in_=O0[:, 0:260].rearrange("p (h c) -> p h c", c=65)[:, :, 64]), reads=[O0.b], writes=[ast.b])
                            S.add("dve", lambda e, hb=hb: e.tensor_tensor(out=ona[:, hb * 64:(hb + 4) * 64].rearrange("p (h c) -> p h c", c=64), in0=O0[:, 0:260].rearrange("p (h c) -> p h c", c=65)[:, :, 0:64],
                                                                         in1=ast[:, hb:hb + 4].unsqueeze(2).to_broadcast([128, 4, 64]), op=ALU.mult), reads=[O0.b, ast.b], writes=[ona.b])
                    yield
                    S.add("act", lambda e: e.activation(out=junk[:, 0:512], in_=ona[:], func=AF.Square, accum_out=ast[:, 8:9]), reads=[ona.b], writes=[junk.b, ast.b])
                    rstd_ops((ast[:, 8:9], ast.b), (ast[:, 10:11], ast.b), (ast[:, 9:10], ast.b), 512)
                    S.add("dve", lambda e: e.tensor_scalar(out=yn[:, 0:512], in0=ona[:], scalar1=ast[:, 10:11], scalar2=None, op0=ALU.mult), reads=[ona.b, ast.b], writes=[yn.b])
                    S.add("dve", lambda e: e.tensor_scalar(out=yn[:, 512:1024], in0=osg_t[:], scalar1=rsg_t[:, 2:3], scalar2=None, op0=ALU.mult), reads=[osg_t.b, rsg_t.b], writes=[yn.b])
                    yield
                    for kc in range(8):
                        S.add("pe", lambda e, kc=kc: e.transpose(out=T1[:, kc * 128:(kc + 1) * 128], in_=yn[:, kc * 128:(kc + 1) * 128], identity=ident[:]), reads=[yn.b, ident.b], writes=[T1.b])
                    for kc in range(8):
                        if kc % 2 == 0:
                            S.add("act", lambda e, kc=kc: e.activation(out=yT[:, kc, :], in_=T1[:, kc * 128:(kc + 1) * 128], func=AF.Identity, scale=og[:, kc:kc + 1]),
                                  reads=[T1.b, og.b], writes=[yT.b])
                        else:
                            S.add("dve", lambda e, kc=kc: e.tensor_scalar(out=yT[:, kc, :], in0=T1[:, kc * 128:(kc + 1) * 128], scalar1=og[:, kc:kc + 1], scalar2=zc[:, 0:1], op0=ALU.mult, op1=ALU.add),
                                  reads=[T1.b, og.b, zc.b], writes=[yT.b])
                    yield
                    S.add("sp", lambda e: e.dma_start(out=xa[:], in_=src_ap), reads=[src_b], writes=[xa.b], slot=xa.b)
                    xo_ = xo[0]
                    acnt["a"] += 1
                    for half in range(2):
                        yield
                        p_ = Sb[half]
                        for kc in range(8):
                            S.add("pe", lambda e, p_=p_, kc=kc, half=half: e.matmul(p_[:], lhsT=yT[:, kc, :], rhs=Wout[:, kc, half * 512:(half + 1) * 512], start=(kc == 0), stop=(kc == 7)),
                                  reads=[yT.b, Wout.b], writes=[p_.b])
                        S.add("dve", lambda e, p_=p_, half=half: e.tensor_tensor(out=xo_[:, half * 512:(half + 1) * 512], in0=p_[:], in1=gates[stream][0][:, half * 512:(half + 1) * 512], op=ALU.mult),
                              reads=[p_.b, gates[stream][0].b], writes=[xo_.b])
                    S.add("dve", lambda e: e.tensor_tensor(out=xo_[:], in0=xo_[:], in1=xa[:], op=ALU.add), reads=[xo_.b, xa.b], writes=[xo_.b])
                    S.add("pool", lambda e: e.dma_start(out=dst_ap, in_=xo_[:]), reads=[xo_.b], writes=[dst_b], slot=xo_.b)

                pa_mode = _os.environ.get("DBG_PA", "full")
                def run(*gens):
                    gens = list(gens)
                    while gens:
                        for g_ in list(gens):
                            try:
                                next(g_)
                            except StopIteration:
                                gens.remove(g_)

                for ci in range(2 if pa_mode != "setup" else 0):
                    run(proj(cin[ci * 128:(ci + 1) * 128, :], cin_b, 1, cKT[ci], cV[ci], cQT[ci], cosg[ci], crsg[ci], l == 0))
                if l == 0 and pa_mode == "full":
                    for ci in range(2):
                        run(attn(cin[ci * 128:(ci + 1) * 128, :], cin_b, ctxm[ci * 128:(ci + 1) * 128, :], dbuf("ctxm"), 1, cQT[ci],
                                 [(cKT[0], cV[0], None), (cKT[1], cV[1], None)], Eint, cosg[ci], crsg[ci]))

                def do_attn(t):
                    if t in (4, 5, 34, 35):
                        sidx = {4: 1, 5: 2, 34: 3, 35: 4}[t]
                        load_E(Esp, sidx)
                        offs = list(range(-2, 4)) if t in (4, 5) else list(range(-3, 3))
                        Eset = Esp
                    else:
                        offs = list(range(-2, 3))
                        Eset = Eint
                    keys = [(KT[(t + o) % 8], V[(t + o) % 8], i) for i, o in enumerate(offs)]
                    keys += [(cKT[0], cV[0], None), (cKT[1], cV[1], None)]
                    return attn(xin[t * 128:(t + 1) * 128, :], xin_b, xm[t * 128:(t + 1) * 128, :], dbuf("xm"), 0, QT[t % 4], keys, Eset, osg[t % 4], rsg[t % 4])

                if pa_mode == "setup":
                    kv_hi = kv_lo
                if pa_mode != "full":
                    q_hi = q_lo
                for t in range(kv_lo, kv_hi):
                    nq = q_lo <= t < q_hi
                    pg_ = proj(xin[t * 128:(t + 1) * 128, :], xin_b, 0, KT[t % 8], V[t % 8], QT[t % 4], osg[t % 4], rsg[t % 4], nq)
                    ta = t - 3
                    if q_lo <= ta < q_hi:
                        if ta in (4, 5):
                            run(pg_)
                            run(do_attn(ta))
                        else:
                            run(pg_, do_attn(ta))
                    else:
                        run(pg_)
                    emit_wjobs(10)
                emit_wjobs(len(wjobs))
                for t in range(kv_hi - 3, q_hi):
                    if t >= q_lo:
                        run(do_attn(t))
                S.barrier()

            if not do_f:
                return
            with ExitStack() as es2:
              if "F" in stages:
                U32 = mybir.dt.uint32
                mk2 = lambda name, shape, dt, psum=False: T(S, es2, nc, name + "_L%d" % l, shape, dt, psum)
                TS = 512
                tiles_all = []
                if l == 0:
                    for ci in range(2):
                        tiles_all.append((ctxm[ci * 128:(ci + 1) * 128, :], dbuf("ctxm"), ctx1[ci * 128:(ci + 1) * 128, :], dbuf("ctx1"), 1, 38 + ci))
                for t in range(q_lo, q_hi):
                    if l == n_layers - 1 and l == 1:
                        dap, db_ = out[(t - 4) * 128:(t - 3) * 128, :], dbuf("out")
                    else:
                        dap, db_ = x1[t * 128:(t + 1) * 128, :], dbuf("x1")
                    tiles_all.append((xm[t * 128:(t + 1) * 128, :], dbuf("xm"), dap, db_, 0, t if t < 38 else t))
                NTOK = len(tiles_all)
                NT = (2 * NTOK * 128 + TS - 1) // TS + 16
                assert NT <= NTMAX
                ew1 = [mk2("ew1_%d" % i, [128, 4096], BF16) for i in range(2)]
                ew3 = [mk2("ew3_%d" % i, [128, 4096], BF16) for i in range(2)]
                ew2 = [mk2("ew2_%d" % i, [128, 4096], BF16) for i in range(2)]
                xg = mk2("xg", [128, 4, D], F32)
                junkf = mk2("junkf", [128, D], BF16)
                xn32 = mk2("xn32", [128, D], F32)
                hf32 = mk2("hf32", [128, 8, 128], F32)
                hs = [mk2("hs%d" % i, [128, D], BF16) for i in range(2)]
                rwt = mk2("rwt", [128, 8, 20], F32)
                rbb = mk2("rbb", [128, 20], F32)
                lg = mk2("lg", [128, 4, 20], F32)
                r = {n_: mk2("r_" + n_, [128, 4, 16], F32) for n_ in ("a", "b", "c", "d", "e", "f", "g", "h", "i", "j", "k")}
                fs = mk2("fs", [128, 16], F32)
                OH = [mk2("OH%d" % i, [128, NTOK, 16], F32) for i in range(2)]
                POS = mk2("POS", [128, NTOK, 16], F32)
                WAB = [mk2("WAB%d" % i, [128, NTOK], F32) for i in range(2)]
                base = mk2("base", [128, 16], F32)
                sel = mk2("sel", [128, 4, 16], BF16)
                cU = mk2("cU", [128, 128], BF16)
                cOnes = mk2("cOnes", [128, 128], BF16)
                itc = mk2("itc", [128, NTMAX, 16], F32)
                cmpt = mk2("cmpt", [128, NTMAX, 16], F32)
                pidx = mk2("pidx", [128, 2], F32)
                offs = mk2("offs", [128, 4, 16], F32)
                eid = mk2("eid", [128, NTMAX], F32)
                widx = mk2("widx", [128, NTMAX], U32)
                slf = [mk2("slf%d" % i, [128, NTOK], F32) for i in range(2)]
                slu = [mk2("slu%d" % i, [128, NTOK], U32) for i in range(2)]
                ptmp = mk2("ptmp", [128, NTOK, 16], F32)
                srt = [mk2("srt%d" % i, [128, 4, D], BF16) for i in range(2)]
                hfT = mk2("hfT", [128, 8, 512], BF16)
                sl = [mk2("sl%d" % i, [128, 512], BF16) for i in range(2)]
                hidT = [mk2("hidT%d" % i, [128, 4, 512], BF16) for i in range(2)]
                ob = [mk2("ob%d" % i, [128, D], F32) for i in range(2)]
                ra = mk2("ra", [128, D], F32)
                rb_ = mk2("rb", [128, D], F32)
                xr = mk2("xr", [128, D], F32)
                xo = [mk2("fxo%d" % i, [128, D], F32) for i in range(1)]
                H1 = [mk2("H1_%d" % i, [128, 512], F32, True) for i in range(2)]
                H3 = [mk2("H3_%d" % i, [128, 512], F32, True) for i in range(2)]
                OUT = [mk2("OUT%d" % i, [128, 512], F32, True) for i in range(2)]
                TB = mk2("TB", [128, 1024], BF16, True)
                TF = mk2("TF", [128, 512], F32, True)
                S.add("sp", lambda e: e.dma_start(out=rwt[:], in_=rw[l].rearrange("(kc p) n -> p kc n", p=128)), writes=[rwt.b], slot=rwt.b)
                S.add("sp", lambda e: e.dma_start(out=rbb[:], in_=rb[l].partition_broadcast(128)), writes=[rbb.b], slot=rbb.b)
                fbc = [[mk2("ffbc%d%d" % (s_, k_), [128, D], F32) for k_ in range(2)] for s_ in range(2)]
                for s_ in range(2):
                    for k_ in range(2):
                        f_ = fbc[s_][k_]
                        S.add("sp", lambda e, f_=f_, s_=s_, k_=k_: e.dma_start(out=f_[:], in_=fbc_d[s_ * 2 + k_]), writes=[f_.b], slot=f_.b)
                S.add("sp", lambda e: e.dma_start(out=cU[:], in_=cU_d[:, :]), writes=[cU.b], slot=cU.b)
                S.add("sp", lambda e: e.dma_start(out=cOnes[:], in_=cOnes_d[:, :]), writes=[cOnes.b], slot=cOnes.b)
                S.add("sp", lambda e: e.dma_start(out=itc[:].rearrange("p a b -> p (a b)"), in_=itc_d[:, :]), writes=[itc.b], slot=itc.b)
                S.add("sp", lambda e: e.dma_start(out=pidx[:], in_=pidx_d[:, :]), writes=[pidx.b], slot=pidx.b)
                S.add("pool", lambda e: e.memset(base[:], 0.0), writes=[base.b])
                S.add("pool", lambda e: e.memset(hfT[:], 0.0), writes=[hfT.b])
                srt_flat = srt_d.rearrange("r n -> (r n)").rearrange("(p f) -> p f", p=128)
                for i in range(NT):
                    S.add("sp", lambda e, i=i: e.dma_start(out=srt_d[i * 512:(i + 1) * 512, :].rearrange("(p a) n -> p (a n)", p=128), in_=hfT[:].rearrange("p a b -> p (a b)")),
                          reads=[hfT.b], writes=[dbuf("srt") if i == NT - 1 else S.buf("zf")], slot=hfT.b)

                def dv(fn, reads, writes):
                    S.add("dve", fn, reads=[x_.b for x_ in reads], writes=[x_.b for x_ in writes])

                hcnt = {"h": 0}

                def route_group(idx0, G):
                    for ti in range(G):
                        sap, sbuf_, dap, dbuf_, stream, hrow = tiles_all[idx0 + ti]
                        S.add("sp", lambda e, ti=ti, sap=sap: e.dma_start(out=xg[:, ti, :], in_=sap), reads=[sbuf_], writes=[xg.b], slot=xg.b)
                    for ti in range(G):
                        sap, sbuf_, dap, dbuf_, stream, hrow = tiles_all[idx0 + ti]
                        S.add("act", lambda e, ti=ti: e.activation(out=junkf[:], in_=xg[:, ti, :], func=AF.Square, accum_out=fs[:, 0:1]), reads=[xg.b], writes=[junkf.b, fs.b])
                        rstd_ops((fs[:, 0:1], fs.b), (fs[:, 2:3], fs.b), (fs[:, 1:2], fs.b), D)
                        S.add("dve", lambda e, ti=ti: e.tensor_scalar(out=xn32[:], in0=xg[:, ti, :], scalar1=fs[:, 2:3], scalar2=None, op0=ALU.mult), reads=[xg.b, fs.b], writes=[xn32.b])
                        h_ = hs[hcnt["h"] % 2]
                        hcnt["h"] += 1
                        S.add("dve", lambda e, stream=stream: e.tensor_tensor(out=ra[:], in0=xn32[:], in1=fbc[stream][1][:], op=ALU.mult), reads=[xn32.b, fbc[stream][1].b], writes=[ra.b])
                        S.add("dve", lambda e, stream=stream, h_=h_: e.tensor_tensor(out=h_[:], in0=ra[:], in1=fbc[stream][0][:], op=ALU.add), reads=[ra.b, fbc[stream][0].b], writes=[h_.b])
                        S.add("pool", lambda e, h_=h_, hrow=hrow: e.dma_start(out=hfd[hrow * 128:(hrow + 1) * 128, :], in_=h_[:]), reads=[h_.b], writes=[dbuf("hfd")], slot=h_.b)
                        for half in range(2):
                            for kc in range(4):
                                S.add("pe", lambda e, kc=kc, half=half: e.transpose(out=TF[:, kc * 128:(kc + 1) * 128], in_=xn32[:, (half * 4 + kc) * 128:(half * 4 + kc + 1) * 128], identity=identf[:]),
                                      reads=[xn32.b, identf.b], writes=[TF.b])
                            for kc in range(4):
                                k8 = half * 4 + kc
                                S.add("dve", lambda e, kc=kc, k8=k8, stream=stream: e.tensor_scalar(out=hf32[:, k8, :], in0=TF[:, kc * 128:(kc + 1) * 128], scalar1=modf[:, 3, k8, stream:stream + 1],
                                                                                                 scalar2=modf[:, 2, k8, stream:stream + 1], op0=ALU.mult, op1=ALU.add), reads=[TF.b, modf.b], writes=[hf32.b])
                        for kc in range(8):
                            S.add("pe", lambda e, kc=kc, ti=ti: e.matmul(H1[0][:, ti * 20:(ti + 1) * 20], lhsT=hf32[:, kc, :], rhs=rwt[:, kc, :], start=(kc == 0), stop=(kc == 7)),
                                  reads=[hf32.b, rwt.b], writes=[H1[0].b])
                        S.add("dve", lambda e, ti=ti: e.tensor_tensor(out=lg[:, ti, :], in0=H1[0][:, ti * 20:(ti + 1) * 20], in1=rbb[:], op=ALU.add), reads=[H1[0].b, rbb.b], writes=[lg.b])
                    lgG = lg[:, 0:G, 0:4]
                    lgE = lg[:, 0:G, 4:20]
                    mg, ohg, dg, eg, sg, pt = r["a"], r["b"], r["c"], r["d"], r["e"], r["f"]
                    dv(lambda e: e.tensor_reduce(out=mg[:, 0:G, 0], in_=lgG, axis=AX.X, op=ALU.max), [lg], [mg])
                    dv(lambda e: e.tensor_tensor(out=ohg[:, 0:G, 0:4], in0=lgG, in1=mg[:, 0:G, 0:1].to_broadcast([128, G, 4]), op=ALU.is_equal), [lg, mg], [ohg])
                    dv(lambda e: e.tensor_tensor(out=dg[:, 0:G, 0:4], in0=lgG, in1=mg[:, 0:G, 0:1].to_broadcast([128, G, 4]), op=ALU.subtract), [lg, mg], [dg])
                    S.add("act", lambda e: e.activation(out=eg[:, 0:G, 0:4], in_=dg[:, 0:G, 0:4], func=AF.Exp), reads=[dg.b], writes=[eg.b])
                    dv(lambda e: e.tensor_reduce(out=sg[:, 0:G, 0], in_=eg[:, 0:G, 0:4], axis=AX.X, op=ALU.add), [eg], [sg])
                    dv(lambda e: e.reciprocal(out=pt[:, 0:G, 0], in_=sg[:, 0:G, 0]), [sg], [pt])
                    tmp, el = r["g"], r["h"]
                    dv(lambda e: e.tensor_tensor(out=tmp[:, 0:G, :].rearrange("p t (g j) -> p t g j", j=4), in0=lgE.rearrange("p t (g j) -> p t g j", j=4),
                                                 in1=ohg[:, 0:G, 0:4].unsqueeze(3).to_broadcast([128, G, 4, 4]), op=ALU.mult), [lg, ohg], [tmp])
                    dv(lambda e: e.tensor_reduce(out=el[:, 0:G, 0:4], in_=tmp[:, 0:G, :].rearrange("p t (g j) -> p t j g", j=4), axis=AX.X, op=ALU.add), [tmp], [el])
                    m1, oh1, el2, m2, oh2 = r["i"], r["j"], r["k"], r["c"], r["d"]
                    dv(lambda e: e.tensor_reduce(out=m1[:, 0:G, 0], in_=el[:, 0:G, 0:4], axis=AX.X, op=ALU.max), [el], [m1])
                    dv(lambda e: e.tensor_tensor(out=oh1[:, 0:G, 0:4], in0=el[:, 0:G, 0:4], in1=m1[:, 0:G, 0:1].to_broadcast([128, G, 4]), op=ALU.is_equal), [el, m1], [oh1])
                    dv(lambda e: e.scalar_tensor_tensor(out=el2[:, 0:G, 0:4], in0=oh1[:, 0:G, 0:4], scalar=-1e30, in1=el[:, 0:G, 0:4], op0=ALU.mult, op1=ALU.add), [oh1, el], [el2])
                    dv(lambda e: e.tensor_reduce(out=m2[:, 0:G, 0], in_=el2[:, 0:G, 0:4], axis=AX.X, op=ALU.max), [el2], [m2])
                    dv(lambda e: e.tensor_tensor(out=oh2[:, 0:G, 0:4], in0=el2[:, 0:G, 0:4], in1=m2[:, 0:G, 0:1].to_broadcast([128, G, 4]), op=ALU.is_equal), [el2, m2], [oh2])
                    dd, ee, w1_, w2_ = r["a"], r["e"], r["g"], r["h"]
                    dv(lambda e: e.tensor_tensor(out=dd[:, 0:G, 0], in0=m2[:, 0:G, 0], in1=m1[:, 0:G, 0], op=ALU.subtract), [m2, m1], [dd])
                    S.add("act", lambda e: e.activation(out=ee[:, 0:G, 0], in_=dd[:, 0:G, 0], func=AF.Exp), reads=[dd.b], writes=[ee.b])
                    dv(lambda e: e.tensor_scalar(out=dd[:, 0:G, 1], in0=ee[:, 0:G, 0], scalar1=1.0, scalar2=None, op0=ALU.add), [ee], [dd])
                    dv(lambda e: e.reciprocal(out=dd[:, 0:G, 2], in_=dd[:, 0:G, 1]), [dd], [dd])
                    dv(lambda e: e.tensor_tensor(out=w1_[:, 0:G, 0], in0=dd[:, 0:G, 2], in1=pt[:, 0:G, 0], op=ALU.mult), [dd, pt], [w1_])
                    dv(lambda e: e.tensor_tensor(out=w2_[:, 0:G, 0], in0=w1_[:, 0:G, 0], in1=ee[:, 0:G, 0], op=ALU.mult), [w1_, ee], [w2_])
                    wa_, wb_ = r["i"], r["k"]
                    dv(lambda e: e.tensor_tensor(out=wa_[:, 0:G, 0:4], in0=oh1[:, 0:G, 0:4], in1=w1_[:, 0:G, 0:1].to_broadcast([128, G, 4]), op=ALU.mult), [oh1, w1_], [wa_])
                    dv(lambda e: e.tensor_tensor(out=wb_[:, 0:G, 0:4], in0=oh2[:, 0:G, 0:4], in1=w2_[:, 0:G, 0:1].to_broadcast([128, G, 4]), op=ALU.mult), [oh2, w2_], [wb_])
                    dv(lambda e: e.tensor_tensor(out=wa_[:, 0:G, 0:4], in0=wa_[:, 0:G, 0:4], in1=wb_[:, 0:G, 0:4], op=ALU.add), [wa_, wb_], [wa_])
                    for k_, oh_ in enumerate((oh1, oh2)):
                        dv(lambda e, k_=k_, oh_=oh_: e.tensor_tensor(out=OH[k_][:, idx0:idx0 + G, :].rearrange("p t (g j) -> p t g j", j=4), in0=ohg[:, 0:G, 0:4].unsqueeze(3).to_broadcast([128, G, 4, 4]),
                                                                    in1=oh_[:, 0:G, 0:4].unsqueeze(2).to_broadcast([128, G, 4, 4]), op=ALU.mult), [ohg, oh_], [OH[k_]])
                    dv(lambda e: e.tensor_copy(out=WAB[0][:, idx0:idx0 + G], in_=w1_[:, 0:G, 0]), [w1_], [WAB[0]])
                    dv(lambda e: e.tensor_copy(out=WAB[1][:, idx0:idx0 + G], in_=w2_[:, 0:G, 0]), [w2_], [WAB[1]])
                    dv(lambda e: e.tensor_tensor(out=sel[:, 0:G, :], in0=OH[0][:, idx0:idx0 + G, :], in1=OH[1][:, idx0:idx0 + G, :], op=ALU.add), [OH[0], OH[1]], [sel])
                    for ti in range(G):
                        S.add("pe", lambda e, ti=ti: e.matmul(H3[0][:, ti * 32:ti * 32 + 16], lhsT=cU[:], rhs=sel[:, ti, :], start=True, stop=True), reads=[cU.b, sel.b], writes=[H3[0].b])
                        S.add("pe", lambda e, ti=ti: e.matmul(H3[0][:, ti * 32 + 16:ti * 32 + 32], lhsT=cOnes[:], rhs=sel[:, ti, :], start=True, stop=True), reads=[cOnes.b, sel.b], writes=[H3[0].b])
                        dv(lambda e, ti=ti: e.tensor_tensor(out=POS[:, idx0 + ti, :], in0=H3[0][:, ti * 32:ti * 32 + 16], in1=base[:], op=ALU.add), [H3[0], base], [POS])
                        dv(lambda e, ti=ti: e.tensor_tensor(out=base[:], in0=H3[0][:, ti * 32 + 16:ti * 32 + 32], in1=base[:], op=ALU.add), [H3[0], base], [base])

                for g0 in range(0, NTOK, 4):
                    route_group(g0, min(4, NTOK - g0))
                o_tmp, o_pad, o_off, o_end = offs[:, 0, :], offs[:, 1, :], offs[:, 2, :], offs[:, 3, :]
                dv(lambda e: e.tensor_tensor(out=cmpt[:, 0:19, :].rearrange("p j e -> p e j"), in0=itc[:, 0:19, :].rearrange("p j e -> p e j"),
                                             in1=base[:].unsqueeze(2).to_broadcast([128, 16, 19]), op=ALU.is_lt), [itc, base], [cmpt])
                dv(lambda e: e.tensor_reduce(out=o_tmp, in_=cmpt[:, 0:19, :].rearrange("p j e -> p e j"), axis=AX.X, op=ALU.add), [cmpt], [offs])
                dv(lambda e: e.tensor_scalar(out=o_pad, in0=o_tmp, scalar1=float(TS), scalar2=None, op0=ALU.mult), [offs], [offs])
                S.add("pool", lambda e: e.memset(offs[:, 2, 0:1], 0.0), reads=[offs.b], writes=[offs.b])
                for ex in range(1, 16):
                    dv(lambda e, ex=ex: e.tensor_tensor(out=offs[:, 2, ex:ex + 1], in0=offs[:, 2, ex - 1:ex], in1=offs[:, 1, ex - 1:ex], op=ALU.add), [offs], [offs])
                dv(lambda e: e.tensor_tensor(out=o_end, in0=o_off, in1=o_pad, op=ALU.add), [offs], [offs])
                dv(lambda e: e.tensor_tensor(out=cmpt[:, 0:NT, :], in0=itc[:, 0:NT, :], in1=offs[:, 3:4, :].to_broadcast([128, NT, 16]), op=ALU.is_ge), [itc, offs], [cmpt])
                dv(lambda e: e.tensor_reduce(out=eid[:, 0:NT], in_=cmpt[:, 0:NT, :], axis=AX.X, op=ALU.add), [cmpt], [eid])
                dv(lambda e: e.tensor_scalar(out=eid[:, 0:NT], in0=eid[:, 0:NT], scalar1=15.0, scalar2=128.0, op0=ALU.min, op1=ALU.mult), [eid], [eid])
                dv(lambda e: e.tensor_scalar(out=eid[:, 0:NT], in0=eid[:, 0:NT], scalar1=pidx[:, l:l + 1], scalar2=None, op0=ALU.add), [eid, pidx], [eid])
                dv(lambda e: e.tensor_copy(out=widx[:, 0:NT], in_=eid[:, 0:NT]), [eid], [widx])
                dv(lambda e: e.tensor_tensor(out=POS[:], in0=POS[:], in1=offs[:, 2:3, :].to_broadcast([128, NTOK, 16]), op=ALU.add), [POS, offs], [POS])
                for k_ in range(2):
                    dv(lambda e, k_=k_: e.tensor_tensor(out=ptmp[:], in0=POS[:], in1=OH[k_][:], op=ALU.mult), [POS, OH[k_]], [ptmp])
                    dv(lambda e, k_=k_: e.tensor_reduce(out=slf[k_][:], in_=ptmp[:], axis=AX.X, op=ALU.add), [ptmp], [slf[k_]])
                    dv(lambda e, k_=k_: e.tensor_copy(out=slu[k_][:], in_=slf[k_][:]), [slf[k_]], [slu[k_]])
                for ti in range(NTOK):
                    hrow = tiles_all[ti][5]
                    h_ = hs[ti % 2]
                    S.add("sp", lambda e, h_=h_, hrow=hrow: e.dma_start(out=h_[:], in_=hfd[hrow * 128:(hrow + 1) * 128, :]), reads=[dbuf("hfd")], writes=[h_.b], slot=h_.b)
                    for k_ in range(2):
                        S.add("pool", lambda e, h_=h_, k_=k_, ti=ti: e.indirect_dma_start(out=srt_d[0:NT * 512, :], out_offset=bass.IndirectOffsetOnAxis(ap=slu[k_][:, ti:ti + 1], axis=0), in_=h_[:], in_offset=None),
                              reads=[h_.b, slu[k_].b], writes=[dbuf("srt")], slot=h_.b)
                for i in range(NT):
                    a1, a3, a2 = ew1[i % 2], ew3[i % 2], ew2[i % 2]
                    for a_, k_ in ((a1, "w1"), (a3, "w3"), (a2, "w2")):
                        S.add("pool", lambda e, a_=a_, k_=k_, i=i: e.indirect_dma_start(out=a_[:], out_offset=None, in_=wb[k_][:, :], in_offset=bass.IndirectOffsetOnAxis(ap=widx[:, i:i + 1], axis=0)),
                              reads=[widx.b], writes=[a_.b], slot=a_.b)
                    sr = srt[i % 2]
                    for ii in ([0, 1] if i == 0 else [i + 1]):
                        if ii < NT:
                            sr2 = srt[ii % 2]
                            S.add("sp", lambda e, sr2=sr2, ii=ii: e.dma_start(out=sr2[:], in_=srt_d[ii * 512:(ii + 1) * 512, :].rearrange("(a p) n -> p a n", p=128)), reads=[dbuf("srt")], writes=[sr2.b], slot=sr2.b)
                    for a in range(4):
                        for kc in range(8):
                            S.add("pe", lambda e, sr=sr, a=a, kc=kc: e.transpose(out=TB[:, kc * 128:(kc + 1) * 128], in_=sr[:, a, kc * 128:(kc + 1) * 128], identity=ident[:]), reads=[sr.b, ident.b], writes=[TB.b])
                        S.add("act", lambda e, a=a: e.activation(out=hfT[:, :, a * 128:(a + 1) * 128], in_=TB[:].rearrange("p (c t) -> p c t", t=128), func=AF.Copy), reads=[TB.b], writes=[hfT.b])
                    hd = hidT[i % 2]
                    a1v = a1[:].rearrange("p (c n) -> p c n", c=8)
                    a3v = a3[:].rearrange("p (c n) -> p c n", c=8)
                    a2v = a2[:].rearrange("p (c n) -> p c n", c=4)
                    for hc in range(4):
                        h1, h3, s_ = H1[hc % 2], H3[hc % 2], sl[hc % 2]
                        for kc in range(8):
                            S.add("pe", lambda e, h1=h1, a1v=a1v, kc=kc, hc=hc: e.matmul(h1[:], lhsT=a1v[:, kc, hc * 128:(hc + 1) * 128], rhs=hfT[:, kc, :], start=(kc == 0), stop=(kc == 7)),
                                  reads=[a1.b, hfT.b], writes=[h1.b])
                        for kc in range(8):
                            S.add("pe", lambda e, h3=h3, a3v=a3v, kc=kc, hc=hc: e.matmul(h3[:], lhsT=a3v[:, kc, hc * 128:(hc + 1) * 128], rhs=hfT[:, kc, :], start=(kc == 0), stop=(kc == 7)),
                                  reads=[a3.b, hfT.b], writes=[h3.b])
                        S.add("act", lambda e, h1=h1, s_=s_: e.activation(out=s_[:], in_=h1[:], func=AF.Silu), reads=[h1.b], writes=[s_.b])
                        S.add("dve", lambda e, h3=h3, s_=s_, hd=hd, hc=hc: e.tensor_tensor(out=hd[:, hc, :], in0=s_[:], in1=h3[:], op=ALU.mult), reads=[s_.b, h3.b], writes=[hd.b])
                    for ti in range(4):
                        o_b = ob[ti % 2]
                        for half in range(2):
                            o_ = OUT[half]
                            for hc in range(4):
                                S.add("pe", lambda e, o_=o_, hd=hd, a2v=a2v, hc=hc, ti=ti, half=half: e.matmul(o_[:], lhsT=hd[:, hc, ti * 128:(ti + 1) * 128], rhs=a2v[:, hc, half * 512:(half + 1) * 512],
                                                                                                        start=(hc == 0), stop=(hc == 3)), reads=[hd.b, a2.b], writes=[o_.b])
                            if half == 0:
                                S.add("act", lambda e, o_=o_, o_b=o_b: e.activation(out=o_b[:, 0:512], in_=o_[:], func=AF.Copy), reads=[o_.b], writes=[o_b.b])
                            else:
                                S.add("dve", lambda e, o_=o_, o_b=o_b: e.tensor_copy(out=o_b[:, 512:1024], in_=o_[:]), reads=[o_.b], writes=[o_b.b])
                        S.add("sp", lambda e, o_b=o_b, i=i, ti=ti: e.dma_start(out=sout[i * 512 + ti * 128:i * 512 + (ti + 1) * 128, :], in_=o_b[:]), reads=[o_b.b], writes=[(dbuf("sout") if ti == 3 else dbuf("sout2")) if i == NT - 1 and ti >= 2 else S.buf("so")], slot=o_b.b)
                for ti in range(NTOK):
                    sap, sbuf_, dap, dbuf_, stream, hrow = tiles_all[ti]
                    S.add("pool", lambda e, ti=ti: e.indirect_dma_start(out=ra[:], out_offset=None, in_=sout[0:NT * 512, :], in_offset=bass.IndirectOffsetOnAxis(ap=slu[0][:, ti:ti + 1], axis=0)),
                          reads=[dbuf("sout"), dbuf("sout2"), slu[0].b], writes=[ra.b], slot=ra.b)
                    S.add("pool", lambda e, ti=ti: e.indirect_dma_start(out=rb_[:], out_offset=None, in_=sout[0:NT * 512, :], in_offset=bass.IndirectOffsetOnAxis(ap=slu[1][:, ti:ti + 1], axis=0)),
                          reads=[dbuf("sout"), dbuf("sout2"), slu[1].b], writes=[rb_.b], slot=rb_.b)
                    S.add("sp", lambda e, sap=sap: e.dma_start(out=xr[:], in_=sap), reads=[sbuf_], writes=[xr.b], slot=xr.b)
                    xo_ = xo[0]
                    dv(lambda e, ti=ti: e.tensor_scalar(out=ra[:], in0=ra[:], scalar1=WAB[0][:, ti:ti + 1], scalar2=None, op0=ALU.mult), [ra, WAB[0]], [ra])
                    dv(lambda e, ti=ti: e.scalar_tensor_tensor(out=ra[:], in0=rb_[:], scalar=WAB[1][:, ti:ti + 1], in1=ra[:], op0=ALU.mult, op1=ALU.add), [rb_, WAB[1], ra], [ra])
                    dv(lambda e, stream=stream: e.tensor_tensor(out=ra[:], in0=ra[:], in1=gates[stream][1][:], op=ALU.mult), [ra, gates[stream][1]], [ra])
                    dv(lambda e, xo_=xo_: e.tensor_tensor(out=xo_[:], in0=ra[:], in1=xr[:], op=ALU.add), [ra, xr], [xo_])
                    S.add("sp", lambda e, dap=dap, xo_=xo_: e.dma_start(out=dap, in_=xo_[:]), reads=[xo_.b], writes=[dbuf_], slot=xo_.b)
                S.barrier()
        for l_ in range(n_layers):
            layer(l_)
        nsem = S.emit(es)
    return nc, nsem, len(S.ops)


def _bias_sets(rpb_l, j):
    sets = [(64, list(range(-2, 4))), (32 * j, list(range(-2, 4))), (32 * j + 1, list(range(-2, 4))),
            (32 * j + 30, list(range(-3, 3))), (32 * j + 31, list(range(-3, 3)))]
    outp = np.full((5, 6, 128, 8, 128), NEG, np.float32)
    idx = np.arange(128)
    qc = idx % 64
    kc = idx % 64
    cs = np.clip(qc - 8, 0, 48)
    for si, (m, offs) in enumerate(sets):
        qr = 2 * m + idx // 64
        rs = np.clip(qr - 4, 0, 248)
        for oi, o in enumerate(offs):
            kr = 2 * (m + o) + idx // 64
            valid = ((kr[:, None] >= 0) & (kr[:, None] < 256) & (kr[:, None] >= rs[None, :]) & (kr[:, None] < rs[None, :] + 8)
                     & (kc[:, None] >= cs[None, :]) & (kc[:, None] < cs[None, :] + 16))
            dr = np.clip(kr[:, None] - qr[None, :] + 7, 0, 14)
            dc = np.clip(kc[:, None] - qc[None, :] + 15, 0, 30)
            vals = rpb_l[:, dr, dc]
            vals = np.where(valid[None], vals, NEG)
            outp[si, oi] = np.transpose(vals, (1, 0, 2))
    return outp.reshape(5, 6, 128, 1024)


_CACHE = {}


def kernel(x, c, ctx, c_ctx, w_ada, b_ada, w_in, q_gain, k_gain, rpb, sgu_ln, sgu_w, sgu_b, out_gain, w_out,
           rg_w, rg_b, re_w, re_b, w1, w3, w2):
    f = lambda a: np.ascontiguousarray(np.asarray(a, dtype=np.float32))
    x, c, ctx, c_ctx, w_ada, b_ada, w_in, q_gain, k_gain, rpb = map(f, (x, c, ctx, c_ctx, w_ada, b_ada, w_in, q_gain, k_gain, rpb))
    sgu_ln, sgu_w, sgu_b, out_gain, w_out, rg_w, rg_b, re_w, re_b, w1, w3, w2 = map(
        f, (sgu_ln, sgu_w, sgu_b, out_gain, w_out, rg_w, rg_b, re_w, re_b, w1, w3, w2))
    if "nc" not in _CACHE:
        _CACHE["nc"] = build()[0]
    nc = _CACHE["nc"]
    shared = {
        "ident": np.eye(128, dtype=np.float32).astype(ml_dtypes.bfloat16),
        "identf": np.eye(128, dtype=np.float32),
        "w_ada": w_ada, "b_ada": b_ada,
        "b_adaT": np.ascontiguousarray(b_ada.reshape(2, 48, 128).transpose(0, 2, 1)),
        "w_in": w_in.reshape(128, -1), "w_out": w_out.reshape(128, -1),
        "w1": w1.reshape(32, 1024, 512), "w3": w3.reshape(32, 1024, 512), "w2": w2.reshape(32, 512, 1024),
        "cU": np.triu(np.ones((128, 128), np.float32), 1).astype(ml_dtypes.bfloat16),
        "cOnes": np.ones((128, 128), np.float32).astype(ml_dtypes.bfloat16),
        "itc": np.ascontiguousarray(np.broadcast_to((np.arange(35, dtype=np.float32) * 512.0)[None, :, None], (128, 35, 16)).reshape(128, 35 * 16)),
        "pidx": np.ascontiguousarray(np.stack([np.arange(128, dtype=np.float32), np.arange(128, dtype=np.float32) + 2048.0], axis=1)),
        "rw": np.ascontiguousarray(np.concatenate([rg_w, re_w.transpose(0, 2, 1, 3).reshape(2, 1024, 16)], axis=2)),
        "rb": np.ascontiguousarray(np.concatenate([rg_b, re_b.reshape(2, 16)], axis=1)),
        "qg": np.ascontiguousarray(np.tile(q_gain, (1, 2))[:, :, None]),
        "kg": np.ascontiguousarray(np.tile(k_gain, (1, 2))[:, :, None]),
        "sln": sgu_ln,
        "swT": np.ascontiguousarray(sgu_w.transpose(0, 3, 1, 2).reshape(2, 128, 1024)),
        "sbT": np.ascontiguousarray(sgu_b.transpose(0, 2, 1)),
        "ogT": np.ascontiguousarray(out_gain.reshape(2, 8, 128).transpose(0, 2, 1)),
    }
    xpad = np.zeros((2, 272, 64, D), np.float32)
    xpad[:, 8:264] = x.reshape(2, 256, 64, D)
    in_maps = []
    for core in range(8):
        b, j = core // 4, core % 4
        m = dict(shared)
        m["xw"] = np.ascontiguousarray(xpad[b, 64 * j:64 * j + 80].reshape(5120, D))
        m["ctx"] = ctx[b]
        cv = np.stack([c[b].reshape(8, 128).T, c_ctx.reshape(8, 128).T], axis=2)
        m["cvec"] = np.ascontiguousarray(cv.reshape(128, 16))
        m["bias"] = np.stack([_bias_sets(rpb[0], j), _bias_sets(rpb[1], j)], axis=0)
        in_maps.append(m)
    res = run_bass_kernel_spmd(nc, in_maps, core_ids=list(range(8)))
    outp = np.empty((2, 16384, D), np.float32)
    for core in range(8):
        b, j = core // 4, core % 4
        outp[b, 4096 * j:4096 * (j + 1)] = res.results[core]["out"]
    return outp
```

```python
import os as _os
import numpy as np
import ml_dtypes
from contextlib import ExitStack
import concourse.bass as bass
import concourse.mybir as mybir
from concourse.bass_utils import run_bass_kernel_spmd

F32, BF16 = mybir.dt.float32, mybir.dt.bfloat16
AF = mybir.ActivationFunctionType
ALU = mybir.AluOpType
AX = mybir.AxisListType

D = 1024
NEG = -30000.0
EPS = 1e-6
ENGS = ("sp", "pe", "act", "dve", "pool")


class Buf:
    __slots__ = ("name", "w", "r")

    def __init__(self, name):
        self.name, self.w, self.r = name, None, []


class Op:
    __slots__ = ("eng", "fn", "dma", "key", "deps", "signal", "val")


class Sched:
    def __init__(self, nc):
        self.nc = nc
        self.ops = []
        self.bufs = []
        self.dma_out = []
        self.last = {}

    def buf(self, name):
        b = Buf(name)
        self.bufs.append(b)
        return b

    def _dep(self, op, d, raw):
        if d is op:
            return
        if (not d.dma) and (not op.dma) and d.eng == op.eng and not raw:
            return
        op.deps[d] = True
        d.signal = True

    def add(self, eng, fn, reads=(), writes=(), slot=None):
        op = Op()
        op.eng, op.fn, op.dma = eng, fn, slot is not None
        op.key = ("slot", slot.name, eng) if slot is not None else eng
        op.deps, op.signal, op.val = {}, op.dma, 0
        for b in reads:
            if b.w is not None:
                self._dep(op, b.w, True)
        for b in writes:
            if b.w is not None:
                self._dep(op, b.w, False)
            for r in b.r:
                self._dep(op, r, False)
        for b in reads:
            b.r.append(op)
        for b in writes:
            b.w = op
            b.r = []
        self.ops.append(op)
        if op.dma:
            self.dma_out.append(op)
        else:
            self.last[eng] = op
        return op

    def barrier(self):
        a = self.add("sp", lambda e: e.nop())
        for d in self.dma_out:
            a.deps[d] = True
        for eng, o in self.last.items():
            if eng != "sp":
                a.deps[o] = True
                o.signal = True
        a.signal = True
        self.dma_out = []
        for eng in ENGS:
            if eng != "sp":
                o = self.add(eng, lambda e: e.nop())
                o.deps[a] = True
        for b in self.bufs:
            b.w, b.r = None, []

    def emit(self, es):
        nc = self.nc
        cnt = {}
        for op in self.ops:
            if op.signal:
                cnt[op.key] = cnt.get(op.key, 0) + (16 if op.dma else 1)
                op.val = cnt[op.key]
        sems = {}
        for i, k in enumerate(cnt):
            sems[k] = es.enter_context(nc.semaphore("s%d" % i))
        per = {e: [o for o in self.ops if o.eng == e] for e in ENGS}
        block = es.enter_context(nc.Block())

        def body(eng):
            def f(e):
                seen = {}
                for op in per[eng]:
                    need = {}
                    for d in op.deps:
                        if need.get(d.key, 0) < d.val:
                            need[d.key] = d.val
                    for k, v in need.items():
                        if seen.get(k, 0) < v:
                            e.wait_ge(sems[k], v)
                            seen[k] = v
                    ins = op.fn(e)
                    if op.signal:
                        ins.then_inc(sems[op.key], 16 if op.dma else 1)
            return f

        block.sync(body("sp"))
        block.tensor(body("pe"))
        block.scalar(body("act"))
        block.vector(body("dve"))
        block.gpsimd(body("pool"))
        return len(sems)


class T:
    def __init__(self, S, es, nc, name, shape, dt, psum=False):
        self.t = es.enter_context((nc.psum_tensor if psum else nc.sbuf_tensor)("t_" + name, list(shape), dt))
        self.b = S.buf(name.split("_L")[0] if name[-3:-1] == "_L" else name)

    def __getitem__(self, k):
        return self.t[k]


def build(n_layers=2, do_f=True, stages=("W", "M", "PA", "F"), dbg=False, tile_limit=None):
    nc = bass.Bass("TRN2", target_bir_lowering=False)
    S = Sched(nc)

    def din(name, shape, dt=F32):
        return nc.dram_tensor(name, list(shape), dt, kind="ExternalInput").ap()

    def dscr(name, shape, dt=F32):
        return nc.dram_tensor(name, list(shape), dt, kind="ExternalOutput" if dbg else "Internal").ap()

    xw = din("xw", [40 * 128, D])
    ctx0 = din("ctx", [256, D])
    cvec = din("cvec", [128, 16])
    ident_d = din("ident", [128, 128], BF16)
    identf_d = din("identf", [128, 128])
    w_ada = din("w_ada", [2, D, 6 * D])
    b_ada = din("b_ada", [2, 6 * D])
    b_adaT = din("b_adaT", [2, 128, 48])
    FW = {"w_in": 2 * D * 2560 // 128, "w_out": 2 * D * D // 128, "w1": 2 * 16 * D * 512 // 128,
          "w3": 2 * 16 * D * 512 // 128, "w2": 2 * 16 * 512 * D // 128}
    wf = {k: din(k, [128, v]) for k, v in FW.items() if k in ("w_in", "w_out")}
    wb = {k: dscr(k + "_b", [128, v], BF16) for k, v in FW.items() if k in ("w_in", "w_out")}
    wf["w1"] = din("w1", [32, D, 512]); wf["w3"] = din("w3", [32, D, 512]); wf["w2"] = din("w2", [32, 512, D])
    for k_ in ("w1", "w3", "w2"):
        wb[k_] = dscr(k_ + "_b", [32 * 128, 4096], BF16)
    rw = din("rw", [2, D, 20])
    rb = din("rb", [2, 20])
    qg = din("qg", [2, 128, 1])
    kg = din("kg", [2, 128, 1])
    bias = din("bias", [2, 5, 6, 128, 1024])
    sln = din("sln", [2, 512])
    swT = din("swT", [2, 128, 1024])
    sbT = din("sbT", [2, 128, 8])
    ogT = din("ogT", [2, 128, 8])
    out = nc.dram_tensor("out", [32 * 128, D], F32, kind="ExternalOutput").ap()
    x1 = dscr("x1", [40 * 128, D])
    xm = dscr("xm", [40 * 128, D])
    ctx1 = dscr("ctx1", [256, D])
    ctxm = dscr("ctxm", [256, D])
    if dbg:
        dbg_modf = nc.dram_tensor("dbg_modf", [2, 128, 64], F32, kind="ExternalOutput").ap()
        dbg_gates = nc.dram_tensor("dbg_gates", [2, 4, 128, D], F32, kind="ExternalOutput").ap()
    NTMAX = 35
    hfd = dscr("hfd", [40 * 128, D], BF16)
    srt_d = dscr("srt_d", [NTMAX * 512, D], BF16)
    sout = dscr("sout", [NTMAX * 512, D])
    cU_d = din("cU", [128, 128], BF16)
    cOnes_d = din("cOnes", [128, 128], BF16)
    itc_d = din("itc", [128, NTMAX * 16])
    pidx_d = din("pidx", [128, 2])
    fbc_d = dscr("fbc_d", [4, 128, D])
    dbufs = {}

    def dbuf(name):
        if name not in dbufs:
            dbufs[name] = S.buf("dram_" + name)
        return dbufs[name]

    def wview(k, l):
        flat = wb[k].rearrange("p f -> (p f)")
        if k == "w_in":
            return flat.rearrange("(l kc p n) -> l p kc n", l=2, kc=8, p=128, n=2560)[l]
        if k == "w_out":
            return flat.rearrange("(l kc p n) -> l p kc n", l=2, kc=8, p=128, n=1024)[l]
        if k in ("w1", "w3"):
            return flat.rearrange("(l e kc p n) -> l e p kc n", l=2, e=16, kc=8, p=128, n=512)[l]
        return flat.rearrange("(l e kc p n) -> l e p kc n", l=2, e=16, kc=4, p=128, n=1024)[l]

    with ExitStack() as es:
        mk = lambda name, shape, dt, psum=False: T(S, es, nc, name, shape, dt, psum)
        ident = mk("ident", [128, 128], BF16)
        identf = mk("identf", [128, 128], F32)
        modf = mk("modf", [128, 4, 8, 2], F32)
        zc = mk("zc", [128, 4], F32)
        S.add("pool", lambda e: e.memset(zc[:], 0.0), writes=[zc.b])
        epsc = mk("epsc", [128, 2], F32)
        S.add("pool", lambda e: e.memset(epsc[:], EPS), writes=[epsc.b])
        gates = [[mk("gate%d%d" % (s, k), [128, D], F32) for k in range(2)] for s in range(2)]
        S.add("sp", lambda e: e.dma_start(out=ident[:], in_=ident_d[:, :]), writes=[ident.b], slot=ident.b)
        S.add("sp", lambda e: e.dma_start(out=identf[:], in_=identf_d[:, :]), writes=[identf.b], slot=identf.b)

        def rstd_ops(src, dst, tmp, n, pfx=""):
            S.add("act", lambda e: e.activation(out=tmp[0], in_=src[0], func=AF.Ln, bias=epsc[:, 0:1], scale=1.0 / n), reads=[src[1], epsc.b], writes=[tmp[1]])
            S.add("act", lambda e: e.activation(out=dst[0], in_=tmp[0], func=AF.Exp, scale=-0.5), reads=[tmp[1]], writes=[dst[1]])

        with ExitStack() as es2:
          if "W" in stages:
            mk2 = lambda name, shape, dt, psum=False: T(S, es2, nc, name, shape, dt, psum)
            CH = 4096
            st32 = [mk2("st32_%d" % i, [128, CH], F32) for i in range(2)]
            st16 = [mk2("st16_%d" % i, [128, CH], BF16) for i in range(2)]
            i = 0
            jobs = []
            for k in ("w_in", "w_out"):
                for off in range(0, FW[k], CH):
                    n = min(CH, FW[k] - off)
                    jobs.append((n, wf[k][:, off:off + n], wb[k][:, off:off + n], None))
            for n, src, dst, c3 in jobs:
                a, b = st32[i % 2], st16[i % 2]
                if c3 is None:
                    S.add("sp", lambda e, a=a, src=src, n=n: e.dma_start(out=a[:, 0:n], in_=src), writes=[a.b], slot=a.b)
                else:
                    S.add("sp", lambda e, a=a, src=src, c3=c3: e.dma_start(out=a[:].rearrange("p (c n) -> p c n", c=c3), in_=src), writes=[a.b], slot=a.b)
                if i % 2 == 0:
                    S.add("dve", lambda e, a=a, b=b, n=n: e.tensor_copy(out=b[:, 0:n], in_=a[:, 0:n]), reads=[a.b], writes=[b.b])
                else:
                    S.add("act", lambda e, a=a, b=b, n=n: e.activation(out=b[:, 0:n], in_=a[:, 0:n], func=AF.Copy), reads=[a.b], writes=[b.b])
                S.add("pool", lambda e, b=b, dst=dst, n=n: e.dma_start(out=dst, in_=b[:, 0:n]), reads=[b.b], writes=[S.buf('wtmp')], slot=b.b)
                i += 1
            S.barrier()

        def layer(l):
            with ExitStack() as es2:
              if "M" in stages:
                mk2 = lambda name, shape, dt, psum=False: T(S, es2, nc, name + "_L%d" % l, shape, dt, psum)
                cv = mk2("cv", [128, 16], F32)
                sil = mk2("sil", [128, 16], F32)
                silrep = mk2("silrep", [128, 16, 128], F32)
                wa = [mk2("wa%d" % i, [128, 8, 512], F32) for i in range(2)]
                badT = mk2("badT", [128, 48], F32)
                bbc = [mk2("bbc%d" % i, [128, 512], F32) for i in range(2)]
                pg = [mk2("pg%d" % i, [128, 512], F32, True) for i in range(2)]
                pf = [mk2("pf%d" % i, [128, 8], F32, True) for i in range(2)]
                fbc = [[mk2("fbc%d%d" % (s_, k_), [128, D], F32) for k_ in range(2)] for s_ in range(2)]
                S.add("sp", lambda e: e.dma_start(out=cv[:], in_=cvec[:, :]), writes=[cv.b], slot=cv.b)
                S.add("sp", lambda e: e.dma_start(out=badT[:], in_=b_adaT[l]), writes=[badT.b], slot=badT.b)
                S.add("act", lambda e: e.activation(out=sil[:], in_=cv[:], func=AF.Silu), reads=[cv.b], writes=[sil.b])
                S.add("dve", lambda e: e.tensor_copy(out=silrep[:], in_=sil[:].unsqueeze(2).to_broadcast([128, 16, 128])),
                      reads=[sil.b], writes=[silrep.b])
                wav = w_ada[l].rearrange("(kc p) n -> p kc n", p=128)
                for j in range(12):
                    mod, half = j // 2, j % 2
                    w_ = wa[j % 2]
                    S.add("sp", lambda e, w_=w_, j=j: e.dma_start(out=w_[:], in_=wav[:, :, j * 512:(j + 1) * 512]),
                          writes=[w_.b], slot=w_.b)
                    if mod in (2, 3, 4, 5):
                        bb = bbc[(j // 2) % 2]
                        S.add("sp", lambda e, bb=bb, j=j: e.dma_start(out=bb[:], in_=b_ada[l, j * 512:(j + 1) * 512].partition_broadcast(128)),
                              writes=[bb.b], slot=bb.b)
                        for s in range(2):
                            p_ = pg[s]
                            for kc in range(8):
                                S.add("pe", lambda e, p_=p_, w_=w_, kc=kc, s=s: e.matmul(p_[:], lhsT=silrep[:, kc * 2 + s, :], rhs=w_[:, kc, :],
                                                                                      start=(kc == 0), stop=(kc == 7)),
                                      reads=[silrep.b, w_.b], writes=[p_.b])
                            g_ = {2: gates[s][0], 5: gates[s][1], 3: fbc[s][0], 4: fbc[s][1]}[mod]
                            S.add("dve", lambda e, p_=p_, g_=g_, bb=bb, half=half, mod=mod: e.scalar_tensor_tensor(out=g_[:, half * 512:(half + 1) * 512], in0=p_[:], scalar=(1.0 if mod == 4 else 0.0), in1=bb[:], op0=ALU.add, op1=ALU.add),
                                  reads=[p_.b, bb.b], writes=[g_.b])
                    if mod in (0, 1, 3, 4):
                        kind = {0: 0, 1: 1, 3: 2, 4: 3}[mod]
                        p_ = pf[j % 2]
                        for blk in range(4):
                            for kc in range(8):
                                S.add("pe", lambda e, p_=p_, w_=w_, kc=kc, blk=blk: e.matmul(p_[:, blk * 2:blk * 2 + 2], lhsT=w_[:, kc, blk * 128:(blk + 1) * 128],
                                                                                          rhs=sil[:, kc * 2:kc * 2 + 2], start=(kc == 0), stop=(kc == 7)),
                                      reads=[sil.b, w_.b], writes=[p_.b])
                        for blk in range(4):
                            ch = half * 4 + blk
                            S.add("dve", lambda e, p_=p_, blk=blk, ch=ch, kind=kind, mod=mod: e.tensor_scalar(
                                out=modf[:, kind, ch, :], in0=p_[:, blk * 2:blk * 2 + 2], scalar1=badT[:, mod * 8 + ch:mod * 8 + ch + 1],
                                scalar2=(1.0 if kind in (1, 3) else 0.0), op0=ALU.add, op1=ALU.add),
                                reads=[p_.b, badT.b], writes=[modf.b])
                for s_ in range(2):
                    for k_ in range(2):
                        f_ = fbc[s_][k_]
                        S.add("sp", lambda e, f_=f_, s_=s_, k_=k_: e.dma_start(out=fbc_d[s_ * 2 + k_], in_=f_[:]), reads=[f_.b], writes=[S.buf("fbcd")], slot=f_.b)
                S.barrier()

            kv_lo, kv_hi = (0, 40) if l == 0 else (2, 38)
            q_lo, q_hi = (2, 38) if l == 0 else (4, 36)
            if tile_limit is not None:
                kv_hi = min(kv_hi, tile_limit)
                q_hi = min(q_hi, tile_limit - 3)
            xin = xw if l == 0 else x1
            cin = ctx0 if l == 0 else ctx1
            xin_b = dbuf("xin%d" % l) if l == 0 else dbuf("x1")
            cin_b = dbuf("cin%d" % l) if l == 0 else dbuf("ctx1")

            if dbg and "M" in stages:
                S.add("sp", lambda e: e.dma_start(out=dbg_modf[l], in_=modf[:].rearrange("p a b c -> p (a b c)")), reads=[modf.b], writes=[S.buf("dbgm")], slot=modf.b)
                for s_i in range(2):
                    for k_i in range(2):
                        g_ = gates[s_i][k_i]
                        S.add("sp", lambda e, g_=g_, s_i=s_i, k_i=k_i: e.dma_start(out=dbg_gates[l, s_i * 2 + k_i], in_=g_[:]), reads=[g_.b], writes=[S.buf("dbgg")], slot=g_.b)
                S.barrier()
            with ExitStack() as es2:
              if "PA" in stages:
                mk2 = lambda name, shape, dt, psum=False: T(S, es2, nc, name + "_L%d" % l, shape, dt, psum)
                Win = mk2("Win", [128, 8, 2560], BF16)
                Wout = mk2("Wout", [128, 8, 1024], BF16)
                Eint = mk2("Eint", [128, 6, 1024], BF16)
                Esp = mk2("Esp", [128, 6, 1024], BF16)
                bst = [mk2("bst%d" % i, [128, 1024], F32) for i in range(1)]
                g2 = mk2("g2", [128, 4], F32)
                slnb = mk2("slnb", [128, 512], F32)
                swTb = mk2("swTb", [128, 1024], BF16)
                sb = mk2("sb", [128, 8], F32)
                og = mk2("og", [128, 8], F32)
                xt = [mk2("xt%d" % i, [128, D], F32) for i in range(2)]
                junk = mk2("junk", [128, D], BF16)
                xn = [mk2("xn%d" % i, [128, D], BF16) for i in range(2)]
                hT = [mk2("hT%d" % i, [128, 8, 128], BF16) for i in range(2)]
                qk = mk2("qk", [128, D], F32)
                guv = mk2("guv", [128, D], F32)
                sqb = mk2("sqb", [128, D], F32)
                qkn = mk2("qkn", [128, D], BF16)
                st = [mk2("stat%d" % i, [128, 64], F32) for i in range(2)]
                KT = [mk2("KT%d" % i, [128, 4, 128], BF16) for i in range(8)]
                QT = [mk2("QT%d" % i, [128, 4, 128], BF16) for i in range(4)]
                V = [mk2("V%d" % i, [128, 520], BF16) for i in range(8)]
                cKT = [mk2("cKT%d" % i, [128, 4, 128], BF16) for i in range(2)]
                cQT = [mk2("cQT%d" % i, [128, 4, 128], BF16) for i in range(2)]
                cV = [mk2("cV%d" % i, [128, 520], BF16) for i in range(2)]
                vsn = mk2("vsn", [128, 512], BF16)
                t1 = mk2("t1", [128, 512], F32)
                osg = [mk2("osg%d" % i, [128, 512], F32) for i in range(4)]
                cosg = [mk2("cosg%d" % i, [128, 512], F32) for i in range(2)]
                rsg = [mk2("rsg%d" % i, [128, 4], F32) for i in range(4)]
                crsg = [mk2("crsg%d" % i, [128, 4], F32) for i in range(2)]
                pT = [mk2("pT%d" % i, [128, 1024], BF16) for i in range(2)]
                ona = mk2("ona", [128, 512], F32)
                ast = mk2("ast", [128, 16], F32)
                yn = mk2("yn", [128, D], BF16)
                yT = mk2("yT", [128, 8, 128], BF16)
                xa = mk2("xa", [128, D], F32)
                xo = [mk2("xo%d" % i, [128, D], F32) for i in range(1)]
                wjobs = []
                if l == 0:
                    wst32 = [mk2("wst32_%d" % i, [128, 1024], F32) for i in range(2)]
                    wst16 = [mk2("wst16_%d" % i, [128, 1024], BF16) for i in range(1)]
                    for le in range(32):
                        for k_ in ("w1", "w3", "w2"):
                            for q_ in range(4):
                                v_ = wf[k_][le].rearrange("(c p) n -> p c n", p=128)
                                src = v_[:, 2 * q_:2 * q_ + 2, :] if k_ != "w2" else v_[:, q_:q_ + 1, :]
                                wjobs.append((src, wb[k_][le * 128:(le + 1) * 128, q_ * 1024:(q_ + 1) * 1024]))
                wcnt = {"i": 0}

                def _wload(j):
                    src, dst = wjobs[j]
                    a = wst32[j % 2]
                    c3 = src.shape[1]
                    S.add("pool", lambda e, a=a, src=src, c3=c3: e.dma_start(out=a[:].rearrange("p (c n) -> p c n", c=c3), in_=src), writes=[a.b], slot=a.b)

                def emit_wjobs(n):
                    for _ in range(n):
                        j = wcnt["i"]
                        if j >= len(wjobs):
                            return
                        if j == 0:
                            _wload(0)
                        if j + 1 < len(wjobs):
                            _wload(j + 1)
                        src, dst = wjobs[j]
                        a, b = wst32[j % 2], wst16[0]
                        wcnt["i"] += 1
                        S.add("pool", lambda e, a=a, b=b: e.tensor_copy(out=b[:], in_=a[:]), reads=[a.b], writes=[b.b])
                        S.add("pool", lambda e, b=b, dst=dst: e.dma_start(out=dst, in_=b[:]), reads=[b.b], writes=[S.buf("wtmp")], slot=b.b)

                P = [mk2("P%d" % i, [128, 512], F32, True) for i in range(1)]
                T0 = mk2("T0", [128, 1024], BF16, True)
                T1 = mk2("T1", [128, 1024], BF16, True)
                Sb = [mk2("S%d" % i, [128, 512], F32, True) for i in range(4)]
                O0 = mk2("O0", [128, 512], F32, True)

                S.add("sp", lambda e: e.dma_start(out=Win[:], in_=wview("w_in", l)), reads=[dbuf("w_in")], writes=[Win.b], slot=Win.b)
                S.add("sp", lambda e: e.dma_start(out=Wout[:], in_=wview("w_out", l)), reads=[dbuf("w_out")], writes=[Wout.b], slot=Wout.b)

                def load_E(dst, sidx):
                    for o in range(6):
                        b_ = bst[0]
                        S.add("sp", lambda e, b_=b_, o=o: e.dma_start(out=b_[:], in_=bias[l, sidx, o]), writes=[b_.b], slot=b_.b)
                        S.add("act", lambda e, b_=b_, o=o: e.activation(out=dst[:, o, :], in_=b_[:], func=AF.Exp), reads=[b_.b], writes=[dst.b])

                load_E(Eint, 0)
                S.add("sp", lambda e: e.dma_start(out=g2[:, 0:1], in_=qg[l]), writes=[g2.b], slot=g2.b)
                S.add("sp", lambda e: e.dma_start(out=g2[:, 1:2], in_=kg[l]), writes=[g2.b], slot=g2.b)
                S.add("dve", lambda e: e.scalar_tensor_tensor(out=g2[:, 2:3], in0=g2[:, 0:1], scalar=0.125, in1=g2[:, 1:2], op0=ALU.mult, op1=ALU.mult),
                      reads=[g2.b], writes=[g2.b])
                S.add("sp", lambda e: e.dma_start(out=slnb[:], in_=sln[l].partition_broadcast(128)), writes=[slnb.b], slot=slnb.b)
                S.add("sp", lambda e: e.dma_start(out=bst[0][:], in_=swT[l]), writes=[bst[0].b], slot=bst[0].b)
                S.add("dve", lambda e: e.tensor_copy(out=swTb[:], in_=bst[0][:]), reads=[bst[0].b], writes=[swTb.b])
                S.add("sp", lambda e: e.dma_start(out=sb[:], in_=sbT[l]), writes=[sb.b], slot=sb.b)
                S.add("sp", lambda e: e.dma_start(out=og[:], in_=ogT[l]), writes=[og.b], slot=og.b)
                for v_ in V + cV:
                    S.add("pool", lambda e, v_=v_: e.memset(v_[:].rearrange("p (h c) -> p h c", c=65)[:, :, 64:65], 1.0), writes=[v_.b])

                cnt = {"p": 0}

                def proj(src_ap, src_b, stream, kt, vt, qt, osg_t, rsg_t, need_q):
                    i = cnt["p"] % 2
                    cnt["p"] += 1
                    x_, xn_, h_, s_ = xt[i], xn[i], hT[i], st[i]
                    S.add("sp", lambda e: e.dma_start(out=x_[:], in_=src_ap), reads=[src_b], writes=[x_.b], slot=x_.b)
                    S.add("act", lambda e: e.activation(out=junk[:], in_=x_[:], func=AF.Square, accum_out=s_[:, 0:1]), reads=[x_.b], writes=[junk.b, s_.b])
                    rstd_ops((s_[:, 0:1], s_.b), (s_[:, 2:3], s_.b), (s_[:, 1:2], s_.b), D)
                    S.add("dve", lambda e: e.tensor_scalar(out=xn_[:], in0=x_[:], scalar1=s_[:, 2:3], scalar2=None, op0=ALU.mult), reads=[x_.b, s_.b], writes=[xn_.b])
                    yield
                    for kc in range(8):
                        S.add("pe", lambda e, kc=kc: e.transpose(out=T0[:, kc * 128:(kc + 1) * 128], in_=xn_[:, kc * 128:(kc + 1) * 128], identity=ident[:]),
                              reads=[xn_.b, ident.b], writes=[T0.b])
                    yield
                    for kc in range(8):
                        if kc % 2 == 0:
                            S.add("act", lambda e, kc=kc: e.activation(out=h_[:, kc, :], in_=T0[:, kc * 128:(kc + 1) * 128], func=AF.Identity,
                                                                      bias=modf[:, 0, kc, stream:stream + 1], scale=modf[:, 1, kc, stream:stream + 1]),
                                  reads=[T0.b, modf.b], writes=[h_.b])
                        else:
                            S.add("dve", lambda e, kc=kc: e.tensor_scalar(out=h_[:, kc, :], in0=T0[:, kc * 128:(kc + 1) * 128], scalar1=modf[:, 1, kc, stream:stream + 1],
                                                                         scalar2=modf[:, 0, kc, stream:stream + 1], op0=ALU.mult, op1=ALU.add),
                                  reads=[T0.b, modf.b], writes=[h_.b])
                    yield
                    plevel = float(_os.environ.get("DBG_P", "9"))
                    if plevel < 2:
                        return
                    chunks = [0, 1, 2, 3, 4] if need_q else [1, 2]
                    for nch in chunks:
                        yield
                        p_ = P[0]
                        for kc in range(8):
                            S.add("pe", lambda e, p_=p_, kc=kc, nch=nch: e.matmul(p_[:], lhsT=h_[:, kc, :], rhs=Win[:, kc, nch * 512:(nch + 1) * 512], start=(kc == 0), stop=(kc == 7)),
                                  reads=[h_.b, Win.b], writes=[p_.b])
                        if nch < 2:
                            S.add("act", lambda e, p_=p_, nch=nch: e.activation(out=qk[:, nch * 512:(nch + 1) * 512], in_=p_[:], func=AF.Copy), reads=[p_.b], writes=[qk.b])
                        elif nch == 2:
                            S.add("dve", lambda e, p_=p_: e.tensor_copy(out=vt[:].rearrange("p (h c) -> p h c", c=65)[:, :, 0:64], in_=p_[:].rearrange("p (h c) -> p h c", c=64)),
                                  reads=[p_.b], writes=[vt.b])
                        elif nch == 3:
                            S.add("act", lambda e, p_=p_: e.activation(out=guv[:, 0:512], in_=p_[:], func=AF.Gelu_apprx_tanh), reads=[p_.b], writes=[guv.b])
                        else:
                            S.add("act", lambda e, p_=p_: e.activation(out=guv[:, 512:1024], in_=p_[:], func=AF.Gelu_apprx_tanh, accum_out=s_[:, 4:5]), reads=[p_.b], writes=[guv.b, s_.b])
                    yield
                    if plevel < 2.2:
                        return
                    c0, nh = (0, 16) if need_q else (512, 8)
                    S.add("dve", lambda e: e.tensor_tensor(out=sqb[:, c0:1024], in0=qk[:, c0:1024], in1=qk[:, c0:1024], op=ALU.mult), reads=[qk.b], writes=[sqb.b])
                    S.add("dve", lambda e: e.tensor_reduce(out=s_[:, 16:16 + nh], in_=sqb[:, c0:1024].rearrange("p (h c) -> p h c", c=64), axis=AX.X, op=ALU.add),
                          reads=[sqb.b], writes=[s_.b])
                    rstd_ops((s_[:, 16:16 + nh], s_.b), (s_[:, 48:48 + nh], s_.b), (s_[:, 32:32 + nh], s_.b), 64)
                    yield
                    if plevel < 2.5:
                        return
                    S.add("dve", lambda e: e.tensor_tensor(out=qkn[:, c0:1024].rearrange("p (h c) -> p h c", c=64), in0=qk[:, c0:1024].rearrange("p (h c) -> p h c", c=64),
                                                          in1=s_[:, 48:48 + nh].unsqueeze(2).to_broadcast([128, nh, 64]), op=ALU.mult), reads=[qk.b, s_.b], writes=[qkn.b])
                    yield
                    for j in range(0 if need_q else 4, 8):
                        S.add("pe", lambda e, j=j: e.transpose(out=T0[:, j * 128:(j + 1) * 128], in_=qkn[:, j * 128:(j + 1) * 128], identity=ident[:]),
                              reads=[qkn.b, ident.b], writes=[T0.b])
                    yield
                    if plevel < 2.8:
                        return
                    ev = _os.environ.get("DBG_EV", "both")
                    if need_q and ev in ("q", "both"):
                        S.add("act", lambda e: e.activation(out=qt[:].rearrange("p a b -> p (a b)"), in_=T0[:, 0:512], func=AF.Identity, scale=g2[:, 2:3]),
                              reads=[T0.b, g2.b], writes=[qt.b])
                    if ev in ("k", "both"):
                        S.add("act", lambda e: e.activation(out=kt[:].rearrange("p a b -> p (a b)"), in_=T0[:, 512:1024], func=AF.Copy), reads=[T0.b], writes=[kt.b])
                    yield
                    if not need_q or plevel < 4:
                        return
                    S.add("dve", lambda e: e.tensor_scalar(out=s_[:, 5:6], in0=s_[:, 4:5], scalar1=-1.0 / 512, scalar2=None, op0=ALU.mult), reads=[s_.b], writes=[s_.b])
                    S.add("act", lambda e: e.activation(out=junk[:, 0:512], in_=guv[:, 512:1024], func=AF.Square, bias=s_[:, 5:6], accum_out=s_[:, 6:7]),
                          reads=[guv.b, s_.b], writes=[junk.b, s_.b])
                    rstd_ops((s_[:, 6:7], s_.b), (s_[:, 8:9], s_.b), (s_[:, 7:8], s_.b), 512)
                    S.add("dve", lambda e: e.tensor_scalar(out=vsn[:], in0=guv[:, 512:1024], scalar1=s_[:, 5:6], scalar2=s_[:, 8:9], op0=ALU.add, op1=ALU.mult),
                          reads=[guv.b, s_.b], writes=[vsn.b])
                    yield
                    p_ = P[0]
                    for h in range(8):
                        S.add("pe", lambda e, h=h: e.matmul(p_[:, h * 64:(h + 1) * 64], lhsT=swTb[:, h * 128:(h + 1) * 128], rhs=vsn[:, h * 64:(h + 1) * 64], start=True, stop=True),
                              reads=[swTb.b, vsn.b], writes=[p_.b])
                    yield
                    S.add("dve", lambda e: e.tensor_tensor(out=t1[:], in0=p_[:], in1=slnb[:], op=ALU.mult), reads=[p_.b, slnb.b], writes=[t1.b])
                    S.add("dve", lambda e: e.tensor_tensor(out=t1[:].rearrange("p (h c) -> p h c", c=64), in0=t1[:].rearrange("p (h c) -> p h c", c=64),
                                                          in1=sb[:].unsqueeze(2).to_broadcast([128, 8, 64]), op=ALU.add), reads=[t1.b, sb.b], writes=[t1.b])
                    S.add("dve", lambda e: e.tensor_tensor(out=osg_t[:], in0=t1[:], in1=guv[:, 0:512], op=ALU.mult), reads=[t1.b, guv.b], writes=[osg_t.b])
                    yield
                    S.add("act", lambda e: e.activation(out=junk[:, 512:1024], in_=osg_t[:], func=AF.Square, accum_out=rsg_t[:, 0:1]), reads=[osg_t.b], writes=[junk.b, rsg_t.b])
                    rstd_ops((rsg_t[:, 0:1], rsg_t.b), (rsg_t[:, 2:3], rsg_t.b), (rsg_t[:, 1:2], rsg_t.b), 512)

                acnt = {"a": 0}

                def attn(src_ap, src_b, dst_ap, dst_b, stream, qt, keys, Eset, osg_t, rsg_t):
                    nk = len(keys)
                    nw = sum(1 for k_ in keys if k_[2] is not None)
                    def emit_qk(h):
                        hp, po = h // 2, (h % 2) * 64
                        SA, SB2 = Sb[(h % 2) * 2], Sb[(h % 2) * 2 + 1]
                        for i, (kt, vt, eo) in enumerate(keys):
                            bank = SA if i < 4 else SB2
                            col = (i % 4) * 128
                            S.add("pe", lambda e, kt=kt, bank=bank, col=col, po=po, hp=hp: e.matmul(bank[:, col:col + 128], lhsT=kt[po:po + 64, hp, :], rhs=qt[po:po + 64, hp, :], start=True, stop=True),
                                  reads=[kt.b, qt.b], writes=[bank.b])

                    emit_qk(0)
                    for h in range(8):
                        SA, SB2 = Sb[(h % 2) * 2], Sb[(h % 2) * 2 + 1]
                        p_ = pT[h % 2]
                        if h + 1 < 8:
                            emit_qk(h + 1)
                        yield
                        na = min(nk, 4)
                        S.add("act", lambda e, SA=SA, p_=p_, na=na: e.activation(out=p_[:, 0:na * 128], in_=SA[:, 0:na * 128], func=AF.Exp), reads=[SA.b], writes=[p_.b])
                        if nk > 4:
                            S.add("act", lambda e, SB2=SB2, p_=p_: e.activation(out=p_[:, 512:nk * 128], in_=SB2[:, 0:(nk - 4) * 128], func=AF.Exp), reads=[SB2.b], writes=[p_.b])
                        yield
                        if nw > 0:
                            S.add("dve", lambda e, p_=p_, h=h: e.tensor_tensor(out=p_[:, 0:nw * 128].rearrange("p (a b) -> p a b", b=128), in0=p_[:, 0:nw * 128].rearrange("p (a b) -> p a b", b=128),
                                                                              in1=Eset[:, 0:nw, h * 128:(h + 1) * 128], op=ALU.mult), reads=[p_.b, Eset.b], writes=[p_.b])
                        yield
                        oc = (h % 4) * 65
                        for i, (kt, vt, eo) in enumerate(keys):
                            S.add("pe", lambda e, vt=vt, i=i, p_=p_, oc=oc, h=h: e.matmul(O0[:, oc:oc + 65], lhsT=p_[:, i * 128:(i + 1) * 128], rhs=vt[:, h * 65:(h + 1) * 65], start=(i == 0), stop=(i == nk - 1)),
                                  reads=[p_.b, vt.b], writes=[O0.b])
                        yield
                        if h % 4 == 3:
                            hb = h - 3
                            S.add("dve", lambda e, hb=hb: e.reciprocal(out=ast[:, hb:hb + 4], in_=O0[:, 0:260].rearrange("p (h c) -> p h c", c=65)[:, :, 64]), reads=[O0.b], writes=[ast.b])
                            S.add("dve", lambda e, hb=hb: e.tensor_tensor(out=ona[:, hb * 64:(hb + 4) * 64].rearrange("p (h c) -> p h c", c=64), in0=O0[:, 0:260].rearrange("p (h c) -> p h c", c=65)[:, :, 0:64],
                                                                         in1=ast[:, hb:hb + 4].unsqueeze(2).to_broadcast([128, 4, 64]), op=ALU.mult), reads=[O0.b, ast.b], writes=[ona.b])
                    yield
                    S.add("act", lambda e: e.activation(out=junk[:, 0:512], in_=ona[:], func=AF.Square, accum_out=ast[:, 8:9]), reads=[ona.b], writes=[junk.b, ast.b])
                    rstd_ops((ast[:, 8:9], ast.b), (ast[:, 10:11], ast.b), (ast[:, 9:10], ast.b), 512)
                    S.add("dve", lambda e: e.tensor_scalar(out=yn[:, 0:512], in0=ona[:], scalar1=ast[:, 10:11], scalar2=None, op0=ALU.mult), reads=[ona.b, ast.b], writes=[yn.b])
                    S.add("dve", lambda e: e.tensor_scalar(out=yn[:, 512:1024], in0=osg_t[:], scalar1=rsg_t[:, 2:3], scalar2=None, op0=ALU.mult), reads=[osg_t.b, rsg_t.b], writes=[yn.b])
                    yield
                    for kc in range(8):
                        S.add("pe", lambda e, kc=kc: e.transpose(out=T1[:, kc * 128:(kc + 1) * 128], in_=yn[:, kc * 128:(kc + 1) * 128], identity=ident[:]), reads=[yn.b, ident.b], writes=[T1.b])
                    for kc in range(8):
                        if kc % 2 == 0:
                            S.add("act", lambda e, kc=kc: e.activation(out=yT[:, kc, :], in_=T1[:, kc * 128:(kc + 1) * 128], func=AF.Identity, scale=og[:, kc:kc + 1]),
                                  reads=[T1.b, og.b], writes=[yT.b])
                        else:
                            S.add("dve", lambda e, kc=kc: e.tensor_scalar(out=yT[:, kc, :], in0=T1[:, kc * 128:(kc + 1) * 128], scalar1=og[:, kc:kc + 1], scalar2=zc[:, 0:1], op0=ALU.mult, op1=ALU.add),
                                  reads=[T1.b, og.b, zc.b], writes=[yT.b])
                    yield
                    S.add("sp", lambda e: e.dma_start(out=xa[:], in_=src_ap), reads=[src_b], writes=[xa.b], slot=xa.b)
                    xo_ = xo[0]
                    acnt["a"] += 1
                    for half in range(2):
                        yield
                        p_ = Sb[half]
                        for kc in range(8):
                            S.add("pe", lambda e, p_=p_, kc=kc, half=half: e.matmul(p_[:], lhsT=yT[:, kc, :], rhs=Wout[:, kc, half * 512:(half + 1) * 512], start=(kc == 0), stop=(kc == 7)),
                                  reads=[yT.b, Wout.b], writes=[p_.b])
                        S.add("dve", lambda e, p_=p_, half=half: e.tensor_tensor(out=xo_[:, half * 512:(half + 1) * 512], in0=p_[:], in1=gates[stream][0][:, half * 512:(half + 1) * 512], op=ALU.mult),
                              reads=[p_.b, gates[stream][0].b], writes=[xo_.b])
                    S.add("dve", lambda e: e.tensor_tensor(out=xo_[:], in0=xo_[:], in1=xa[:], op=ALU.add), reads=[xo_.b, xa.b], writes=[xo_.b])
                    S.add("pool", lambda e: e.dma_start(out=dst_ap, in_=xo_[:]), reads=[xo_.b], writes=[dst_b], slot=xo_.b)

                pa_mode = _os.environ.get("DBG_PA", "full")
                def run(*gens):
                    gens = list(gens)
                    while gens:
                        for g_ in list(gens):
                            try:
                                next(g_)
                            except StopIteration:
                                gens.remove(g_)

                for ci in range(2 if pa_mode != "setup" else 0):
                    run(proj(cin[ci * 128:(ci + 1) * 128, :], cin_b, 1, cKT[ci], cV[ci], cQT[ci], cosg[ci], crsg[ci], l == 0))
                if l == 0 and pa_mode == "full":
                    for ci in range(2):
                        run(attn(cin[ci * 128:(ci + 1) * 128, :], cin_b, ctxm[ci * 128:(ci + 1) * 128, :], dbuf("ctxm"), 1, cQT[ci],
                                 [(cKT[0], cV[0], None), (cKT[1], cV[1], None)], Eint, cosg[ci], crsg[ci]))

                def do_attn(t):
                    if t in (4, 5, 34, 35):
                        sidx = {4: 1, 5: 2, 34: 3, 35: 4}[t]
                        load_E(Esp, sidx)
                        offs = list(range(-2, 4)) if t in (4, 5) else list(range(-3, 3))
                        Eset = Esp
                    else:
                        offs = list(range(-2, 3))
                        Eset = Eint
                    keys = [(KT[(t + o) % 8], V[(t + o) % 8], i) for i, o in enumerate(offs)]
                    keys += [(cKT[0], cV[0], None), (cKT[1], cV[1], None)]
                    return attn(xin[t * 128:(t + 1) * 128, :], xin_b, xm[t * 128:(t + 1) * 128, :], dbuf("xm"), 0, QT[t % 4], keys, Eset, osg[t % 4], rsg[t % 4])

                if pa_mode == "setup":
                    kv_hi = kv_lo
                if pa_mode != "full":
                    q_hi = q_lo
                for t in range(kv_lo, kv_hi):
                    nq = q_lo <= t < q_hi
                    pg_ = proj(xin[t * 128:(t + 1) * 128, :], xin_b, 0, KT[t % 8], V[t % 8], QT[t % 4], osg[t % 4], rsg[t % 4], nq)
                    ta = t - 3
                    if q_lo <= ta < q_hi:
                        if ta in (4, 5):
                            run(pg_)
                            run(do_attn(ta))
                        else:
                            run(pg_, do_attn(ta))
                    else:
                        run(pg_)
                    emit_wjobs(10)
                emit_wjobs(len(wjobs))
                for t in range(kv_hi - 3, q_hi):
                    if t >= q_lo:
                        run(do_attn(t))
                S.barrier()

            if not do_f:
                return
            with ExitStack() as es2:
              if "F" in stages:
                U32 = mybir.dt.uint32
                mk2 = lambda name, shape, dt, psum=False: T(S, es2, nc, name + "_L%d" % l, shape, dt, psum)
                TS = 512
                tiles_all = []
                if l == 0:
                    for ci in range(2):
                        tiles_all.append((ctxm[ci * 128:(ci + 1) * 128, :], dbuf("ctxm"), ctx1[ci * 128:(ci + 1) * 128, :], dbuf("ctx1"), 1, 38 + ci))
                for t in range(q_lo, q_hi):
                    if l == n_layers - 1 and l == 1:
                        dap, db_ = out[(t - 4) * 128:(t - 3) * 128, :], dbuf("out")
                    else:
                        dap, db_ = x1[t * 128:(t + 1) * 128, :], dbuf("x1")
                    tiles_all.append((xm[t * 128:(t + 1) * 128, :], dbuf("xm"), dap, db_, 0, t if t < 38 else t))
                NTOK = len(tiles_all)
                NT = (2 * NTOK * 128 + TS - 1) // TS + 16
                assert NT <= NTMAX
                ew1 = [mk2("ew1_%d" % i, [128, 4096], BF16) for i in range(2)]
                ew3 = [mk2("ew3_%d" % i, [128, 4096], BF16) for i in range(2)]
                ew2 = [mk2("ew2_%d" % i, [128, 4096], BF16) for i in range(2)]
                xg = mk2("xg", [128, 4, D], F32)
                junkf = mk2("junkf", [128, D], BF16)
                xn32 = mk2("xn32", [128, D], F32)
                hf32 = mk2("hf32", [128, 8, 128], F32)
                hs = [mk2("hs%d" % i, [128, D], BF16) for i in range(2)]
                rwt = mk2("rwt", [128, 8, 20], F32)
                rbb = mk2("rbb", [128, 20], F32)
                lg = mk2("lg", [128, 4, 20], F32)
                r = {n_: mk2("r_" + n_, [128, 4, 16], F32) for n_ in ("a", "b", "c", "d", "e", "f", "g", "h", "i", "j", "k")}
                fs = mk2("fs", [128, 16], F32)
                OH = [mk2("OH%d" % i, [128, NTOK, 16], F32) for i in range(2)]
                POS = mk2("POS", [128, NTOK, 16], F32)
                WAB = [mk2("WAB%d" % i, [128, NTOK], F32) for i in range(2)]
                base = mk2("base", [128, 16], F32)
                sel = mk2("sel", [128, 4, 16], BF16)
                cU = mk2("cU", [128, 128], BF16)
                cOnes = mk2("cOnes", [128, 128], BF16)
                itc = mk2("itc", [128, NTMAX, 16], F32)
                cmpt = mk2("cmpt", [128, NTMAX, 16], F32)
                pidx = mk2("pidx", [128, 2], F32)
                offs = mk2("offs", [128, 4, 16], F32)
                eid = mk2("eid", [128, NTMAX], F32)
                widx = mk2("widx", [128, NTMAX], U32)
                slf = [mk2("slf%d" % i, [128, NTOK], F32) for i in range(2)]
                slu = [mk2("slu%d" % i, [128, NTOK], U32) for i in range(2)]
                ptmp = mk2("ptmp", [128, NTOK, 16], F32)
                srt = [mk2("srt%d" % i, [128, 4, D], BF16) for i in range(2)]
                hfT = mk2("hfT", [128, 8, 512], BF16)
                sl = [mk2("sl%d" % i, [128, 512], BF16) for i in range(2)]
                hidT = [mk2("hidT%d" % i, [128, 4, 512], BF16) for i in range(2)]
                ob = [mk2("ob%d" % i, [128, D], F32) for i in range(2)]
                ra = mk2("ra", [128, D], F32)
                rb_ = mk2("rb", [128, D], F32)
                xr = mk2("xr", [128, D], F32)
                xo = [mk2("fxo%d" % i, [128, D], F32) for i in range(1)]
                H1 = [mk2("H1_%d" % i, [128, 512], F32, True) for i in range(2)]
                H3 = [mk2("H3_%d" % i, [128, 512], F32, True) for i in range(2)]
                OUT = [mk2("OUT%d" % i, [128, 512], F32, True) for i in range(2)]
                TB = mk2("TB", [128, 1024], BF16, True)
                TF = mk2("TF", [128, 512], F32, True)
                S.add("sp", lambda e: e.dma_start(out=rwt[:], in_=rw[l].rearrange("(kc p) n -> p kc n", p=128)), writes=[rwt.b], slot=rwt.b)
                S.add("sp", lambda e: e.dma_start(out=rbb[:], in_=rb[l].partition_broadcast(128)), writes=[rbb.b], slot=rbb.b)
                fbc = [[mk2("ffbc%d%d" % (s_, k_), [128, D], F32) for k_ in range(2)] for s_ in range(2)]
                for s_ in range(2):
                    for k_ in range(2):
                        f_ = fbc[s_][k_]
                        S.add("sp", lambda e, f_=f_, s_=s_, k_=k_: e.dma_start(out=f_[:], in_=fbc_d[s_ * 2 + k_]), writes=[f_.b], slot=f_.b)
                S.add("sp", lambda e: e.dma_start(out=cU[:], in_=cU_d[:, :]), writes=[cU.b], slot=cU.b)
                S.add("sp", lambda e: e.dma_start(out=cOnes[:], in_=cOnes_d[:, :]), writes=[cOnes.b], slot=cOnes.b)
                S.add("sp", lambda e: e.dma_start(out=itc[:].rearrange("p a b -> p (a b)"), in_=itc_d[:, :]), writes=[itc.b], slot=itc.b)
                S.add("sp", lambda e: e.dma_start(out=pidx[:], in_=pidx_d[:, :]), writes=[pidx.b], slot=pidx.b)
                S.add("pool", lambda e: e.memset(base[:], 0.0), writes=[base.b])
                S.add("pool", lambda e: e.memset(hfT[:], 0.0), writes=[hfT.b])
                srt_flat = srt_d.rearrange("r n -> (r n)").rearrange("(p f) -> p f", p=128)
                for i in range(NT):
                    S.add("sp", lambda e, i=i: e.dma_start(out=srt_d[i * 512:(i + 1) * 512, :].rearrange("(p a) n -> p (a n)", p=128), in_=hfT[:].rearrange("p a b -> p (a b)")),
                          reads=[hfT.b], writes=[dbuf("srt") if i == NT - 1 else S.buf("zf")], slot=hfT.b)

                def dv(fn, reads, writes):
                    S.add("dve", fn, reads=[x_.b for x_ in reads], writes=[x_.b for x_ in writes])

                hcnt = {"h": 0}

                def route_group(idx0, G):
                    for ti in range(G):
                        sap, sbuf_, dap, dbuf_, stream, hrow = tiles_all[idx0 + ti]
                        S.add("sp", lambda e, ti=ti, sap=sap: e.dma_start(out=xg[:, ti, :], in_=sap), reads=[sbuf_], writes=[xg.b], slot=xg.b)
                    for ti in range(G):
                        sap, sbuf_, dap, dbuf_, stream, hrow = tiles_all[idx0 + ti]
                        S.add("act", lambda e, ti=ti: e.activation(out=junkf[:], in_=xg[:, ti, :], func=AF.Square, accum_out=fs[:, 0:1]), reads=[xg.b], writes=[junkf.b, fs.b])
                        rstd_ops((fs[:, 0:1], fs.b), (fs[:, 2:3], fs.b), (fs[:, 1:2], fs.b), D)
                        S.add("dve", lambda e, ti=ti: e.tensor_scalar(out=xn32[:], in0=xg[:, ti, :], scalar1=fs[:, 2:3], scalar2=None, op0=ALU.mult), reads=[xg.b, fs.b], writes=[xn32.b])
                        h_ = hs[hcnt["h"] % 2]
                        hcnt["h"] += 1
                        S.add("dve", lambda e, stream=stream: e.tensor_tensor(out=ra[:], in0=xn32[:], in1=fbc[stream][1][:], op=ALU.mult), reads=[xn32.b, fbc[stream][1].b], writes=[ra.b])
                        S.add("dve", lambda e, stream=stream, h_=h_: e.tensor_tensor(out=h_[:], in0=ra[:], in1=fbc[stream][0][:], op=ALU.add), reads=[ra.b, fbc[stream][0].b], writes=[h_.b])
                        S.add("pool", lambda e, h_=h_, hrow=hrow: e.dma_start(out=hfd[hrow * 128:(hrow + 1) * 128, :], in_=h_[:]), reads=[h_.b], writes=[dbuf("hfd")], slot=h_.b)
                        for half in range(2):
                            for kc in range(4):
                                S.add("pe", lambda e, kc=kc, half=half: e.transpose(out=TF[:, kc * 128:(kc + 1) * 128], in_=xn32[:, (half * 4 + kc) * 128:(half * 4 + kc + 1) * 128], identity=identf[:]),
                                      reads=[xn32.b, identf.b], writes=[TF.b])
                            for kc in range(4):
                                k8 = half * 4 + kc
                                S.add("dve", lambda e, kc=kc, k8=k8, stream=stream: e.tensor_scalar(out=hf32[:, k8, :], in0=TF[:, kc * 128:(kc + 1) * 128], scalar1=modf[:, 3, k8, stream:stream + 1],
                                                                                                 scalar2=modf[:, 2, k8, stream:stream + 1], op0=ALU.mult, op1=ALU.add), reads=[TF.b, modf.b], writes=[hf32.b])
                        for kc in range(8):
                            S.add("pe", lambda e, kc=kc, ti=ti: e.matmul(H1[0][:, ti * 20:(ti + 1) * 20], lhsT=hf32[:, kc, :], rhs=rwt[:, kc, :], start=(kc == 0), stop=(kc == 7)),
                                  reads=[hf32.b, rwt.b], writes=[H1[0].b])
                        S.add("dve", lambda e, ti=ti: e.tensor_tensor(out=lg[:, ti, :], in0=H1[0][:, ti * 20:(ti + 1) * 20], in1=rbb[:], op=ALU.add), reads=[H1[0].b, rbb.b], writes=[lg.b])
                    lgG = lg[:, 0:G, 0:4]
                    lgE = lg[:, 0:G, 4:20]
                    mg, ohg, dg, eg, sg, pt = r["a"], r["b"], r["c"], r["d"], r["e"], r["f"]
                    dv(lambda e: e.tensor_reduce(out=mg[:, 0:G, 0], in_=lgG, axis=AX.X, op=ALU.max), [lg], [mg])
                    dv(lambda e: e.tensor_tensor(out=ohg[:, 0:G, 0:4], in0=lgG, in1=mg[:, 0:G, 0:1].to_broadcast([128, G, 4]), op=ALU.is_equal), [lg, mg], [ohg])
                    dv(lambda e: e.tensor_tensor(out=dg[:, 0:G, 0:4], in0=lgG, in1=mg[:, 0:G, 0:1].to_broadcast([128, G, 4]), op=ALU.subtract), [lg, mg], [dg])
                    S.add("act", lambda e: e.activation(out=eg[:, 0:G, 0:4], in_=dg[:, 0:G, 0:4], func=AF.Exp), reads=[dg.b], writes=[eg.b])
                    dv(lambda e: e.tensor_reduce(out=sg[:, 0:G, 0], in_=eg[:, 0:G, 0:4], axis=AX.X, op=ALU.add), [eg], [sg])
                    dv(lambda e: e.reciprocal(out=pt[:, 0:G, 0], in_=sg[:, 0:G, 0]), [sg], [pt])
                    tmp, el = r["g"], r["h"]
                    dv(lambda e: e.tensor_tensor(out=tmp[:, 0:G, :].rearrange("p t (g j) -> p t g j", j=4), in0=lgE.rearrange("p t (g j) -> p t g j", j=4),
                                                 in1=ohg[:, 0:G, 0:4].unsqueeze(3).to_broadcast([128, G, 4, 4]), op=ALU.mult), [lg, ohg], [tmp])
                    dv(lambda e: e.tensor_reduce(out=el[:, 0:G, 0:4], in_=tmp[:, 0:G, :].rearrange("p t (g j) -> p t j g", j=4), axis=AX.X, op=ALU.add), [tmp], [el])
                    m1, oh1, el2, m2, oh2 = r["i"], r["j"], r["k"], r["c"], r["d"]
                    dv(lambda e: e.tensor_reduce(out=m1[:, 0:G, 0], in_=el[:, 0:G, 0:4], axis=AX.X, op=ALU.max), [el], [m1])
                    dv(lambda e: e.tensor_tensor(out=oh1[:, 0:G, 0:4], in0=el[:, 0:G, 0:4], in1=m1[:, 0:G, 0:1].to_broadcast([128, G, 4]), op=ALU.is_equal), [el, m1], [oh1])
                    dv(lambda e: e.scalar_tensor_tensor(out=el2[:, 0:G, 0:4], in0=oh1[:, 0:G, 0:4], scalar=-1e30, in1=el[:, 0:G, 0:4], op0=ALU.mult, op1=ALU.add), [oh1, el], [el2])
                    dv(lambda e: e.tensor_reduce(out=m2[:, 0:G, 0], in_=el2[:, 0:G, 0:4], axis=AX.X, op=ALU.max), [el2], [m2])
                    dv(lambda e: e.tensor_tensor(out=oh2[:, 0:G, 0:4], in0=el2[:, 0:G, 0:4], in1=m2[:, 0:G, 0:1].to_broadcast([128, G, 4]), op=ALU.is_equal), [el2, m2], [oh2])
                    dd, ee, w1_, w2_ = r["a"], r["e"], r["g"], r["h"]
                    dv(lambda e: e.tensor_tensor(out=dd[:, 0:G, 0], in0=m2[:, 0:G, 0], in1=m1[:, 0:G, 0], op=ALU.subtract), [m2, m1], [dd])
                    S.add("act", lambda e: e.activation(out=ee[:, 0:G, 0], in_=dd[:, 0:G, 0], func=AF.Exp), reads=[dd.b], writes=[ee.b])
                    dv(lambda e: e.tensor_scalar(out=dd[:, 0:G, 1], in0=ee[:, 0:G, 0], scalar1=1.0, scalar2=None, op0=ALU.add), [ee], [dd])
                    dv(lambda e: e.reciprocal(out=dd[:, 0:G, 2], in_=dd[:, 0:G, 1]), [dd], [dd])
                    dv(lambda e: e.tensor_tensor(out=w1_[:, 0:G, 0], in0=dd[:, 0:G, 2], in1=pt[:, 0:G, 0], op=ALU.mult), [dd, pt], [w1_])
                    dv(lambda e: e.tensor_tensor(out=w2_[:, 0:G, 0], in0=w1_[:, 0:G, 0], in1=ee[:, 0:G, 0], op=ALU.mult), [w1_, ee], [w2_])
                    wa_, wb_ = r["i"], r["k"]
                    dv(lambda e: e.tensor_tensor(out=wa_[:, 0:G, 0:4], in0=oh1[:, 0:G, 0:4], in1=w1_[:, 0:G, 0:1].to_broadcast([128, G, 4]), op=ALU.mult), [oh1, w1_], [wa_])
                    dv(lambda e: e.tensor_tensor(out=wb_[:, 0:G, 0:4], in0=oh2[:, 0:G, 0:4], in1=w2_[:, 0:G, 0:1].to_broadcast([128, G, 4]), op=ALU.mult), [oh2, w2_], [wb_])
                    dv(lambda e: e.tensor_tensor(out=wa_[:, 0:G, 0:4], in0=wa_[:, 0:G, 0:4], in1=wb_[:, 0:G, 0:4], op=ALU.add), [wa_, wb_], [wa_])
                    for k_, oh_ in enumerate((oh1, oh2)):
                        dv(lambda e, k_=k_, oh_=oh_: e.tensor_tensor(out=OH[k_][:, idx0:idx0 + G, :].rearrange("p t (g j) -> p t g j", j=4), in0=ohg[:, 0:G, 0:4].unsqueeze(3).to_broadcast([128, G, 4, 4]),
                                                                    in1=oh_[:, 0:G, 0:4].unsqueeze(2).to_broadcast([128, G, 4, 4]), op=ALU.mult), [ohg, oh_], [OH[k_]])
                    dv(lambda e: e.tensor_copy(out=WAB[0][:, idx0:idx0 + G], in_=w1_[:, 0:G, 0]), [w1_], [WAB[0]])
                    dv(lambda e: e.tensor_copy(out=WAB[1][:, idx0:idx0 + G], in_=w2_[:, 0:G, 0]), [w2_], [WAB[1]])
                    dv(lambda e: e.tensor_tensor(out=sel[:, 0:G, :], in0=OH[0][:, idx0:idx0 + G, :], in1=OH[1][:, idx0:idx0 + G, :], op=ALU.add), [OH[0], OH[1]], [sel])
                    for ti in range(G):
                        S.add("pe", lambda e, ti=ti: e.matmul(H3[0][:, ti * 32:ti * 32 + 16], lhsT=cU[:], rhs=sel[:, ti, :], start=True, stop=True), reads=[cU.b, sel.b], writes=[H3[0].b])
                        S.add("pe", lambda e, ti=ti: e.matmul(H3[0][:, ti * 32 + 16:ti * 32 + 32], lhsT=cOnes[:], rhs=sel[:, ti, :], start=True, stop=True), reads=[cOnes.b, sel.b], writes=[H3[0].b])
                        dv(lambda e, ti=ti: e.tensor_tensor(out=POS[:, idx0 + ti, :], in0=H3[0][:, ti * 32:ti * 32 + 16], in1=base[:], op=ALU.add), [H3[0], base], [POS])
                        dv(lambda e, ti=ti: e.tensor_tensor(out=base[:], in0=H3[0][:, ti * 32 + 16:ti * 32 + 32], in1=base[:], op=ALU.add), [H3[0], base], [base])

                for g0 in range(0, NTOK, 4):
                    route_group(g0, min(4, NTOK - g0))
                o_tmp, o_pad, o_off, o_end = offs[:, 0, :], offs[:, 1, :], offs[:, 2, :], offs[:, 3, :]
                dv(lambda e: e.tensor_tensor(out=cmpt[:, 0:19, :].rearrange("p j e -> p e j"), in0=itc[:, 0:19, :].rearrange("p j e -> p e j"),
                                             in1=base[:].unsqueeze(2).to_broadcast([128, 16, 19]), op=ALU.is_lt), [itc, base], [cmpt])
                dv(lambda e: e.tensor_reduce(out=o_tmp, in_=cmpt[:, 0:19, :].rearrange("p j e -> p e j"), axis=AX.X, op=ALU.add), [cmpt], [offs])
                dv(lambda e: e.tensor_scalar(out=o_pad, in0=o_tmp, scalar1=float(TS), scalar2=None, op0=ALU.mult), [offs], [offs])
                S.add("pool", lambda e: e.memset(offs[:, 2, 0:1], 0.0), reads=[offs.b], writes=[offs.b])
                for ex in range(1, 16):
                    dv(lambda e, ex=ex: e.tensor_tensor(out=offs[:, 2, ex:ex + 1], in0=offs[:, 2, ex - 1:ex], in1=offs[:, 1, ex - 1:ex], op=ALU.add), [offs], [offs])
                dv(lambda e: e.tensor_tensor(out=o_end, in0=o_off, in1=o_pad, op=ALU.add), [offs], [offs])
                dv(lambda e: e.tensor_tensor(out=cmpt[:, 0:NT, :], in0=itc[:, 0:NT, :], in1=offs[:, 3:4, :].to_broadcast([128, NT, 16]), op=ALU.is_ge), [itc, offs], [cmpt])
                dv(lambda e: e.tensor_reduce(out=eid[:, 0:NT], in_=cmpt[:, 0:NT, :], axis=AX.X, op=ALU.add), [cmpt], [eid])
                dv(lambda e: e.tensor_scalar(out=eid[:, 0:NT], in0=eid[:, 0:NT], scalar1=15.0, scalar2=128.0, op0=ALU.min, op1=ALU.mult), [eid], [eid])
                dv(lambda e: e.tensor_scalar(out=eid[:, 0:NT], in0=eid[:, 0:NT], scalar1=pidx[:, l:l + 1], scalar2=None, op0=ALU.add), [eid, pidx], [eid])
                dv(lambda e: e.tensor_copy(out=widx[:, 0:NT], in_=eid[:, 0:NT]), [eid], [widx])
                dv(lambda e: e.tensor_tensor(out=POS[:], in0=POS[:], in1=offs[:, 2:3, :].to_broadcast([128, NTOK, 16]), op=ALU.add), [POS, offs], [POS])
                for k_ in range(2):
                    dv(lambda e, k_=k_: e.tensor_tensor(out=ptmp[:], in0=POS[:], in1=OH[k_][:], op=ALU.mult), [POS, OH[k_]], [ptmp])
                    dv(lambda e, k_=k_: e.tensor_reduce(out=slf[k_][:], in_=ptmp[:], axis=AX.X, op=ALU.add), [ptmp], [slf[k_]])
                    dv(lambda e, k_=k_: e.tensor_copy(out=slu[k_][:], in_=slf[k_][:]), [slf[k_]], [slu[k_]])
                for ti in range(NTOK):
                    hrow = tiles_all[ti][5]
                    h_ = hs[ti % 2]
                    S.add("sp", lambda e, h_=h_, hrow=hrow: e.dma_start(out=h_[:], in_=hfd[hrow * 128:(hrow + 1) * 128, :]), reads=[dbuf("hfd")], writes=[h_.b], slot=h_.b)
                    for k_ in range(2):
                        S.add("pool", lambda e, h_=h_, k_=k_, ti=ti: e.indirect_dma_start(out=srt_d[0:NT * 512, :], out_offset=bass.IndirectOffsetOnAxis(ap=slu[k_][:, ti:ti + 1], axis=0), in_=h_[:], in_offset=None),
                              reads=[h_.b, slu[k_].b], writes=[dbuf("srt")], slot=h_.b)
                for i in range(NT):
                    a1, a3, a2 = ew1[i % 2], ew3[i % 2], ew2[i % 2]
                    for a_, k_ in ((a1, "w1"), (a3, "w3"), (a2, "w2")):
                        S.add("pool", lambda e, a_=a_, k_=k_, i=i: e.indirect_dma_start(out=a_[:], out_offset=None, in_=wb[k_][:, :], in_offset=bass.IndirectOffsetOnAxis(ap=widx[:, i:i + 1], axis=0)),
                              reads=[widx.b], writes=[a_.b], slot=a_.b)
                    sr = srt[i % 2]
                    for ii in ([0, 1] if i == 0 else [i + 1]):
                        if ii < NT:
                            sr2 = srt[ii % 2]
                            S.add("sp", lambda e, sr2=sr2, ii=ii: e.dma_start(out=sr2[:], in_=srt_d[ii * 512:(ii + 1) * 512, :].rearrange("(a p) n -> p a n", p=128)), reads=[dbuf("srt")], writes=[sr2.b], slot=sr2.b)
                    for a in range(4):
                        for kc in range(8):
                            S.add("pe", lambda e, sr=sr, a=a, kc=kc: e.transpose(out=TB[:, kc * 128:(kc + 1) * 128], in_=sr[:, a, kc * 128:(kc + 1) * 128], identity=ident[:]), reads=[sr.b, ident.b], writes=[TB.b])
                        S.add("act", lambda e, a=a: e.activation(out=hfT[:, :, a * 128:(a + 1) * 128], in_=TB[:].rearrange("p (c t) -> p c t", t=128), func=AF.Copy), reads=[TB.b], writes=[hfT.b])
                    hd = hidT[i % 2]
                    a1v = a1[:].rearrange("p (c n) -> p c n", c=8)
                    a3v = a3[:].rearrange("p (c n) -> p c n", c=8)
                    a2v = a2[:].rearrange("p (c n) -> p c n", c=4)
                    for hc in range(4):
                        h1, h3, s_ = H1[hc % 2], H3[hc % 2], sl[hc % 2]
                        for kc in range(8):
                            S.add("pe", lambda e, h1=h1, a1v=a1v, kc=kc, hc=hc: e.matmul(h1[:], lhsT=a1v[:, kc, hc * 128:(hc + 1) * 128], rhs=hfT[:, kc, :], start=(kc == 0), stop=(kc == 7)),
                                  reads=[a1.b, hfT.b], writes=[h1.b])
                        for kc in range(8):
                            S.add("pe", lambda e, h3=h3, a3v=a3v, kc=kc, hc=hc: e.matmul(h3[:], lhsT=a3v[:, kc, hc * 128:(hc + 1) * 128], rhs=hfT[:, kc, :], start=(kc == 0), stop=(kc == 7)),
                                  reads=[a3.b, hfT.b], writes=[h3.b])
                        S.add("act", lambda e, h1=h1, s_=s_: e.activation(out=s_[:], in_=h1[:], func=AF.Silu), reads=[h1.b], writes=[s_.b])
                        S.add("dve", lambda e, h3=h3, s_=s_, hd=hd, hc=hc: e.tensor_tensor(out=hd[:, hc, :], in0=s_[:], in1=h3[:], op=ALU.mult), reads=[s_.b, h3.b], writes=[hd.b])
                    for ti in range(4):
                        o_b = ob[ti % 2]
                        for half in range(2):
                            o_ = OUT[half]
                            for hc in range(4):
                                S.add("pe", lambda e, o_=o_, hd=hd, a2v=a2v, hc=hc, ti=ti, half=half: e.matmul(o_[:], lhsT=hd[:, hc, ti * 128:(ti + 1) * 128], rhs=a2v[:, hc, half * 512:(half + 1) * 512],
                                                                                                        start=(hc == 0), stop=(hc == 3)), reads=[hd.b, a2.b], writes=[o_.b])
                            if half == 0:
                                S.add("act", lambda e, o_=o_, o_b=o_b: e.activation(out=o_b[:, 0:512], in_=o_[:], func=AF.Copy), reads=[o_.b], writes=[o_b.b])
                            else:
                                S.add("dve", lambda e, o_=o_, o_b=o_b: e.tensor_copy(out=o_b[:, 512:1024], in_=o_[:]), reads=[o_.b], writes=[o_b.b])
                        S.add("sp", lambda e, o_b=o_b, i=i, ti=ti: e.dma_start(out=sout[i * 512 + ti * 128:i * 512 + (ti + 1) * 128, :], in_=o_b[:]), reads=[o_b.b], writes=[(dbuf("sout") if ti == 3 else dbuf("sout2")) if i == NT - 1 and ti >= 2 else S.buf("so")], slot=o_b.b)
                for ti in range(NTOK):
                    sap, sbuf_, dap, dbuf_, stream, hrow = tiles_all[ti]
                    S.add("pool", lambda e, ti=ti: e.indirect_dma_start(out=ra[:], out_offset=None, in_=sout[0:NT * 512, :], in_offset=bass.IndirectOffsetOnAxis(ap=slu[0][:, ti:ti + 1], axis=0)),
                          reads=[dbuf("sout"), dbuf("sout2"), slu[0].b], writes=[ra.b], slot=ra.b)
                    S.add("pool", lambda e, ti=ti: e.indirect_dma_start(out=rb_[:], out_offset=None, in_=sout[0:NT * 512, :], in_offset=bass.IndirectOffsetOnAxis(ap=slu[1][:, ti:ti + 1], axis=0)),
                          reads=[dbuf("sout"), dbuf("sout2"), slu[1].b], writes=[rb_.b], slot=rb_.b)
                    S.add("sp", lambda e, sap=sap: e.dma_start(out=xr[:], in_=sap), reads=[sbuf_], writes=[xr.b], slot=xr.b)
                    xo_ = xo[0]
                    dv(lambda e, ti=ti: e.tensor_scalar(out=ra[:], in0=ra[:], scalar1=WAB[0][:, ti:ti + 1], scalar2=None, op0=ALU.mult), [ra, WAB[0]], [ra])
                    dv(lambda e, ti=ti: e.scalar_tensor_tensor(out=ra[:], in0=rb_[:], scalar=WAB[1][:, ti:ti + 1], in1=ra[:], op0=ALU.mult, op1=ALU.add), [rb_, WAB[1], ra], [ra])
                    dv(lambda e, stream=stream: e.tensor_tensor(out=ra[:], in0=ra[:], in1=gates[stream][1][:], op=ALU.mult), [ra, gates[stream][1]], [ra])
                    dv(lambda e, xo_=xo_: e.tensor_tensor(out=xo_[:], in0=ra[:], in1=xr[:], op=ALU.add), [ra, xr], [xo_])
                    S.add("sp", lambda e, dap=dap, xo_=xo_: e.dma_start(out=dap, in_=xo_[:]), reads=[xo_.b], writes=[dbuf_], slot=xo_.b)
                S.barrier()
        for l_ in range(n_layers):
            layer(l_)
        nsem = S.emit(es)
    return nc, nsem, len(S.ops)


def _bias_sets(rpb_l, j):
    sets = [(64, list(range(-2, 4))), (32 * j, list(range(-2, 4))), (32 * j + 1, list(range(-2, 4))),
            (32 * j + 30, list(range(-3, 3))), (32 * j + 31, list(range(-3, 3)))]
    outp = np.full((5, 6, 128, 8, 128), NEG, np.float32)
    idx = np.arange(128)
    qc = idx % 64
    kc = idx % 64
    cs = np.clip(qc - 8, 0, 48)
    for si, (m, offs) in enumerate(sets):
        qr = 2 * m + idx // 64
        rs = np.clip(qr - 4, 0, 248)
        for oi, o in enumerate(offs):
            kr = 2 * (m + o) + idx // 64
            valid = ((kr[:, None] >= 0) & (kr[:, None] < 256) & (kr[:, None] >= rs[None, :]) & (kr[:, None] < rs[None, :] + 8)
                     & (kc[:, None] >= cs[None, :]) & (kc[:, None] < cs[None, :] + 16))
            dr = np.clip(kr[:, None] - qr[None, :] + 7, 0, 14)
            dc = np.clip(kc[:, None] - qc[None, :] + 15, 0, 30)
            vals = rpb_l[:, dr, dc]
            vals = np.where(valid[None], vals, NEG)
            outp[si, oi] = np.transpose(vals, (1, 0, 2))
    return outp.reshape(5, 6, 128, 1024)


_CACHE = {}


def kernel(x, c, ctx, c_ctx, w_ada, b_ada, w_in, q_gain, k_gain, rpb, sgu_ln, sgu_w, sgu_b, out_gain, w_out,
           rg_w, rg_b, re_w, re_b, w1, w3, w2):
    f = lambda a: np.ascontiguousarray(np.asarray(a, dtype=np.float32))
    x, c, ctx, c_ctx, w_ada, b_ada, w_in, q_gain, k_gain, rpb = map(f, (x, c, ctx, c_ctx, w_ada, b_ada, w_in, q_gain, k_gain, rpb))
    sgu_ln, sgu_w, sgu_b, out_gain, w_out, rg_w, rg_b, re_w, re_b, w1, w3, w2 = map(
        f, (sgu_ln, sgu_w, sgu_b, out_gain, w_out, rg_w, rg_b, re_w, re_b, w1, w3, w2))
    if "nc" not in _CACHE:
        _CACHE["nc"] = build()[0]
    nc = _CACHE["nc"]
    shared = {
        "ident": np.eye(128, dtype=np.float32).astype(ml_dtypes.bfloat16),
        "identf": np.eye(128, dtype=np.float32),
        "w_ada": w_ada, "b_ada": b_ada,
        "b_adaT": np.ascontiguousarray(b_ada.reshape(2, 48, 128).transpose(0, 2, 1)),
        "w_in": w_in.reshape(128, -1), "w_out": w_out.reshape(128, -1),
        "w1": w1.reshape(32, 1024, 512), "w3": w3.reshape(32, 1024, 512), "w2": w2.reshape(32, 512, 1024),
        "cU": np.triu(np.ones((128, 128), np.float32), 1).astype(ml_dtypes.bfloat16),
        "cOnes": np.ones((128, 128), np.float32).astype(ml_dtypes.bfloat16),
        "itc": np.ascontiguousarray(np.broadcast_to((np.arange(35, dtype=np.float32) * 512.0)[None, :, None], (128, 35, 16)).reshape(128, 35 * 16)),
        "pidx": np.ascontiguousarray(np.stack([np.arange(128, dtype=np.float32), np.arange(128, dtype=np.float32) + 2048.0], axis=1)),
        "rw": np.ascontiguousarray(np.concatenate([rg_w, re_w.transpose(0, 2, 1, 3).reshape(2, 1024, 16)], axis=2)),
        "rb": np.ascontiguousarray(np.concatenate([rg_b, re_b.reshape(2, 16)], axis=1)),
        "qg": np.ascontiguousarray(np.tile(q_gain, (1, 2))[:, :, None]),
        "kg": np.ascontiguousarray(np.tile(k_gain, (1, 2))[:, :, None]),
        "sln": sgu_ln,
        "swT": np.ascontiguousarray(sgu_w.transpose(0, 3, 1, 2).reshape(2, 128, 1024)),
        "sbT": np.ascontiguousarray(sgu_b.transpose(0, 2, 1)),
        "ogT": np.ascontiguousarray(out_gain.reshape(2, 8, 128).transpose(0, 2, 1)),
    }
    xpad = np.zeros((2, 272, 64, D), np.float32)
    xpad[:, 8:264] = x.reshape(2, 256, 64, D)
    in_maps = []
    for core in range(8):
        b, j = core // 4, core % 4
        m = dict(shared)
        m["xw"] = np.ascontiguousarray(xpad[b, 64 * j:64 * j + 80].reshape(5120, D))
        m["ctx"] = ctx[b]
        cv = np.stack([c[b].reshape(8, 128).T, c_ctx.reshape(8, 128).T], axis=2)
        m["cvec"] = np.ascontiguousarray(cv.reshape(128, 16))
        m["bias"] = np.stack([_bias_sets(rpb[0], j), _bias_sets(rpb[1], j)], axis=0)
        in_maps.append(m)
    res = run_bass_kernel_spmd(nc, in_maps, core_ids=list(range(8)))
    outp = np.empty((2, 16384, D), np.float32)
    for core in range(8):
        b, j = core // 4, core % 4
        outp[b, 4096 * j:4096 * (j + 1)] = res.results[core]["out"]
    return outp
```

```python
import os as _os
import numpy as np
import ml_dtypes
from contextlib import ExitStack
import concourse.bass as bass
import concourse.mybir as mybir
from concourse.bass_utils import run_bass_kernel_spmd

F32, BF16 = mybir.dt.float32, mybir.dt.bfloat16
AF = mybir.ActivationFunctionType
ALU = mybir.AluOpType
AX = mybir.AxisListType

D = 1024
NEG = -30000.0
EPS = 1e-6
ENGS = ("sp", "pe", "act", "dve", "pool")


class Buf:
    __slots__ = ("name", "w", "r")

    def __init__(self, name):
        self.name, self.w, self.r = name, None, []


class Op:
    __slots__ = ("eng", "fn", "dma", "key", "deps", "signal", "val")


class Sched:
    def __init__(self, nc):
        self.nc = nc
        self.ops = []
        self.bufs = []
        self.dma_out = []
        self.last = {}

    def buf(self, name):
        b = Buf(name)
        self.bufs.append(b)
        return b

    def _dep(self, op, d, raw):
        if d is op:
            return
        if (not d.dma) and (not op.dma) and d.eng == op.eng and not raw:
            return
        op.deps[d] = True
        d.signal = True

    def add(self, eng, fn, reads=(), writes=(), slot=None):
        op = Op()
        op.eng, op.fn, op.dma = eng, fn, slot is not None
        op.key = ("slot", slot.name, eng) if slot is not None else eng
        op.deps, op.signal, op.val = {}, op.dma, 0
        for b in reads:
            if b.w is not None:
                self._dep(op, b.w, True)
        for b in writes:
            if b.w is not None:
                self._dep(op, b.w, False)
            for r in b.r:
                self._dep(op, r, False)
        for b in reads:
            b.r.append(op)
        for b in writes:
            b.w = op
            b.r = []
        self.ops.append(op)
        if op.dma:
            self.dma_out.append(op)
        else:
            self.last[eng] = op
        return op

    def barrier(self):
        a = self.add("sp", lambda e: e.nop())
        for d in self.dma_out:
            a.deps[d] = True
        for eng, o in self.last.items():
            if eng != "sp":
                a.deps[o] = True
                o.signal = True
        a.signal = True
        self.dma_out = []
        for eng in ENGS:
            if eng != "sp":
                o = self.add(eng, lambda e: e.nop())
                o.deps[a] = True
        for b in self.bufs:
            b.w, b.r = None, []

    def emit(self, es):
        nc = self.nc
        cnt = {}
        for op in self.ops:
            if op.signal:
                cnt[op.key] = cnt.get(op.key, 0) + (16 if op.dma else 1)
                op.val = cnt[op.key]
        sems = {}
        for i, k in enumerate(cnt):
            sems[k] = es.enter_context(nc.semaphore("s%d" % i))
        per = {e: [o for o in self.ops if o.eng == e] for e in ENGS}
        block = es.enter_context(nc.Block())

        def body(eng):
            def f(e):
                seen = {}
                for op in per[eng]:
                    need = {}
                    for d in op.deps:
                        if need.get(d.key, 0) < d.val:
                            need[d.key] = d.val
                    for k, v in need.items():
                        if seen.get(k, 0) < v:
                            e.wait_ge(sems[k], v)
                            seen[k] = v
                    ins = op.fn(e)
                    if op.signal:
                        ins.then_inc(sems[op.key], 16 if op.dma else 1)
            return f

        block.sync(body("sp"))
        block.tensor(body("pe"))
        block.scalar(body("act"))
        block.vector(body("dve"))
        block.gpsimd(body("pool"))
        return len(sems)


class T:
    def __init__(self, S, es, nc, name, shape, dt, psum=False):
        self.t = es.enter_context((nc.psum_tensor if psum else nc.sbuf_tensor)("t_" + name, list(shape), dt))
        self.b = S.buf(name.split("_L")[0] if name[-3:-1] == "_L" else name)

    def __getitem__(self, k):
        return self.t[k]


def build(n_layers=2, do_f=True, stages=("W", "M", "PA", "F"), dbg=False, tile_limit=None):
    nc = bass.Bass("TRN2", target_bir_lowering=False)
    S = Sched(nc)

    def din(name, shape, dt=F32):
        return nc.dram_tensor(name, list(shape), dt, kind="ExternalInput").ap()

    def dscr(name, shape, dt=F32):
        return nc.dram_tensor(name, list(shape), dt, kind="ExternalOutput" if dbg else "Internal").ap()

    xw = din("xw", [40 * 128, D])
    ctx0 = din("ctx", [256, D])
    cvec = din("cvec", [128, 16])
    ident_d = din("ident", [128, 128], BF16)
    identf_d = din("identf", [128, 128])
    w_ada = din("w_ada", [2, D, 6 * D])
    b_ada = din("b_ada", [2, 6 * D])
    b_adaT = din("b_adaT", [2, 128, 48])
    FW = {"w_in": 2 * D * 2560 // 128, "w_out": 2 * D * D // 128, "w1": 2 * 16 * D * 512 // 128,
          "w3": 2 * 16 * D * 512 // 128, "w2": 2 * 16 * 512 * D // 128}
    wf = {k: din(k, [128, v]) for k, v in FW.items() if k in ("w_in", "w_out")}
    wb = {k: dscr(k + "_b", [128, v], BF16) for k, v in FW.items() if k in ("w_in", "w_out")}
    wf["w1"] = din("w1", [32, D, 512]); wf["w3"] = din("w3", [32, D, 512]); wf["w2"] = din("w2", [32, 512, D])
    for k_ in ("w1", "w3", "w2"):
        wb[k_] = dscr(k_ + "_b", [32 * 128, 4096], BF16)
    rw = din("rw", [2, D, 20])
    rb = din("rb", [2, 20])
    qg = din("qg", [2, 128, 1])
    kg = din("kg", [2, 128, 1])
    bias = din("bias", [2, 5, 6, 128, 1024])
    sln = din("sln", [2, 512])
    swT = din("swT", [2, 128, 1024])
    sbT = din("sbT", [2, 128, 8])
    ogT = din("ogT", [2, 128, 8])
    out = nc.dram_tensor("out", [32 * 128, D], F32, kind="ExternalOutput").ap()
    x1 = dscr("x1", [40 * 128, D])
    xm = dscr("xm", [40 * 128, D])
    ctx1 = dscr("ctx1", [256, D])
    ctxm = dscr("ctxm", [256, D])
    if dbg:
        dbg_modf = nc.dram_tensor("dbg_modf", [2, 128, 64], F32, kind="ExternalOutput").ap()
        dbg_gates = nc.dram_tensor("dbg_gates", [2, 4, 128, D], F32, kind="ExternalOutput").ap()
    NTMAX = 35
    hfd = dscr("hfd", [40 * 128, D], BF16)
    srt_d = dscr("srt_d", [NTMAX * 512, D], BF16)
    sout = dscr("sout", [NTMAX * 512, D])
    cU_d = din("cU", [128, 128], BF16)
    cOnes_d = din("cOnes", [128, 128], BF16)
    itc_d = din("itc", [128, NTMAX * 16])
    pidx_d = din("pidx", [128, 2])
    fbc_d = dscr("fbc_d", [4, 128, D])
    dbufs = {}

    def dbuf(name):
        if name not in dbufs:
            dbufs[name] = S.buf("dram_" + name)
        return dbufs[name]

    def wview(k, l):
        flat = wb[k].rearrange("p f -> (p f)")
        if k == "w_in":
            return flat.rearrange("(l kc p n) -> l p kc n", l=2, kc=8, p=128, n=2560)[l]
        if k == "w_out":
            return flat.rearrange("(l kc p n) -> l p kc n", l=2, kc=8, p=128, n=1024)[l]
        if k in ("w1", "w3"):
            return flat.rearrange("(l e kc p n) -> l e p kc n", l=2, e=16, kc=8, p=128, n=512)[l]
        return flat.rearrange("(l e kc p n) -> l e p kc n", l=2, e=16, kc=4, p=128, n=1024)[l]

    with ExitStack() as es:
        mk = lambda name, shape, dt, psum=False: T(S, es, nc, name, shape, dt, psum)
        ident = mk("ident", [128, 128], BF16)
        identf = mk("identf", [128, 128], F32)
        modf = mk("modf", [128, 4, 8, 2], F32)
        zc = mk("zc", [128, 4], F32)
        S.add("pool", lambda e: e.memset(zc[:], 0.0), writes=[zc.b])
        epsc = mk("epsc", [128, 2], F32)
        S.add("pool", lambda e: e.memset(epsc[:], EPS), writes=[epsc.b])
        gates = [[mk("gate%d%d" % (s, k), [128, D], F32) for k in range(2)] for s in range(2)]
        S.add("sp", lambda e: e.dma_start(out=ident[:], in_=ident_d[:, :]), writes=[ident.b], slot=ident.b)
        S.add("sp", lambda e: e.dma_start(out=identf[:], in_=identf_d[:, :]), writes=[identf.b], slot=identf.b)

        def rstd_ops(src, dst, tmp, n, pfx=""):
            S.add("act", lambda e: e.activation(out=tmp[0], in_=src[0], func=AF.Ln, bias=epsc[:, 0:1], scale=1.0 / n), reads=[src[1], epsc.b], writes=[tmp[1]])
            S.add("act", lambda e: e.activation(out=dst[0], in_=tmp[0], func=AF.Exp, scale=-0.5), reads=[tmp[1]], writes=[dst[1]])

        with ExitStack() as es2:
          if "W" in stages:
            mk2 = lambda name, shape, dt, psum=False: T(S, es2, nc, name, shape, dt, psum)
            CH = 4096
            st32 = [mk2("st32_%d" % i, [128, CH], F32) for i in range(2)]
            st16 = [mk2("st16_%d" % i, [128, CH], BF16) for i in range(2)]
            i = 0
            jobs = []
            for k in ("w_in", "w_out"):
                for off in range(0, FW[k], CH):
                    n = min(CH, FW[k] - off)
                    jobs.append((n, wf[k][:, off:off + n], wb[k][:, off:off + n], None))
            for n, src, dst, c3 in jobs:
                a, b = st32[i % 2], st16[i % 2]
                if c3 is None:
                    S.add("sp", lambda e, a=a, src=src, n=n: e.dma_start(out=a[:, 0:n], in_=src), writes=[a.b], slot=a.b)
                else:
                    S.add("sp", lambda e, a=a, src=src, c3=c3: e.dma_start(out=a[:].rearrange("p (c n) -> p c n", c=c3), in_=src), writes=[a.b], slot=a.b)
                if i % 2 == 0:
                    S.add("dve", lambda e, a=a, b=b, n=n: e.tensor_copy(out=b[:, 0:n], in_=a[:, 0:n]), reads=[a.b], writes=[b.b])
                else:
                    S.add("act", lambda e, a=a, b=b, n=n: e.activation(out=b[:, 0:n], in_=a[:, 0:n], func=AF.Copy), reads=[a.b], writes=[b.b])
                S.add("pool", lambda e, b=b, dst=dst, n=n: e.dma_start(out=dst, in_=b[:, 0:n]), reads=[b.b], writes=[S.buf('wtmp')], slot=b.b)
                i += 1
            S.barrier()

        def layer(l):
            with ExitStack() as es2:
              if "M" in stages:
                mk2 = lambda name, shape, dt, psum=False: T(S, es2, nc, name + "_L%d" % l, shape, dt, psum)
                cv = mk2("cv", [128, 16], F32)
                sil = mk2("sil", [128, 16], F32)
                silrep = mk2("silrep", [128, 16, 128], F32)
                wa = [mk2("wa%d" % i, [128, 8, 512], F32) for i in range(2)]
                badT = mk2("badT", [128, 48], F32)
                bbc = [mk2("bbc%d" % i, [128, 512], F32) for i in range(2)]
                pg = [mk2("pg%d" % i, [128, 512], F32, True) for i in range(2)]
                pf = [mk2("pf%d" % i, [128, 8], F32, True) for i in range(2)]
                fbc = [[mk2("fbc%d%d" % (s_, k_), [128, D], F32) for k_ in range(2)] for s_ in range(2)]
                S.add("sp", lambda e: e.dma_start(out=cv[:], in_=cvec[:, :]), writes=[cv.b], slot=cv.b)
                S.add("sp", lambda e: e.dma_start(out=badT[:], in_=b_adaT[l]), writes=[badT.b], slot=badT.b)
                S.add("act", lambda e: e.activation(out=sil[:], in_=cv[:], func=AF.Silu), reads=[cv.b], writes=[sil.b])
                S.add("dve", lambda e: e.tensor_copy(out=silrep[:], in_=sil[:].unsqueeze(2).to_broadcast([128, 16, 128])),
                      reads=[sil.b], writes=[silrep.b])
                wav = w_ada[l].rearrange("(kc p) n -> p kc n", p=128)
                for j in range(12):
                    mod, half = j // 2, j % 2
                    w_ = wa[j % 2]
                    S.add("sp", lambda e, w_=w_, j=j: e.dma_start(out=w_[:], in_=wav[:, :, j * 512:(j + 1) * 512]),
                          writes=[w_.b], slot=w_.b)
                    if mod in (2, 3, 4, 5):
                        bb = bbc[(j // 2) % 2]
                        S.add("sp", lambda e, bb=bb, j=j: e.dma_start(out=bb[:], in_=b_ada[l, j * 512:(j + 1) * 512].partition_broadcast(128)),
                              writes=[bb.b], slot=bb.b)
                        for s in range(2):
                            p_ = pg[s]
                            for kc in range(8):
                                S.add("pe", lambda e, p_=p_, w_=w_, kc=kc, s=s: e.matmul(p_[:], lhsT=silrep[:, kc * 2 + s, :], rhs=w_[:, kc, :],
                                                                                      start=(kc == 0), stop=(kc == 7)),
                                      reads=[silrep.b, w_.b], writes=[p_.b])
                            g_ = {2: gates[s][0], 5: gates[s][1], 3: fbc[s][0], 4: fbc[s][1]}[mod]
                            S.add("dve", lambda e, p_=p_, g_=g_, bb=bb, half=half, mod=mod: e.scalar_tensor_tensor(out=g_[:, half * 512:(half + 1) * 512], in0=p_[:], scalar=(1.0 if mod == 4 else 0.0), in1=bb[:], op0=ALU.add, op1=ALU.add),
                                  reads=[p_.b, bb.b], writes=[g_.b])
                    if mod in (0, 1, 3, 4):
                        kind = {0: 0, 1: 1, 3: 2, 4: 3}[mod]
                        p_ = pf[j % 2]
                        for blk in range(4):
                            for kc in range(8):
                                S.add("pe", lambda e, p_=p_, w_=w_, kc=kc, blk=blk: e.matmul(p_[:, blk * 2:blk * 2 + 2], lhsT=w_[:, kc, blk * 128:(blk + 1) * 128],
                                                                                          rhs=sil[:, kc * 2:kc * 2 + 2], start=(kc == 0), stop=(kc == 7)),
                                      reads=[sil.b, w_.b], writes=[p_.b])
                        for blk in range(4):
                            ch = half * 4 + blk
                            S.add("dve", lambda e, p_=p_, blk=blk, ch=ch, kind=kind, mod=mod: e.tensor_scalar(
                                out=modf[:, kind, ch, :], in0=p_[:, blk * 2:blk * 2 + 2], scalar1=badT[:, mod * 8 + ch:mod * 8 + ch + 1],
                                scalar2=(1.0 if kind in (1, 3) else 0.0), op0=ALU.add, op1=ALU.add),
                                reads=[p_.b, badT.b], writes=[modf.b])
                for s_ in range(2):
                    for k_ in range(2):
                        f_ = fbc[s_][k_]
                        S.add("sp", lambda e, f_=f_, s_=s_, k_=k_: e.dma_start(out=fbc_d[s_ * 2 + k_], in_=f_[:]), reads=[f_.b], writes=[S.buf("fbcd")], slot=f_.b)
                S.barrier()

            kv_lo, kv_hi = (0, 40) if l == 0 else (2, 38)
            q_lo, q_hi = (2, 38) if l == 0 else (4, 36)
            if tile_limit is not None:
                kv_hi = min(kv_hi, tile_limit)
                q_hi = min(q_hi, tile_limit - 3)
            xin = xw if l == 0 else x1
            cin = ctx0 if l == 0 else ctx1
            xin_b = dbuf("xin%d" % l) if l == 0 else dbuf("x1")
            cin_b = dbuf("cin%d" % l) if l == 0 else dbuf("ctx1")

            if dbg and "M" in stages:
                S.add("sp", lambda e: e.dma_start(out=dbg_modf[l], in_=modf[:].rearrange("p a b c -> p (a b c)")), reads=[modf.b], writes=[S.buf("dbgm")], slot=modf.b)
                for s_i in range(2):
                    for k_i in range(2):
                        g_ = gates[s_i][k_i]
                        S.add("sp", lambda e, g_=g_, s_i=s_i, k_i=k_i: e.dma_start(out=dbg_gates[l, s_i * 2 + k_i], in_=g_[:]), reads=[g_.b], writes=[S.buf("dbgg")], slot=g_.b)
                S.barrier()
            with ExitStack() as es2:
              if "PA" in stages:
                mk2 = lambda name, shape, dt, psum=False: T(S, es2, nc, name + "_L%d" % l, shape, dt, psum)
                Win = mk2("Win", [128, 8, 2560], BF16)
                Wout = mk2("Wout", [128, 8, 1024], BF16)
                Eint = mk2("Eint", [128, 6, 1024], BF16)
                Esp = mk2("Esp", [128, 6, 1024], BF16)
                bst = [mk2("bst%d" % i, [128, 1024], F32) for i in range(1)]
                g2 = mk2("g2", [128, 4], F32)
                slnb = mk2("slnb", [128, 512], F32)
                swTb = mk2("swTb", [128, 1024], BF16)
                sb = mk2("sb", [128, 8], F32)
                og = mk2("og", [128, 8], F32)
                xt = [mk2("xt%d" % i, [128, D], F32) for i in range(2)]
                junk = mk2("junk", [128, D], BF16)
                xn = [mk2("xn%d" % i, [128, D], BF16) for i in range(2)]
                hT = [mk2("hT%d" % i, [128, 8, 128], BF16) for i in range(2)]
                qk = mk2("qk", [128, D], F32)
                guv = mk2("guv", [128, D], F32)
                sqb = mk2("sqb", [128, D], F32)
                qkn = mk2("qkn", [128, D], BF16)
                st = [mk2("stat%d" % i, [128, 64], F32) for i in range(2)]
                KT = [mk2("KT%d" % i, [128, 4, 128], BF16) for i in range(8)]
                QT = [mk2("QT%d" % i, [128, 4, 128], BF16) for i in range(4)]
                V = [mk2("V%d" % i, [128, 520], BF16) for i in range(8)]
                cKT = [mk2("cKT%d" % i, [128, 4, 128], BF16) for i in range(2)]
                cQT = [mk2("cQT%d" % i, [128, 4, 128], BF16) for i in range(2)]
                cV = [mk2("cV%d" % i, [128, 520], BF16) for i in range(2)]
                vsn = mk2("vsn", [128, 512], BF16)
                t1 = mk2("t1", [128, 512], F32)
                osg = [mk2("osg%d" % i, [128, 512], F32) for i in range(4)]
                cosg = [mk2("cosg%d" % i, [128, 512], F32) for i in range(2)]
                rsg = [mk2("rsg%d" % i, [128, 4], F32) for i in range(4)]
                crsg = [mk2("crsg%d" % i, [128, 4], F32) for i in range(2)]
                pT = [mk2("pT%d" % i, [128, 1024], BF16) for i in range(2)]
                ona = mk2("ona", [128, 512], F32)
                ast = mk2("ast", [128, 16], F32)
                yn = mk2("yn", [128, D], BF16)
                yT = mk2("yT", [128, 8, 128], BF16)
                xa = mk2("xa", [128, D], F32)
                xo = [mk2("xo%d" % i, [128, D], F32) for i in range(1)]
                wjobs = []
                if True:
                    wst32 = [mk2("wst32_%d" % i, [128, 1024], F32) for i in range(2)]
                    wst16 = [mk2("wst16_%d" % i, [128, 1024], BF16) for i in range(1)]
                    for le in range(16 * l, 16 * l + 16):
                        for k_ in ("w1", "w3", "w2"):
                            for q_ in range(4):
                                v_ = wf[k_][le].rearrange("(c p) n -> p c n", p=128)
                                src = v_[:, 2 * q_:2 * q_ + 2, :] if k_ != "w2" else v_[:, q_:q_ + 1, :]
                                wjobs.append((src, wb[k_][le * 128:(le + 1) * 128, q_ * 1024:(q_ + 1) * 1024]))
                wcnt = {"i": 0}

                def _wload(j):
                    src, dst = wjobs[j]
                    a = wst32[j % 2]
                    c3 = src.shape[1]
                    S.add("pool", lambda e, a=a, src=src, c3=c3: e.dma_start(out=a[:].rearrange("p (c n) -> p c n", c=c3), in_=src), writes=[a.b], slot=a.b)

                def emit_wjobs(n):
                    for _ in range(n):
                        j = wcnt["i"]
                        if j >= len(wjobs):
                            return
                        if j == 0:
                            _wload(0)
                        if j + 1 < len(wjobs):
                            _wload(j + 1)
                        src, dst = wjobs[j]
                        a, b = wst32[j % 2], wst16[0]
                        wcnt["i"] += 1
                        S.add("pool", lambda e, a=a, b=b: e.tensor_copy(out=b[:], in_=a[:]), reads=[a.b], writes=[b.b])
                        S.add("pool", lambda e, b=b, dst=dst: e.dma_start(out=dst, in_=b[:]), reads=[b.b], writes=[S.buf("wtmp")], slot=b.b)

                P = [mk2("P%d" % i, [128, 512], F32, True) for i in range(1)]
                T0 = mk2("T0", [128, 1024], BF16, True)
                T1 = mk2("T1", [128, 1024], BF16, True)
                Sb = [mk2("S%d" % i, [128, 512], F32, True) for i in range(4)]
                O0 = mk2("O0", [128, 512], F32, True)

                S.add("sp", lambda e: e.dma_start(out=Win[:], in_=wview("w_in", l)), reads=[dbuf("w_in")], writes=[Win.b], slot=Win.b)
                S.add("sp", lambda e: e.dma_start(out=Wout[:], in_=wview("w_out", l)), reads=[dbuf("w_out")], writes=[Wout.b], slot=Wout.b)

                def load_E(dst, sidx):
                    for o in range(6):
                        b_ = bst[0]
                        S.add("sp", lambda e, b_=b_, o=o: e.dma_start(out=b_[:], in_=bias[l, sidx, o]), writes=[b_.b], slot=b_.b)
                        S.add("act", lambda e, b_=b_, o=o: e.activation(out=dst[:, o, :], in_=b_[:], func=AF.Exp), reads=[b_.b], writes=[dst.b])

                load_E(Eint, 0)
                S.add("sp", lambda e: e.dma_start(out=g2[:, 0:1], in_=qg[l]), writes=[g2.b], slot=g2.b)
                S.add("sp", lambda e: e.dma_start(out=g2[:, 1:2], in_=kg[l]), writes=[g2.b], slot=g2.b)
                S.add("dve", lambda e: e.scalar_tensor_tensor(out=g2[:, 2:3], in0=g2[:, 0:1], scalar=0.125, in1=g2[:, 1:2], op0=ALU.mult, op1=ALU.mult),
                      reads=[g2.b], writes=[g2.b])
                S.add("sp", lambda e: e.dma_start(out=slnb[:], in_=sln[l].partition_broadcast(128)), writes=[slnb.b], slot=slnb.b)
                S.add("sp", lambda e: e.dma_start(out=bst[0][:], in_=swT[l]), writes=[bst[0].b], slot=bst[0].b)
                S.add("dve", lambda e: e.tensor_copy(out=swTb[:], in_=bst[0][:]), reads=[bst[0].b], writes=[swTb.b])
                S.add("sp", lambda e: e.dma_start(out=sb[:], in_=sbT[l]), writes=[sb.b], slot=sb.b)
                S.add("sp", lambda e: e.dma_start(out=og[:], in_=ogT[l]), writes=[og.b], slot=og.b)
                for v_ in V + cV:
                    S.add("pool", lambda e, v_=v_: e.memset(v_[:].rearrange("p (h c) -> p h c", c=65)[:, :, 64:65], 1.0), writes=[v_.b])

                cnt = {"p": 0}

                def proj(src_ap, src_b, stream, kt, vt, qt, osg_t, rsg_t, need_q):
                    i = cnt["p"] % 2
                    cnt["p"] += 1
                    x_, xn_, h_, s_ = xt[i], xn[i], hT[i], st[i]
                    S.add("sp", lambda e: e.dma_start(out=x_[:], in_=src_ap), reads=[src_b], writes=[x_.b], slot=x_.b)
                    S.add("act", lambda e: e.activation(out=junk[:], in_=x_[:], func=AF.Square, accum_out=s_[:, 0:1]), reads=[x_.b], writes=[junk.b, s_.b])
                    rstd_ops((s_[:, 0:1], s_.b), (s_[:, 2:3], s_.b), (s_[:, 1:2], s_.b), D)
                    S.add("dve", lambda e: e.tensor_scalar(out=xn_[:], in0=x_[:], scalar1=s_[:, 2:3], scalar2=None, op0=ALU.mult), reads=[x_.b, s_.b], writes=[xn_.b])
                    yield
                    for kc in range(8):
                        S.add("pe", lambda e, kc=kc: e.transpose(out=T0[:, kc * 128:(kc + 1) * 128], in_=xn_[:, kc * 128:(kc + 1) * 128], identity=ident[:]),
                              reads=[xn_.b, ident.b], writes=[T0.b])
                    yield
                    for kc in range(8):
                        if kc % 2 == 0:
                            S.add("act", lambda e, kc=kc: e.activation(out=h_[:, kc, :], in_=T0[:, kc * 128:(kc + 1) * 128], func=AF.Identity,
                                                                      bias=modf[:, 0, kc, stream:stream + 1], scale=modf[:, 1, kc, stream:stream + 1]),
                                  reads=[T0.b, modf.b], writes=[h_.b])
                        else:
                            S.add("dve", lambda e, kc=kc: e.tensor_scalar(out=h_[:, kc, :], in0=T0[:, kc * 128:(kc + 1) * 128], scalar1=modf[:, 1, kc, stream:stream + 1],
                                                                         scalar2=modf[:, 0, kc, stream:stream + 1], op0=ALU.mult, op1=ALU.add),
                                  reads=[T0.b, modf.b], writes=[h_.b])
                    yield
                    plevel = float(_os.environ.get("DBG_P", "9"))
                    if plevel < 2:
                        return
                    chunks = [0, 1, 2, 3, 4] if need_q else [1, 2]
                    for nch in chunks:
                        yield
                        p_ = P[0]
                        for kc in range(8):
                            S.add("pe", lambda e, p_=p_, kc=kc, nch=nch: e.matmul(p_[:], lhsT=h_[:, kc, :], rhs=Win[:, kc, nch * 512:(nch + 1) * 512], start=(kc == 0), stop=(kc == 7)),
                                  reads=[h_.b, Win.b], writes=[p_.b])
                        if nch < 2:
                            S.add("act", lambda e, p_=p_, nch=nch: e.activation(out=qk[:, nch * 512:(nch + 1) * 512], in_=p_[:], func=AF.Copy), reads=[p_.b], writes=[qk.b])
                        elif nch == 2:
                            S.add("dve", lambda e, p_=p_: e.tensor_copy(out=vt[:].rearrange("p (h c) -> p h c", c=65)[:, :, 0:64], in_=p_[:].rearrange("p (h c) -> p h c", c=64)),
                                  reads=[p_.b], writes=[vt.b])
                        elif nch == 3:
                            S.add("act", lambda e, p_=p_: e.activation(out=guv[:, 0:512], in_=p_[:], func=AF.Gelu_apprx_tanh), reads=[p_.b], writes=[guv.b])
                        else:
                            S.add("act", lambda e, p_=p_: e.activation(out=guv[:, 512:1024], in_=p_[:], func=AF.Gelu_apprx_tanh, accum_out=s_[:, 4:5]), reads=[p_.b], writes=[guv.b, s_.b])
                    yield
                    if plevel < 2.2:
                        return
                    c0, nh = (0, 16) if need_q else (512, 8)
                    S.add("dve", lambda e: e.tensor_tensor(out=sqb[:, c0:1024], in0=qk[:, c0:1024], in1=qk[:, c0:1024], op=ALU.mult), reads=[qk.b], writes=[sqb.b])
                    S.add("dve", lambda e: e.tensor_reduce(out=s_[:, 16:16 + nh], in_=sqb[:, c0:1024].rearrange("p (h c) -> p h c", c=64), axis=AX.X, op=ALU.add),
                          reads=[sqb.b], writes=[s_.b])
                    rstd_ops((s_[:, 16:16 + nh], s_.b), (s_[:, 48:48 + nh], s_.b), (s_[:, 32:32 + nh], s_.b), 64)
                    yield
                    if plevel < 2.5:
                        return
                    S.add("dve", lambda e: e.tensor_tensor(out=qkn[:, c0:1024].rearrange("p (h c) -> p h c", c=64), in0=qk[:, c0:1024].rearrange("p (h c) -> p h c", c=64),
                                                          in1=s_[:, 48:48 + nh].unsqueeze(2).to_broadcast([128, nh, 64]), op=ALU.mult), reads=[qk.b, s_.b], writes=[qkn.b])
                    yield
                    for j in range(0 if need_q else 4, 8):
                        S.add("pe", lambda e, j=j: e.transpose(out=T0[:, j * 128:(j + 1) * 128], in_=qkn[:, j * 128:(j + 1) * 128], identity=ident[:]),
                              reads=[qkn.b, ident.b], writes=[T0.b])
                    yield
                    if plevel < 2.8:
                        return
                    ev = _os.environ.get("DBG_EV", "both")
                    if need_q and ev in ("q", "both"):
                        S.add("act", lambda e: e.activation(out=qt[:].rearrange("p a b -> p (a b)"), in_=T0[:, 0:512], func=AF.Identity, scale=g2[:, 2:3]),
                              reads=[T0.b, g2.b], writes=[qt.b])
                    if ev in ("k", "both"):
                        S.add("act", lambda e: e.activation(out=kt[:].rearrange("p a b -> p (a b)"), in_=T0[:, 512:1024], func=AF.Copy), reads=[T0.b], writes=[kt.b])
                    yield
                    if not need_q or plevel < 4:
                        return
                    S.add("dve", lambda e: e.tensor_scalar(out=s_[:, 5:6], in0=s_[:, 4:5], scalar1=-1.0 / 512, scalar2=None, op0=ALU.mult), reads=[s_.b], writes=[s_.b])
                    S.add("act", lambda e: e.activation(out=junk[:, 0:512], in_=guv[:, 512:1024], func=AF.Square, bias=s_[:, 5:6], accum_out=s_[:, 6:7]),
                          reads=[guv.b, s_.b], writes=[junk.b, s_.b])
                    rstd_ops((s_[:, 6:7], s_.b), (s_[:, 8:9], s_.b), (s_[:, 7:8], s_.b), 512)
                    S.add("dve", lambda e: e.tensor_scalar(out=vsn[:], in0=guv[:, 512:1024], scalar1=s_[:, 5:6], scalar2=s_[:, 8:9], op0=ALU.add, op1=ALU.mult),
                          reads=[guv.b, s_.b], writes=[vsn.b])
                    yield
                    p_ = P[0]
                    for h in range(8):
                        S.add("pe", lambda e, h=h: e.matmul(p_[:, h * 64:(h + 1) * 64], lhsT=swTb[:, h * 128:(h + 1) * 128], rhs=vsn[:, h * 64:(h + 1) * 64], start=True, stop=True),
                              reads=[swTb.b, vsn.b], writes=[p_.b])
                    yield
                    S.add("dve", lambda e: e.tensor_tensor(out=t1[:], in0=p_[:], in1=slnb[:], op=ALU.mult), reads=[p_.b, slnb.b], writes=[t1.b])
                    S.add("dve", lambda e: e.tensor_tensor(out=t1[:].rearrange("p (h c) -> p h c", c=64), in0=t1[:].rearrange("p (h c) -> p h c", c=64),
                                                          in1=sb[:].unsqueeze(2).to_broadcast([128, 8, 64]), op=ALU.add), reads=[t1.b, sb.b], writes=[t1.b])
                    S.add("dve", lambda e: e.tensor_tensor(out=osg_t[:], in0=t1[:], in1=guv[:, 0:512], op=ALU.mult), reads=[t1.b, guv.b], writes=[osg_t.b])
                    yield
                    S.add("act", lambda e: e.activation(out=junk[:, 512:1024], in_=osg_t[:], func=AF.Square, accum_out=rsg_t[:, 0:1]), reads=[osg_t.b], writes=[junk.b, rsg_t.b])
                    rstd_ops((rsg_t[:, 0:1], rsg_t.b), (rsg_t[:, 2:3], rsg_t.b), (rsg_t[:, 1:2], rsg_t.b), 512)

                acnt = {"a": 0}

                def attn(src_ap, src_b, dst_ap, dst_b, stream, qt, keys, Eset, osg_t, rsg_t):
                    nk = len(keys)
                    nw = sum(1 for k_ in keys if k_[2] is not None)
                    def emit_qk(h):
                        hp, po = h // 2, (h % 2) * 64
                        SA, SB2 = Sb[(h % 2) * 2], Sb[(h % 2) * 2 + 1]
                        for i, (kt, vt, eo) in enumerate(keys):
                            bank = SA if i < 4 else SB2
                            col = (i % 4) * 128
                            S.add("pe", lambda e, kt=kt, bank=bank, col=col, po=po, hp=hp: e.matmul(bank[:, col:col + 128], lhsT=kt[po:po + 64, hp, :], rhs=qt[po:po + 64, hp, :], start=True, stop=True),
                                  reads=[kt.b, qt.b], writes=[bank.b])

                    emit_qk(0)
                    for h in range(8):
                        SA, SB2 = Sb[(h % 2) * 2], Sb[(h % 2) * 2 + 1]
                        p_ = pT[h % 2]
                        if h + 1 < 8:
                            emit_qk(h + 1)
                        yield
                        na = min(nk, 4)
                        S.add("act", lambda e, SA=SA, p_=p_, na=na: e.activation(out=p_[:, 0:na * 128], in_=SA[:, 0:na * 128], func=AF.Exp), reads=[SA.b], writes=[p_.b])
                        if nk > 4:
                            S.add("act", lambda e, SB2=SB2, p_=p_: e.activation(out=p_[:, 512:nk * 128], in_=SB2[:, 0:(nk - 4) * 128], func=AF.Exp), reads=[SB2.b], writes=[p_.b])
                        yield
                        if nw > 0:
                            S.add("dve", lambda e, p_=p_, h=h: e.tensor_tensor(out=p_[:, 0:nw * 128].rearrange("p (a b) -> p a b", b=128), in0=p_[:, 0:nw * 128].rearrange("p (a b) -> p a b", b=128),
                                                                              in1=Eset[:, 0:nw, h * 128:(h + 1) * 128], op=ALU.mult), reads=[p_.b, Eset.b], writes=[p_.b])
                        yield
                        oc = (h % 4) * 65
                        for i, (kt, vt, eo) in enumerate(keys):
                            S.add("pe", lambda e, vt=vt, i=i, p_=p_, oc=oc, h=h: e.matmul(O0[:, oc:oc + 65], lhsT=p_[:, i * 128:(i + 1) * 128], rhs=vt[:, h * 65:(h + 1) * 65], start=(i == 0), stop=(i == nk - 1)),
                                  reads=[p_.b, vt.b], writes=[O0.b])
                        yield
                        if h % 4 == 3:
                            hb = h - 3
                            S.add("dve", lambda e, hb=hb: e.reciprocal(out=ast[:, hb:hb + 4], in_=O0[:, 0:260].rearrange("p (h c) -> p h c", c=65)[:, :, 64]), reads=[O0.b], writes=[ast.b])
                            S.add("dve", lambda e, hb=hb: e.tensor_tensor(out=ona[:, hb * 64:(hb + 4) * 64].rearrange("p (h c) -> p h c", c=64), in0=O0[:, 0:260].rearrange("p (h c) -> p h c", c=65)[:, :, 0:64],
                                                                         in1=ast[:, hb:hb + 4].unsqueeze(2).to_broadcast([128, 4, 64]), op=ALU.mult), reads=[O0.b, ast.b], writes=[ona.b])
                    yield
                    S.add("act", lambda e: e.activation(out=junk[:, 0:512], in_=ona[:], func=AF.Square, accum_out=ast[:, 8:9]), reads=[ona.b], writes=[junk.b, ast.b])
                    rstd_ops((ast[:, 8:9], ast.b), (ast[:, 10:11], ast.b), (ast[:, 9:10], ast.b), 512)
                    S.add("dve", lambda e: e.tensor_scalar(out=yn[:, 0:512], in0=ona[:], scalar1=ast[:, 10:11], scalar2=None, op0=ALU.mult), reads=[ona.b, ast.b], writes=[yn.b])
                    S.add("dve", lambda e: e.tensor_scalar(out=yn[:, 512:1024], in0=osg_t[:], scalar1=rsg_t[:, 2:3], scalar2=None, op0=ALU.mult), reads=[osg_t.b, rsg_t.b], writes=[yn.b])
                    yield
                    for kc in range(8):
                        S.add("pe", lambda e, kc=kc: e.transpose(out=T1[:, kc * 128:(kc + 1) * 128], in_=yn[:, kc * 128:(kc + 1) * 128], identity=ident[:]), reads=[yn.b, ident.b], writes=[T1.b])
                    for kc in range(8):
                        if kc % 2 == 0:
                            S.add("act", lambda e, kc=kc: e.activation(out=yT[:, kc, :], in_=T1[:, kc * 128:(kc + 1) * 128], func=AF.Identity, scale=og[:, kc:kc + 1]),
                                  reads=[T1.b, og.b], writes=[yT.b])
                        else:
                            S.add("dve", lambda e, kc=kc: e.tensor_scalar(out=yT[:, kc, :], in0=T1[:, kc * 128:(kc + 1) * 128], scalar1=og[:, kc:kc + 1], scalar2=zc[:, 0:1], op0=ALU.mult, op1=ALU.add),
                                  reads=[T1.b, og.b, zc.b], writes=[yT.b])
                    yield
                    S.add("sp", lambda e: e.dma_start(out=xa[:], in_=src_ap), reads=[src_b], writes=[xa.b], slot=xa.b)
                    xo_ = xo[0]
                    acnt["a"] += 1
                    for half in range(2):
                        yield
                        p_ = Sb[half]
                        for kc in range(8):
                            S.add("pe", lambda e, p_=p_, kc=kc, half=half: e.matmul(p_[:], lhsT=yT[:, kc, :], rhs=Wout[:, kc, half * 512:(half + 1) * 512], start=(kc == 0), stop=(kc == 7)),
                                  reads=[yT.b, Wout.b], writes=[p_.b])
                        S.add("dve", lambda e, p_=p_, half=half: e.tensor_tensor(out=xo_[:, half * 512:(half + 1) * 512], in0=p_[:], in1=gates[stream][0][:, half * 512:(half + 1) * 512], op=ALU.mult),
                              reads=[p_.b, gates[stream][0].b], writes=[xo_.b])
                    S.add("dve", lambda e: e.tensor_tensor(out=xo_[:], in0=xo_[:], in1=xa[:], op=ALU.add), reads=[xo_.b, xa.b], writes=[xo_.b])
                    S.add("pool", lambda e: e.dma_start(out=dst_ap, in_=xo_[:]), reads=[xo_.b], writes=[dst_b], slot=xo_.b)

                pa_mode = _os.environ.get("DBG_PA", "full")
                def run(*gens):
                    gens = list(gens)
                    while gens:
                        for g_ in list(gens):
                            try:
                                next(g_)
                            except StopIteration:
                                gens.remove(g_)

                for ci in range(2 if pa_mode != "setup" else 0):
                    run(proj(cin[ci * 128:(ci + 1) * 128, :], cin_b, 1, cKT[ci], cV[ci], cQT[ci], cosg[ci], crsg[ci], l == 0))
                if l == 0 and pa_mode == "full":
                    for ci in range(2):
                        run(attn(cin[ci * 128:(ci + 1) * 128, :], cin_b, ctxm[ci * 128:(ci + 1) * 128, :], dbuf("ctxm"), 1, cQT[ci],
                                 [(cKT[0], cV[0], None), (cKT[1], cV[1], None)], Eint, cosg[ci], crsg[ci]))

                def do_attn(t):
                    if t in (4, 5, 34, 35):
                        sidx = {4: 1, 5: 2, 34: 3, 35: 4}[t]
                        load_E(Esp, sidx)
                        offs = list(range(-2, 4)) if t in (4, 5) else list(range(-3, 3))
                        Eset = Esp
                    else:
                        offs = list(range(-2, 3))
                        Eset = Eint
                    keys = [(KT[(t + o) % 8], V[(t + o) % 8], i) for i, o in enumerate(offs)]
                    keys += [(cKT[0], cV[0], None), (cKT[1], cV[1], None)]
                    return attn(xin[t * 128:(t + 1) * 128, :], xin_b, xm[t * 128:(t + 1) * 128, :], dbuf("xm"), 0, QT[t % 4], keys, Eset, osg[t % 4], rsg[t % 4])

                if pa_mode == "setup":
                    kv_hi = kv_lo
                if pa_mode != "full":
                    q_hi = q_lo
                for t in range(kv_lo, kv_hi):
                    nq = q_lo <= t < q_hi
                    pg_ = proj(xin[t * 128:(t + 1) * 128, :], xin_b, 0, KT[t % 8], V[t % 8], QT[t % 4], osg[t % 4], rsg[t % 4], nq)
                    ta = t - 3
                    if q_lo <= ta < q_hi:
                        if ta in (4, 5):
                            run(pg_)
                            run(do_attn(ta))
                        else:
                            run(pg_, do_attn(ta))
                    else:
                        run(pg_)
                    emit_wjobs(6)
                emit_wjobs(len(wjobs))
                for t in range(kv_hi - 3, q_hi):
                    if t >= q_lo:
                        run(do_attn(t))
                S.barrier()

            if not do_f:
                return
            with ExitStack() as es2:
              if "F" in stages:
                U32 = mybir.dt.uint32
                mk2 = lambda name, shape, dt, psum=False: T(S, es2, nc, name + "_L%d" % l, shape, dt, psum)
                TS = 512
                tiles_all = []
                if l == 0:
                    for ci in range(2):
                        tiles_all.append((ctxm[ci * 128:(ci + 1) * 128, :], dbuf("ctxm"), ctx1[ci * 128:(ci + 1) * 128, :], dbuf("ctx1"), 1, 38 + ci))
                for t in range(q_lo, q_hi):
                    if l == n_layers - 1 and l == 1:
                        dap, db_ = out[(t - 4) * 128:(t - 3) * 128, :], dbuf("out")
                    else:
                        dap, db_ = x1[t * 128:(t + 1) * 128, :], dbuf("x1")
                    tiles_all.append((xm[t * 128:(t + 1) * 128, :], dbuf("xm"), dap, db_, 0, t if t < 38 else t))
                NTOK = len(tiles_all)
                NT = (2 * NTOK * 128 + TS - 1) // TS + 16
                assert NT <= NTMAX
                ew1 = [mk2("ew1_%d" % i, [128, 4096], BF16) for i in range(2)]
                ew3 = [mk2("ew3_%d" % i, [128, 4096], BF16) for i in range(2)]
                ew2 = [mk2("ew2_%d" % i, [128, 4096], BF16) for i in range(2)]
                xg = mk2("xg", [128, 4, D], F32)
                junkf = mk2("junkf", [128, D], BF16)
                xn32 = mk2("xn32", [128, D], F32)
                hf32 = mk2("hf32", [128, 8, 128], F32)
                hs = [mk2("hs%d" % i, [128, D], BF16) for i in range(2)]
                rwt = mk2("rwt", [128, 8, 20], F32)
                rbb = mk2("rbb", [128, 20], F32)
                lg = mk2("lg", [128, 4, 20], F32)
                r = {n_: mk2("r_" + n_, [128, 4, 16], F32) for n_ in ("a", "b", "c", "d", "e", "f", "g", "h", "i", "j", "k")}
                fs = mk2("fs", [128, 16], F32)
                OH = [mk2("OH%d" % i, [128, NTOK, 16], F32) for i in range(2)]
                POS = mk2("POS", [128, NTOK, 16], F32)
                WAB = [mk2("WAB%d" % i, [128, NTOK], F32) for i in range(2)]
                base = mk2("base", [128, 16], F32)
                sel = mk2("sel", [128, 4, 16], BF16)
                cU = mk2("cU", [128, 128], BF16)
                cOnes = mk2("cOnes", [128, 128], BF16)
                itc = mk2("itc", [128, NTMAX, 16], F32)
                cmpt = mk2("cmpt", [128, NTMAX, 16], F32)
                pidx = mk2("pidx", [128, 2], F32)
                offs = mk2("offs", [128, 4, 16], F32)
                eid = mk2("eid", [128, NTMAX], F32)
                widx = mk2("widx", [128, NTMAX], U32)
                slf = [mk2("slf%d" % i, [128, NTOK], F32) for i in range(2)]
                slu = [mk2("slu%d" % i, [128, NTOK], U32) for i in range(2)]
                ptmp = mk2("ptmp", [128, NTOK, 16], F32)
                srt = [mk2("srt%d" % i, [128, 4, D], BF16) for i in range(2)]
                hfT = mk2("hfT", [128, 8, 512], BF16)
                sl = [mk2("sl%d" % i, [128, 512], BF16) for i in range(2)]
                hidT = [mk2("hidT%d" % i, [128, 4, 512], BF16) for i in range(2)]
                ob = [mk2("ob%d" % i, [128, D], F32) for i in range(2)]
                ra = mk2("ra", [128, D], F32)
                rb_ = mk2("rb", [128, D], F32)
                xr = mk2("xr", [128, D], F32)
                xo = [mk2("fxo%d" % i, [128, D], F32) for i in range(1)]
                H1 = [mk2("H1_%d" % i, [128, 512], F32, True) for i in range(2)]
                H3 = [mk2("H3_%d" % i, [128, 512], F32, True) for i in range(2)]
                OUT = [mk2("OUT%d" % i, [128, 512], F32, True) for i in range(2)]
                TB = mk2("TB", [128, 1024], BF16, True)
                TF = mk2("TF", [128, 512], F32, True)
                S.add("sp", lambda e: e.dma_start(out=rwt[:], in_=rw[l].rearrange("(kc p) n -> p kc n", p=128)), writes=[rwt.b], slot=rwt.b)
                S.add("sp", lambda e: e.dma_start(out=rbb[:], in_=rb[l].partition_broadcast(128)), writes=[rbb.b], slot=rbb.b)
                fbc = [[mk2("ffbc%d%d" % (s_, k_), [128, D], F32) for k_ in range(2)] for s_ in range(2)]
                for s_ in range(2):
                    for k_ in range(2):
                        f_ = fbc[s_][k_]
                        S.add("sp", lambda e, f_=f_, s_=s_, k_=k_: e.dma_start(out=f_[:], in_=fbc_d[s_ * 2 + k_]), writes=[f_.b], slot=f_.b)
                S.add("sp", lambda e: e.dma_start(out=cU[:], in_=cU_d[:, :]), writes=[cU.b], slot=cU.b)
                S.add("sp", lambda e: e.dma_start(out=cOnes[:], in_=cOnes_d[:, :]), writes=[cOnes.b], slot=cOnes.b)
                S.add("sp", lambda e: e.dma_start(out=itc[:].rearrange("p a b -> p (a b)"), in_=itc_d[:, :]), writes=[itc.b], slot=itc.b)
                S.add("sp", lambda e: e.dma_start(out=pidx[:], in_=pidx_d[:, :]), writes=[pidx.b], slot=pidx.b)
                S.add("pool", lambda e: e.memset(base[:], 0.0), writes=[base.b])
                S.add("pool", lambda e: e.memset(hfT[:], 0.0), writes=[hfT.b])
                srt_flat = srt_d.rearrange("r n -> (r n)").rearrange("(p f) -> p f", p=128)
                for i in range(NT):
                    S.add("sp", lambda e, i=i: e.dma_start(out=srt_d[i * 512:(i + 1) * 512, :].rearrange("(p a) n -> p (a n)", p=128), in_=hfT[:].rearrange("p a b -> p (a b)")),
                          reads=[hfT.b], writes=[dbuf("srt") if i == NT - 1 else S.buf("zf")], slot=hfT.b)

                def dv(fn, reads, writes):
                    S.add("dve", fn, reads=[x_.b for x_ in reads], writes=[x_.b for x_ in writes])

                hcnt = {"h": 0}

                def route_group(idx0, G):
                    for ti in range(G):
                        sap, sbuf_, dap, dbuf_, stream, hrow = tiles_all[idx0 + ti]
                        S.add("sp", lambda e, ti=ti, sap=sap: e.dma_start(out=xg[:, ti, :], in_=sap), reads=[sbuf_], writes=[xg.b], slot=xg.b)
                    for ti in range(G):
                        sap, sbuf_, dap, dbuf_, stream, hrow = tiles_all[idx0 + ti]
                        S.add("act", lambda e, ti=ti: e.activation(out=junkf[:], in_=xg[:, ti, :], func=AF.Square, accum_out=fs[:, 0:1]), reads=[xg.b], writes=[junkf.b, fs.b])
                        rstd_ops((fs[:, 0:1], fs.b), (fs[:, 2:3], fs.b), (fs[:, 1:2], fs.b), D)
                        S.add("dve", lambda e, ti=ti: e.tensor_scalar(out=xn32[:], in0=xg[:, ti, :], scalar1=fs[:, 2:3], scalar2=None, op0=ALU.mult), reads=[xg.b, fs.b], writes=[xn32.b])
                        h_ = hs[hcnt["h"] % 2]
                        hcnt["h"] += 1
                        S.add("dve", lambda e, stream=stream: e.tensor_tensor(out=ra[:], in0=xn32[:], in1=fbc[stream][1][:], op=ALU.mult), reads=[xn32.b, fbc[stream][1].b], writes=[ra.b])
                        S.add("dve", lambda e, stream=stream, h_=h_: e.tensor_tensor(out=h_[:], in0=ra[:], in1=fbc[stream][0][:], op=ALU.add), reads=[ra.b, fbc[stream][0].b], writes=[h_.b])
                        S.add("pool", lambda e, h_=h_, hrow=hrow: e.dma_start(out=hfd[hrow * 128:(hrow + 1) * 128, :], in_=h_[:]), reads=[h_.b], writes=[dbuf("hfd")], slot=h_.b)
                        for half in range(2):
                            for kc in range(4):
                                S.add("pe", lambda e, kc=kc, half=half: e.transpose(out=TF[:, kc * 128:(kc + 1) * 128], in_=xn32[:, (half * 4 + kc) * 128:(half * 4 + kc + 1) * 128], identity=identf[:]),
                                      reads=[xn32.b, identf.b], writes=[TF.b])
                            for kc in range(4):
                                k8 = half * 4 + kc
                                S.add("dve", lambda e, kc=kc, k8=k8, stream=stream: e.tensor_scalar(out=hf32[:, k8, :], in0=TF[:, kc * 128:(kc + 1) * 128], scalar1=modf[:, 3, k8, stream:stream + 1],
                                                                                                 scalar2=modf[:, 2, k8, stream:stream + 1], op0=ALU.mult, op1=ALU.add), reads=[TF.b, modf.b], writes=[hf32.b])
                        for kc in range(8):
                            S.add("pe", lambda e, kc=kc, ti=ti: e.matmul(H1[0][:, ti * 20:(ti + 1) * 20], lhsT=hf32[:, kc, :], rhs=rwt[:, kc, :], start=(kc == 0), stop=(kc == 7)),
                                  reads=[hf32.b, rwt.b], writes=[H1[0].b])
                        S.add("dve", lambda e, ti=ti: e.tensor_tensor(out=lg[:, ti, :], in0=H1[0][:, ti * 20:(ti + 1) * 20], in1=rbb[:], op=ALU.add), reads=[H1[0].b, rbb.b], writes=[lg.b])
                    lgG = lg[:, 0:G, 0:4]
                    lgE = lg[:, 0:G, 4:20]
                    mg, ohg, dg, eg, sg, pt = r["a"], r["b"], r["c"], r["d"], r["e"], r["f"]
                    dv(lambda e: e.tensor_reduce(out=mg[:, 0:G, 0], in_=lgG, axis=AX.X, op=ALU.max), [lg], [mg])
                    dv(lambda e: e.tensor_tensor(out=ohg[:, 0:G, 0:4], in0=lgG, in1=mg[:, 0:G, 0:1].to_broadcast([128, G, 4]), op=ALU.is_equal), [lg, mg], [ohg])
                    dv(lambda e: e.tensor_tensor(out=dg[:, 0:G, 0:4], in0=lgG, in1=mg[:, 0:G, 0:1].to_broadcast([128, G, 4]), op=ALU.subtract), [lg, mg], [dg])
                    S.add("act", lambda e: e.activation(out=eg[:, 0:G, 0:4], in_=dg[:, 0:G, 0:4], func=AF.Exp), reads=[dg.b], writes=[eg.b])
                    dv(lambda e: e.tensor_reduce(out=sg[:, 0:G, 0], in_=eg[:, 0:G, 0:4], axis=AX.X, op=ALU.add), [eg], [sg])
                    dv(lambda e: e.reciprocal(out=pt[:, 0:G, 0], in_=sg[:, 0:G, 0]), [sg], [pt])
                    tmp, el = r["g"], r["h"]
                    dv(lambda e: e.tensor_tensor(out=tmp[:, 0:G, :].rearrange("p t (g j) -> p t g j", j=4), in0=lgE.rearrange("p t (g j) -> p t g j", j=4),
                                                 in1=ohg[:, 0:G, 0:4].unsqueeze(3).to_broadcast([128, G, 4, 4]), op=ALU.mult), [lg, ohg], [tmp])
                    dv(lambda e: e.tensor_reduce(out=el[:, 0:G, 0:4], in_=tmp[:, 0:G, :].rearrange("p t (g j) -> p t j g", j=4), axis=AX.X, op=ALU.add), [tmp], [el])
                    m1, oh1, el2, m2, oh2 = r["i"], r["j"], r["k"], r["c"], r["d"]
                    dv(lambda e: e.tensor_reduce(out=m1[:, 0:G, 0], in_=el[:, 0:G, 0:4], axis=AX.X, op=ALU.max), [el], [m1])
                    dv(lambda e: e.tensor_tensor(out=oh1[:, 0:G, 0:4], in0=el[:, 0:G, 0:4], in1=m1[:, 0:G, 0:1].to_broadcast([128, G, 4]), op=ALU.is_equal), [el, m1], [oh1])
                    dv(lambda e: e.scalar_tensor_tensor(out=el2[:, 0:G, 0:4], in0=oh1[:, 0:G, 0:4], scalar=-1e30, in1=el[:, 0:G, 0:4], op0=ALU.mult, op1=ALU.add), [oh1, el], [el2])
                    dv(lambda e: e.tensor_reduce(out=m2[:, 0:G, 0], in_=el2[:, 0:G, 0:4], axis=AX.X, op=ALU.max), [el2], [m2])
                    dv(lambda e: e.tensor_tensor(out=oh2[:, 0:G, 0:4], in0=el2[:, 0:G, 0:4], in1=m2[:, 0:G, 0:1].to_broadcast([128, G, 4]), op=ALU.is_equal), [el2, m2], [oh2])
                    dd, ee, w1_, w2_ = r["a"], r["e"], r["g"], r["h"]
                    dv(lambda e: e.tensor_tensor(out=dd[:, 0:G, 0], in0=m2[:, 0:G, 0], in1=m1[:, 0:G, 0], op=ALU.subtract), [m2, m1], [dd])
                    S.add("act", lambda e: e.activation(out=ee[:, 0:G, 0], in_=dd[:, 0:G, 0], func=AF.Exp), reads=[dd.b], writes=[ee.b])
                    dv(lambda e: e.tensor_scalar(out=dd[:, 0:G, 1], in0=ee[:, 0:G, 0], scalar1=1.0, scalar2=None, op0=ALU.add), [ee], [dd])
                    dv(lambda e: e.reciprocal(out=dd[:, 0:G, 2], in_=dd[:, 0:G, 1]), [dd], [dd])
                    dv(lambda e: e.tensor_tensor(out=w1_[:, 0:G, 0], in0=dd[:, 0:G, 2], in1=pt[:, 0:G, 0], op=ALU.mult), [dd, pt], [w1_])
                    dv(lambda e: e.tensor_tensor(out=w2_[:, 0:G, 0], in0=w1_[:, 0:G, 0], in1=ee[:, 0:G, 0], op=ALU.mult), [w1_, ee], [w2_])
                    wa_, wb_ = r["i"], r["k"]
                    dv(lambda e: e.tensor_tensor(out=wa_[:, 0:G, 0:4], in0=oh1[:, 0:G, 0:4], in1=w1_[:, 0:G, 0:1].to_broadcast([128, G, 4]), op=ALU.mult), [oh1, w1_], [wa_])
                    dv(lambda e: e.tensor_tensor(out=wb_[:, 0:G, 0:4], in0=oh2[:, 0:G, 0:4], in1=w2_[:, 0:G, 0:1].to_broadcast([128, G, 4]), op=ALU.mult), [oh2, w2_], [wb_])
                    dv(lambda e: e.tensor_tensor(out=wa_[:, 0:G, 0:4], in0=wa_[:, 0:G, 0:4], in1=wb_[:, 0:G, 0:4], op=ALU.add), [wa_, wb_], [wa_])
                    for k_, oh_ in enumerate((oh1, oh2)):
                        dv(lambda e, k_=k_, oh_=oh_: e.tensor_tensor(out=OH[k_][:, idx0:idx0 + G, :].rearrange("p t (g j) -> p t g j", j=4), in0=ohg[:, 0:G, 0:4].unsqueeze(3).to_broadcast([128, G, 4, 4]),
                                                                    in1=oh_[:, 0:G, 0:4].unsqueeze(2).to_broadcast([128, G, 4, 4]), op=ALU.mult), [ohg, oh_], [OH[k_]])
                    dv(lambda e: e.tensor_copy(out=WAB[0][:, idx0:idx0 + G], in_=w1_[:, 0:G, 0]), [w1_], [WAB[0]])
                    dv(lambda e: e.tensor_copy(out=WAB[1][:, idx0:idx0 + G], in_=w2_[:, 0:G, 0]), [w2_], [WAB[1]])
                    dv(lambda e: e.tensor_tensor(out=sel[:, 0:G, :], in0=OH[0][:, idx0:idx0 + G, :], in1=OH[1][:, idx0:idx0 + G, :], op=ALU.add), [OH[0], OH[1]], [sel])
                    for ti in range(G):
                        S.add("pe", lambda e, ti=ti: e.matmul(H3[0][:, ti * 32:ti * 32 + 16], lhsT=cU[:], rhs=sel[:, ti, :], start=True, stop=True), reads=[cU.b, sel.b], writes=[H3[0].b])
                        S.add("pe", lambda e, ti=ti: e.matmul(H3[0][:, ti * 32 + 16:ti * 32 + 32], lhsT=cOnes[:], rhs=sel[:, ti, :], start=True, stop=True), reads=[cOnes.b, sel.b], writes=[H3[0].b])
                        dv(lambda e, ti=ti: e.tensor_tensor(out=POS[:, idx0 + ti, :], in0=H3[0][:, ti * 32:ti * 32 + 16], in1=base[:], op=ALU.add), [H3[0], base], [POS])
                        dv(lambda e, ti=ti: e.tensor_tensor(out=base[:], in0=H3[0][:, ti * 32 + 16:ti * 32 + 32], in1=base[:], op=ALU.add), [H3[0], base], [base])

                for g0 in range(0, NTOK, 4):
                    route_group(g0, min(4, NTOK - g0))
                o_tmp, o_pad, o_off, o_end = offs[:, 0, :], offs[:, 1, :], offs[:, 2, :], offs[:, 3, :]
                dv(lambda e: e.tensor_tensor(out=cmpt[:, 0:19, :].rearrange("p j e -> p e j"), in0=itc[:, 0:19, :].rearrange("p j e -> p e j"),
                                             in1=base[:].unsqueeze(2).to_broadcast([128, 16, 19]), op=ALU.is_lt), [itc, base], [cmpt])
                dv(lambda e: e.tensor_reduce(out=o_tmp, in_=cmpt[:, 0:19, :].rearrange("p j e -> p e j"), axis=AX.X, op=ALU.add), [cmpt], [offs])
                dv(lambda e: e.tensor_scalar(out=o_pad, in0=o_tmp, scalar1=float(TS), scalar2=None, op0=ALU.mult), [offs], [offs])
                S.add("pool", lambda e: e.memset(offs[:, 2, 0:1], 0.0), reads=[offs.b], writes=[offs.b])
                for ex in range(1, 16):
                    dv(lambda e, ex=ex: e.tensor_tensor(out=offs[:, 2, ex:ex + 1], in0=offs[:, 2, ex - 1:ex], in1=offs[:, 1, ex - 1:ex], op=ALU.add), [offs], [offs])
                dv(lambda e: e.tensor_tensor(out=o_end, in0=o_off, in1=o_pad, op=ALU.add), [offs], [offs])
                dv(lambda e: e.tensor_tensor(out=cmpt[:, 0:NT, :], in0=itc[:, 0:NT, :], in1=offs[:, 3:4, :].to_broadcast([128, NT, 16]), op=ALU.is_ge), [itc, offs], [cmpt])
                dv(lambda e: e.tensor_reduce(out=eid[:, 0:NT], in_=cmpt[:, 0:NT, :], axis=AX.X, op=ALU.add), [cmpt], [eid])
                dv(lambda e: e.tensor_scalar(out=eid[:, 0:NT], in0=eid[:, 0:NT], scalar1=15.0, scalar2=128.0, op0=ALU.min, op1=ALU.mult), [eid], [eid])
                dv(lambda e: e.tensor_scalar(out=eid[:, 0:NT], in0=eid[:, 0:NT], scalar1=pidx[:, l:l + 1], scalar2=None, op0=ALU.add), [eid, pidx], [eid])
                dv(lambda e: e.tensor_copy(out=widx[:, 0:NT], in_=eid[:, 0:NT]), [eid], [widx])
                dv(lambda e: e.tensor_tensor(out=POS[:], in0=POS[:], in1=offs[:, 2:3, :].to_broadcast([128, NTOK, 16]), op=ALU.add), [POS, offs], [POS])
                for k_ in range(2):
                    dv(lambda e, k_=k_: e.tensor_tensor(out=ptmp[:], in0=POS[:], in1=OH[k_][:], op=ALU.mult), [POS, OH[k_]], [ptmp])
                    dv(lambda e, k_=k_: e.tensor_reduce(out=slf[k_][:], in_=ptmp[:], axis=AX.X, op=ALU.add), [ptmp], [slf[k_]])
                    dv(lambda e, k_=k_: e.tensor_copy(out=slu[k_][:], in_=slf[k_][:]), [slf[k_]], [slu[k_]])
                for ti in range(NTOK):
                    hrow = tiles_all[ti][5]
                    h_ = hs[ti % 2]
                    S.add("sp", lambda e, h_=h_, hrow=hrow: e.dma_start(out=h_[:], in_=hfd[hrow * 128:(hrow + 1) * 128, :]), reads=[dbuf("hfd")], writes=[h_.b], slot=h_.b)
                    for k_ in range(2):
                        S.add("pool", lambda e, h_=h_, k_=k_, ti=ti: e.indirect_dma_start(out=srt_d[0:NT * 512, :], out_offset=bass.IndirectOffsetOnAxis(ap=slu[k_][:, ti:ti + 1], axis=0), in_=h_[:], in_offset=None),
                              reads=[h_.b, slu[k_].b], writes=[dbuf("srt")], slot=h_.b)
                for i in range(NT):
                    a1, a3, a2 = ew1[i % 2], ew3[i % 2], ew2[i % 2]
                    for a_, k_ in ((a1, "w1"), (a3, "w3"), (a2, "w2")):
                        S.add("pool", lambda e, a_=a_, k_=k_, i=i: e.indirect_dma_start(out=a_[:], out_offset=None, in_=wb[k_][:, :], in_offset=bass.IndirectOffsetOnAxis(ap=widx[:, i:i + 1], axis=0)),
                              reads=[widx.b], writes=[a_.b], slot=a_.b)
                    sr = srt[i % 2]
                    for ii in ([0, 1] if i == 0 else [i + 1]):
                        if ii < NT:
                            sr2 = srt[ii % 2]
                            S.add("sp", lambda e, sr2=sr2, ii=ii: e.dma_start(out=sr2[:], in_=srt_d[ii * 512:(ii + 1) * 512, :].rearrange("(a p) n -> p a n", p=128)), reads=[dbuf("srt")], writes=[sr2.b], slot=sr2.b)
                    for a in range(4):
                        for kc in range(8):
                            S.add("pe", lambda e, sr=sr, a=a, kc=kc: e.transpose(out=TB[:, kc * 128:(kc + 1) * 128], in_=sr[:, a, kc * 128:(kc + 1) * 128], identity=ident[:]), reads=[sr.b, ident.b], writes=[TB.b])
                        S.add("act", lambda e, a=a: e.activation(out=hfT[:, :, a * 128:(a + 1) * 128], in_=TB[:].rearrange("p (c t) -> p c t", t=128), func=AF.Copy), reads=[TB.b], writes=[hfT.b])
                    hd = hidT[i % 2]
                    a1v = a1[:].rearrange("p (c n) -> p c n", c=8)
                    a3v = a3[:].rearrange("p (c n) -> p c n", c=8)
                    a2v = a2[:].rearrange("p (c n) -> p c n", c=4)
                    for hc in range(4):
                        h1, h3, s_ = H1[hc % 2], H3[hc % 2], sl[hc % 2]
                        for kc in range(8):
                            S.add("pe", lambda e, h1=h1, a1v=a1v, kc=kc, hc=hc: e.matmul(h1[:], lhsT=a1v[:, kc, hc * 128:(hc + 1) * 128], rhs=hfT[:, kc, :], start=(kc == 0), stop=(kc == 7)),
                                  reads=[a1.b, hfT.b], writes=[h1.b])
                        for kc in range(8):
                            S.add("pe", lambda e, h3=h3, a3v=a3v, kc=kc, hc=hc: e.matmul(h3[:], lhsT=a3v[:, kc, hc * 128:(hc + 1) * 128], rhs=hfT[:, kc, :], start=(kc == 0), stop=(kc == 7)),
                                  reads=[a3.b, hfT.b], writes=[h3.b])
                        S.add("act", lambda e, h1=h1, s_=s_: e.activation(out=s_[:], in_=h1[:], func=AF.Silu), reads=[h1.b], writes=[s_.b])
                        S.add("dve", lambda e, h3=h3, s_=s_, hd=hd, hc=hc: e.tensor_tensor(out=hd[:, hc, :], in0=s_[:], in1=h3[:], op=ALU.mult), reads=[s_.b, h3.b], writes=[hd.b])
                    for ti in range(4):
                        o_b = ob[ti % 2]
                        for half in range(2):
                            o_ = OUT[half]
                            for hc in range(4):
                                S.add("pe", lambda e, o_=o_, hd=hd, a2v=a2v, hc=hc, ti=ti, half=half: e.matmul(o_[:], lhsT=hd[:, hc, ti * 128:(ti + 1) * 128], rhs=a2v[:, hc, half * 512:(half + 1) * 512],
                                                                                                        start=(hc == 0), stop=(hc == 3)), reads=[hd.b, a2.b], writes=[o_.b])
                            if half == 0:
                                S.add("act", lambda e, o_=o_, o_b=o_b: e.activation(out=o_b[:, 0:512], in_=o_[:], func=AF.Copy), reads=[o_.b], writes=[o_b.b])
                            else:
                                S.add("dve", lambda e, o_=o_, o_b=o_b: e.tensor_copy(out=o_b[:, 512:1024], in_=o_[:]), reads=[o_.b], writes=[o_b.b])
                        S.add("sp", lambda e, o_b=o_b, i=i, ti=ti: e.dma_start(out=sout[i * 512 + ti * 128:i * 512 + (ti + 1) * 128, :], in_=o_b[:]), reads=[o_b.b], writes=[(dbuf("sout") if ti == 3 else dbuf("sout2")) if i == NT - 1 and ti >= 2 else S.buf("so")], slot=o_b.b)
                for ti in range(NTOK):
                    sap, sbuf_, dap, dbuf_, stream, hrow = tiles_all[ti]
                    S.add("pool", lambda e, ti=ti: e.indirect_dma_start(out=ra[:], out_offset=None, in_=sout[0:NT * 512, :], in_offset=bass.IndirectOffsetOnAxis(ap=slu[0][:, ti:ti + 1], axis=0)),
                          reads=[dbuf("sout"), dbuf("sout2"), slu[0].b], writes=[ra.b], slot=ra.b)
                    S.add("pool", lambda e, ti=ti: e.indirect_dma_start(out=rb_[:], out_offset=None, in_=sout[0:NT * 512, :], in_offset=bass.IndirectOffsetOnAxis(ap=slu[1][:, ti:ti + 1], axis=0)),
                          reads=[dbuf("sout"), dbuf("sout2"), slu[1].b], writes=[rb_.b], slot=rb_.b)
                    S.add("sp", lambda e, sap=sap: e.dma_start(out=xr[:], in_=sap), reads=[sbuf_], writes=[xr.b], slot=xr.b)
                    xo_ = xo[0]
                    dv(lambda e, ti=ti: e.tensor_scalar(out=ra[:], in0=ra[:], scalar1=WAB[0][:, ti:ti + 1], scalar2=None, op0=ALU.mult), [ra, WAB[0]], [ra])
                    dv(lambda e, ti=ti: e.scalar_tensor_tensor(out=ra[:], in0=rb_[:], scalar=WAB[1][:, ti:ti + 1], in1=ra[:], op0=ALU.mult, op1=ALU.add), [rb_, WAB[1], ra], [ra])
                    dv(lambda e, stream=stream: e.tensor_tensor(out=ra[:], in0=ra[:], in1=gates[stream][1][:], op=ALU.mult), [ra, gates[stream][1]], [ra])
                    dv(lambda e, xo_=xo_: e.tensor_tensor(out=xo_[:], in0=ra[:], in1=xr[:], op=ALU.add), [ra, xr], [xo_])
                    S.add("sp", lambda e, dap=dap, xo_=xo_: e.dma_start(out=dap, in_=xo_[:]), reads=[xo_.b], writes=[dbuf_], slot=xo_.b)
                S.barrier()
        for l_ in range(n_layers):
            layer(l_)
        nsem = S.emit(es)
    return nc, nsem, len(S.ops)


def _bias_sets(rpb_l, j):
    sets = [(64, list(range(-2, 4))), (32 * j, list(range(-2, 4))), (32 * j + 1, list(range(-2, 4))),
            (32 * j + 30, list(range(-3, 3))), (32 * j + 31, list(range(-3, 3)))]
    outp = np.full((5, 6, 128, 8, 128), NEG, np.float32)
    idx = np.arange(128)
    qc = idx % 64
    kc = idx % 64
    cs = np.clip(qc - 8, 0, 48)
    for si, (m, offs) in enumerate(sets):
        qr = 2 * m + idx // 64
        rs = np.clip(qr - 4, 0, 248)
        for oi, o in enumerate(offs):
            kr = 2 * (m + o) + idx // 64
            valid = ((kr[:, None] >= 0) & (kr[:, None] < 256) & (kr[:, None] >= rs[None, :]) & (kr[:, None] < rs[None, :] + 8)
                     & (kc[:, None] >= cs[None, :]) & (kc[:, None] < cs[None, :] + 16))
            dr = np.clip(kr[:, None] - qr[None, :] + 7, 0, 14)
            dc = np.clip(kc[:, None] - qc[None, :] + 15, 0, 30)
            vals = rpb_l[:, dr, dc]
            vals = np.where(valid[None], vals, NEG)
            outp[si, oi] = np.transpose(vals, (1, 0, 2))
    return outp.reshape(5, 6, 128, 1024)


_CACHE = {}


def kernel(x, c, ctx, c_ctx, w_ada, b_ada, w_in, q_gain, k_gain, rpb, sgu_ln, sgu_w, sgu_b, out_gain, w_out,
           rg_w, rg_b, re_w, re_b, w1, w3, w2):
    f = lambda a: np.ascontiguousarray(np.asarray(a, dtype=np.float32))
    x, c, ctx, c_ctx, w_ada, b_ada, w_in, q_gain, k_gain, rpb = map(f, (x, c, ctx, c_ctx, w_ada, b_ada, w_in, q_gain, k_gain, rpb))
    sgu_ln, sgu_w, sgu_b, out_gain, w_out, rg_w, rg_b, re_w, re_b, w1, w3, w2 = map(
        f, (sgu_ln, sgu_w, sgu_b, out_gain, w_out, rg_w, rg_b, re_w, re_b, w1, w3, w2))
    if "nc" not in _CACHE:
        _CACHE["nc"] = build()[0]
    nc = _CACHE["nc"]
    shared = {
        "ident": np.eye(128, dtype=np.float32).astype(ml_dtypes.bfloat16),
        "identf": np.eye(128, dtype=np.float32),
        "w_ada": w_ada, "b_ada": b_ada,
        "b_adaT": np.ascontiguousarray(b_ada.reshape(2, 48, 128).transpose(0, 2, 1)),
        "w_in": w_in.reshape(128, -1), "w_out": w_out.reshape(128, -1),
        "w1": w1.reshape(32, 1024, 512), "w3": w3.reshape(32, 1024, 512), "w2": w2.reshape(32, 512, 1024),
        "cU": np.triu(np.ones((128, 128), np.float32), 1).astype(ml_dtypes.bfloat16),
        "cOnes": np.ones((128, 128), np.float32).astype(ml_dtypes.bfloat16),
        "itc": np.ascontiguousarray(np.broadcast_to((np.arange(35, dtype=np.float32) * 512.0)[None, :, None], (128, 35, 16)).reshape(128, 35 * 16)),
        "pidx": np.ascontiguousarray(np.stack([np.arange(128, dtype=np.float32), np.arange(128, dtype=np.float32) + 2048.0], axis=1)),
        "rw": np.ascontiguousarray(np.concatenate([rg_w, re_w.transpose(0, 2, 1, 3).reshape(2, 1024, 16)], axis=2)),
        "rb": np.ascontiguousarray(np.concatenate([rg_b, re_b.reshape(2, 16)], axis=1)),
        "qg": np.ascontiguousarray(np.tile(q_gain, (1, 2))[:, :, None]),
        "kg": np.ascontiguousarray(np.tile(k_gain, (1, 2))[:, :, None]),
        "sln": sgu_ln,
        "swT": np.ascontiguousarray(sgu_w.transpose(0, 3, 1, 2).reshape(2, 128, 1024)),
        "sbT": np.ascontiguousarray(sgu_b.transpose(0, 2, 1)),
        "ogT": np.ascontiguousarray(out_gain.reshape(2, 8, 128).transpose(0, 2, 1)),
    }
    xpad = np.zeros((2, 272, 64, D), np.float32)
    xpad[:, 8:264] = x.reshape(2, 256, 64, D)
    in_maps = []
    for core in range(8):
        b, j = core // 4, core % 4
        m = dict(shared)
        m["xw"] = np.ascontiguousarray(xpad[b, 64 * j:64 * j + 80].reshape(5120, D))
        m["ctx"] = ctx[b]
        cv = np.stack([c[b].reshape(8, 128).T, c_ctx.reshape(8, 128).T], axis=2)
        m["cvec"] = np.ascontiguousarray(cv.reshape(128, 16))
        m["bias"] = np.stack([_bias_sets(rpb[0], j), _bias_sets(rpb[1], j)], axis=0)
        in_maps.append(m)
    res = run_bass_kernel_spmd(nc, in_maps, core_ids=list(range(8)))
    outp = np.empty((2, 16384, D), np.float32)
    for core in range(8):
        b, j = core // 4, core % 4
        outp[b, 4096 * j:4096 * (j + 1)] = res.results[core]["out"]
    return outp
```

```python
import os as _os
import numpy as np
import ml_dtypes
from contextlib import ExitStack
import concourse.bass as bass
import concourse.mybir as mybir
from concourse.bass_utils import run_bass_kernel_spmd

F32, BF16 = mybir.dt.float32, mybir.dt.bfloat16
AF = mybir.ActivationFunctionType
ALU = mybir.AluOpType
AX = mybir.AxisListType

D = 1024
NEG = -30000.0
EPS = 1e-6
ENGS = ("sp", "pe", "act", "dve", "pool")


class Buf:
    __slots__ = ("name", "w", "r")

    def __init__(self, name):
        self.name, self.w, self.r = name, None, []


class Op:
    __slots__ = ("eng", "fn", "dma", "key", "deps", "signal", "val")


class Sched:
    def __init__(self, nc):
        self.nc = nc
        self.ops = []
        self.bufs = []
        self.dma_out = []
        self.last = {}

    def buf(self, name):
        b = Buf(name)
        self.bufs.append(b)
        return b

    def _dep(self, op, d, raw):
        if d is op:
            return
        if (not d.dma) and (not op.dma) and d.eng == op.eng and not raw:
            return
        op.deps[d] = True
        d.signal = True

    def add(self, eng, fn, reads=(), writes=(), slot=None):
        op = Op()
        op.eng, op.fn, op.dma = eng, fn, slot is not None
        op.key = ("slot", slot.name, eng) if slot is not None else eng
        op.deps, op.signal, op.val = {}, op.dma, 0
        for b in reads:
            if b.w is not None:
                self._dep(op, b.w, True)
        for b in writes:
            if b.w is not None:
                self._dep(op, b.w, False)
            for r in b.r:
                self._dep(op, r, False)
        for b in reads:
            b.r.append(op)
        for b in writes:
            b.w = op
            b.r = []
        self.ops.append(op)
        if op.dma:
            self.dma_out.append(op)
        else:
            self.last[eng] = op
        return op

    def barrier(self):
        a = self.add("sp", lambda e: e.nop())
        for d in self.dma_out:
            a.deps[d] = True
        for eng, o in self.last.items():
            if eng != "sp":
                a.deps[o] = True
                o.signal = True
        a.signal = True
        self.dma_out = []
        for eng in ENGS:
            if eng != "sp":
                o = self.add(eng, lambda e: e.nop())
                o.deps[a] = True
        for b in self.bufs:
            b.w, b.r = None, []

    def emit(self, es):
        nc = self.nc
        cnt = {}
        for op in self.ops:
            if op.signal:
                cnt[op.key] = cnt.get(op.key, 0) + (16 if op.dma else 1)
                op.val = cnt[op.key]
        sems = {}
        for i, k in enumerate(cnt):
            sems[k] = es.enter_context(nc.semaphore("s%d" % i))
        per = {e: [o for o in self.ops if o.eng == e] for e in ENGS}
        block = es.enter_context(nc.Block())

        def body(eng):
            def f(e):
                seen = {}
                for op in per[eng]:
                    need = {}
                    for d in op.deps:
                        if need.get(d.key, 0) < d.val:
                            need[d.key] = d.val
                    for k, v in need.items():
                        if seen.get(k, 0) < v:
                            e.wait_ge(sems[k], v)
                            seen[k] = v
                    ins = op.fn(e)
                    if op.signal:
                        ins.then_inc(sems[op.key], 16 if op.dma else 1)
            return f

        block.sync(body("sp"))
        block.tensor(body("pe"))
        block.scalar(body("act"))
        block.vector(body("dve"))
        block.gpsimd(body("pool"))
        return len(sems)


class T:
    def __init__(self, S, es, nc, name, shape, dt, psum=False):
        self.t = es.enter_context((nc.psum_tensor if psum else nc.sbuf_tensor)("t_" + name, list(shape), dt))
        self.b = S.buf(name.split("_L")[0] if name[-3:-1] == "_L" else name)

    def __getitem__(self, k):
        return self.t[k]


def build(n_layers=2, do_f=True, stages=("W", "M", "PA", "F"), dbg=False, tile_limit=None):
    nc = bass.Bass("TRN2", target_bir_lowering=False)
    S = Sched(nc)

    def din(name, shape, dt=F32):
        return nc.dram_tensor(name, list(shape), dt, kind="ExternalInput").ap()

    def dscr(name, shape, dt=F32):
        return nc.dram_tensor(name, list(shape), dt, kind="ExternalOutput" if dbg else "Internal").ap()

    xw = din("xw", [40 * 128, D])
    ctx0 = din("ctx", [256, D])
    cvec = din("cvec", [128, 16])
    ident_d = din("ident", [128, 128], BF16)
    identf_d = din("identf", [128, 128])
    w_ada = din("w_ada", [2, D, 6 * D])
    b_ada = din("b_ada", [2, 6 * D])
    b_adaT = din("b_adaT", [2, 128, 48])
    FW = {"w_in": 2 * D * 2560 // 128, "w_out": 2 * D * D // 128, "w1": 2 * 16 * D * 512 // 128,
          "w3": 2 * 16 * D * 512 // 128, "w2": 2 * 16 * 512 * D // 128}
    wf = {k: din(k, [128, v]) for k, v in FW.items() if k in ("w_in", "w_out")}
    wb = {k: dscr(k + "_b", [128, v], BF16) for k, v in FW.items() if k in ("w_in", "w_out")}
    wf["w1"] = din("w1", [32, D, 512]); wf["w3"] = din("w3", [32, D, 512]); wf["w2"] = din("w2", [32, 512, D])
    for k_ in ("w1", "w3", "w2"):
        wb[k_] = dscr(k_ + "_b", [32 * 128, 4096], BF16)
    rw = din("rw", [2, D, 20])
    rb = din("rb", [2, 20])
    qg = din("qg", [2, 128, 1])
    kg = din("kg", [2, 128, 1])
    bias = din("bias", [2, 5, 6, 128, 1024])
    sln = din("sln", [2, 512])
    swT = din("swT", [2, 128, 1024])
    sbT = din("sbT", [2, 128, 8])
    ogT = din("ogT", [2, 128, 8])
    out = nc.dram_tensor("out", [32 * 128, D], F32, kind="ExternalOutput").ap()
    x1 = dscr("x1", [40 * 128, D])
    xm = dscr("xm", [40 * 128, D])
    ctx1 = dscr("ctx1", [256, D])
    ctxm = dscr("ctxm", [256, D])
    if dbg:
        dbg_modf = nc.dram_tensor("dbg_modf", [2, 128, 64], F32, kind="ExternalOutput").ap()
        dbg_gates = nc.dram_tensor("dbg_gates", [2, 4, 128, D], F32, kind="ExternalOutput").ap()
    NTMAX = 35
    hfd = dscr("hfd", [40 * 128, D], BF16)
    srt_d = dscr("srt_d", [NTMAX * 512, D], BF16)
    sout = dscr("sout", [NTMAX * 512, D])
    cU_d = din("cU", [128, 128], BF16)
    cOnes_d = din("cOnes", [128, 128], BF16)
    itc_d = din("itc", [128, NTMAX * 16])
    pidx_d = din("pidx", [128, 2])
    fbc_d = dscr("fbc_d", [4, 128, D])
    dbufs = {}

    def dbuf(name):
        if name not in dbufs:
            dbufs[name] = S.buf("dram_" + name)
        return dbufs[name]

    def wview(k, l):
        flat = wb[k].rearrange("p f -> (p f)")
        if k == "w_in":
            return flat.rearrange("(l kc p n) -> l p kc n", l=2, kc=8, p=128, n=2560)[l]
        if k == "w_out":
            return flat.rearrange("(l kc p n) -> l p kc n", l=2, kc=8, p=128, n=1024)[l]
        if k in ("w1", "w3"):
            return flat.rearrange("(l e kc p n) -> l e p kc n", l=2, e=16, kc=8, p=128, n=512)[l]
        return flat.rearrange("(l e kc p n) -> l e p kc n", l=2, e=16, kc=4, p=128, n=1024)[l]

    with ExitStack() as es:
        mk = lambda name, shape, dt, psum=False: T(S, es, nc, name, shape, dt, psum)
        ident = mk("ident", [128, 128], BF16)
        identf = mk("identf", [128, 128], F32)
        modf = mk("modf", [128, 4, 8, 2], F32)
        zc = mk("zc", [128, 4], F32)
        S.add("pool", lambda e: e.memset(zc[:], 0.0), writes=[zc.b])
        epsc = mk("epsc", [128, 2], F32)
        S.add("pool", lambda e: e.memset(epsc[:], EPS), writes=[epsc.b])
        gates = [[mk("gate%d%d" % (s, k), [128, D], F32) for k in range(2)] for s in range(2)]
        S.add("sp", lambda e: e.dma_start(out=ident[:], in_=ident_d[:, :]), writes=[ident.b], slot=ident.b)
        S.add("sp", lambda e: e.dma_start(out=identf[:], in_=identf_d[:, :]), writes=[identf.b], slot=identf.b)

        def rstd_ops(src, dst, tmp, n, pfx=""):
            S.add("act", lambda e: e.activation(out=tmp[0], in_=src[0], func=AF.Ln, bias=epsc[:, 0:1], scale=1.0 / n), reads=[src[1], epsc.b], writes=[tmp[1]])
            S.add("act", lambda e: e.activation(out=dst[0], in_=tmp[0], func=AF.Exp, scale=-0.5), reads=[tmp[1]], writes=[dst[1]])

        with ExitStack() as es2:
          if "W" in stages:
            mk2 = lambda name, shape, dt, psum=False: T(S, es2, nc, name, shape, dt, psum)
            CH = 4096
            st32 = [mk2("st32_%d" % i, [128, CH], F32) for i in range(2)]
            st16 = [mk2("st16_%d" % i, [128, CH], BF16) for i in range(2)]
            i = 0
            jobs = []
            for k in ("w_in", "w_out"):
                for off in range(0, FW[k], CH):
                    n = min(CH, FW[k] - off)
                    jobs.append((n, wf[k][:, off:off + n], wb[k][:, off:off + n], None))
            for n, src, dst, c3 in jobs:
                a, b = st32[i % 2], st16[i % 2]
                if c3 is None:
                    S.add("sp", lambda e, a=a, src=src, n=n: e.dma_start(out=a[:, 0:n], in_=src), writes=[a.b], slot=a.b)
                else:
                    S.add("sp", lambda e, a=a, src=src, c3=c3: e.dma_start(out=a[:].rearrange("p (c n) -> p c n", c=c3), in_=src), writes=[a.b], slot=a.b)
                if i % 2 == 0:
                    S.add("dve", lambda e, a=a, b=b, n=n: e.tensor_copy(out=b[:, 0:n], in_=a[:, 0:n]), reads=[a.b], writes=[b.b])
                else:
                    S.add("act", lambda e, a=a, b=b, n=n: e.activation(out=b[:, 0:n], in_=a[:, 0:n], func=AF.Copy), reads=[a.b], writes=[b.b])
                S.add("pool", lambda e, b=b, dst=dst, n=n: e.dma_start(out=dst, in_=b[:, 0:n]), reads=[b.b], writes=[S.buf('wtmp')], slot=b.b)
                i += 1
            S.barrier()

        def layer(l):
            with ExitStack() as es2:
              if "M" in stages:
                mk2 = lambda name, shape, dt, psum=False: T(S, es2, nc, name + "_L%d" % l, shape, dt, psum)
                cv = mk2("cv", [128, 16], F32)
                sil = mk2("sil", [128, 16], F32)
                silrep = mk2("silrep", [128, 16, 128], F32)
                wa = [mk2("wa%d" % i, [128, 8, 512], F32) for i in range(2)]
                badT = mk2("badT", [128, 48], F32)
                bbc = [mk2("bbc%d" % i, [128, 512], F32) for i in range(2)]
                pg = [mk2("pg%d" % i, [128, 512], F32, True) for i in range(2)]
                pf = [mk2("pf%d" % i, [128, 8], F32, True) for i in range(2)]
                fbc = [[mk2("fbc%d%d" % (s_, k_), [128, D], F32) for k_ in range(2)] for s_ in range(2)]
                S.add("sp", lambda e: e.dma_start(out=cv[:], in_=cvec[:, :]), writes=[cv.b], slot=cv.b)
                S.add("sp", lambda e: e.dma_start(out=badT[:], in_=b_adaT[l]), writes=[badT.b], slot=badT.b)
                S.add("act", lambda e: e.activation(out=sil[:], in_=cv[:], func=AF.Silu), reads=[cv.b], writes=[sil.b])
                S.add("dve", lambda e: e.tensor_copy(out=silrep[:], in_=sil[:].unsqueeze(2).to_broadcast([128, 16, 128])),
                      reads=[sil.b], writes=[silrep.b])
                wav = w_ada[l].rearrange("(kc p) n -> p kc n", p=128)
                for j in range(12):
                    mod, half = j // 2, j % 2
                    w_ = wa[j % 2]
                    S.add("sp", lambda e, w_=w_, j=j: e.dma_start(out=w_[:], in_=wav[:, :, j * 512:(j + 1) * 512]),
                          writes=[w_.b], slot=w_.b)
                    if mod in (2, 3, 4, 5):
                        bb = bbc[(j // 2) % 2]
                        S.add("sp", lambda e, bb=bb, j=j: e.dma_start(out=bb[:], in_=b_ada[l, j * 512:(j + 1) * 512].partition_broadcast(128)),
                              writes=[bb.b], slot=bb.b)
                        for s in range(2):
                            p_ = pg[s]
                            for kc in range(8):
                                S.add("pe", lambda e, p_=p_, w_=w_, kc=kc, s=s: e.matmul(p_[:], lhsT=silrep[:, kc * 2 + s, :], rhs=w_[:, kc, :],
                                                                                      start=(kc == 0), stop=(kc == 7)),
                                      reads=[silrep.b, w_.b], writes=[p_.b])
                            g_ = {2: gates[s][0], 5: gates[s][1], 3: fbc[s][0], 4: fbc[s][1]}[mod]
                            S.add("dve", lambda e, p_=p_, g_=g_, bb=bb, half=half, mod=mod: e.scalar_tensor_tensor(out=g_[:, half * 512:(half + 1) * 512], in0=p_[:], scalar=(1.0 if mod == 4 else 0.0), in1=bb[:], op0=ALU.add, op1=ALU.add),
                                  reads=[p_.b, bb.b], writes=[g_.b])
                    if mod in (0, 1, 3, 4):
                        kind = {0: 0, 1: 1, 3: 2, 4: 3}[mod]
                        p_ = pf[j % 2]
                        for blk in range(4):
                            for kc in range(8):
                                S.add("pe", lambda e, p_=p_, w_=w_, kc=kc, blk=blk: e.matmul(p_[:, blk * 2:blk * 2 + 2], lhsT=w_[:, kc, blk * 128:(blk + 1) * 128],
                                                                                          rhs=sil[:, kc * 2:kc * 2 + 2], start=(kc == 0), stop=(kc == 7)),
                                      reads=[sil.b, w_.b], writes=[p_.b])
                        for blk in range(4):
                            ch = half * 4 + blk
                            S.add("dve", lambda e, p_=p_, blk=blk, ch=ch, kind=kind, mod=mod: e.tensor_scalar(
                                out=modf[:, kind, ch, :], in0=p_[:, blk * 2:blk * 2 + 2], scalar1=badT[:, mod * 8 + ch:mod * 8 + ch + 1],
                                scalar2=(1.0 if kind in (1, 3) else 0.0), op0=ALU.add, op1=ALU.add),
                                reads=[p_.b, badT.b], writes=[modf.b])
                for s_ in range(2):
                    for k_ in range(2):
                        f_ = fbc[s_][k_]
                        S.add("sp", lambda e, f_=f_, s_=s_, k_=k_: e.dma_start(out=fbc_d[s_ * 2 + k_], in_=f_[:]), reads=[f_.b], writes=[S.buf("fbcd")], slot=f_.b)
                S.barrier()

            kv_lo, kv_hi = (0, 40) if l == 0 else (2, 38)
            q_lo, q_hi = (2, 38) if l == 0 else (4, 36)
            if tile_limit is not None:
                kv_hi = min(kv_hi, tile_limit)
                q_hi = min(q_hi, tile_limit - 3)
            xin = xw if l == 0 else x1
            cin = ctx0 if l == 0 else ctx1
            xin_b = dbuf("xin%d" % l) if l == 0 else dbuf("x1")
            cin_b = dbuf("cin%d" % l) if l == 0 else dbuf("ctx1")

            if dbg and "M" in stages:
                S.add("sp", lambda e: e.dma_start(out=dbg_modf[l], in_=modf[:].rearrange("p a b c -> p (a b c)")), reads=[modf.b], writes=[S.buf("dbgm")], slot=modf.b)
                for s_i in range(2):
                    for k_i in range(2):
                        g_ = gates[s_i][k_i]
                        S.add("sp", lambda e, g_=g_, s_i=s_i, k_i=k_i: e.dma_start(out=dbg_gates[l, s_i * 2 + k_i], in_=g_[:]), reads=[g_.b], writes=[S.buf("dbgg")], slot=g_.b)
                S.barrier()
            with ExitStack() as es2:
              if "PA" in stages:
                mk2 = lambda name, shape, dt, psum=False: T(S, es2, nc, name + "_L%d" % l, shape, dt, psum)
                Win = mk2("Win", [128, 8, 2560], BF16)
                Wout = mk2("Wout", [128, 8, 1024], BF16)
                Eint = mk2("Eint", [128, 6, 1024], BF16)
                Esp = mk2("Esp", [128, 6, 1024], BF16)
                bst = [mk2("bst%d" % i, [128, 1024], F32) for i in range(1)]
                g2 = mk2("g2", [128, 4], F32)
                slnb = mk2("slnb", [128, 512], F32)
                swTb = mk2("swTb", [128, 1024], BF16)
                sb = mk2("sb", [128, 8], F32)
                og = mk2("og", [128, 8], F32)
                xt = [mk2("xt%d" % i, [128, D], F32) for i in range(2)]
                junk = mk2("junk", [128, D], BF16)
                xn = [mk2("xn%d" % i, [128, D], BF16) for i in range(2)]
                hT = [mk2("hT%d" % i, [128, 8, 128], BF16) for i in range(2)]
                qk = mk2("qk", [128, D], F32)
                guv = mk2("guv", [128, D], F32)
                sqb = mk2("sqb", [128, D], F32)
                qkn = mk2("qkn", [128, D], BF16)
                st = [mk2("stat%d" % i, [128, 64], F32) for i in range(2)]
                KT = [mk2("KT%d" % i, [128, 4, 128], BF16) for i in range(8)]
                QT = [mk2("QT%d" % i, [128, 4, 128], BF16) for i in range(4)]
                V = [mk2("V%d" % i, [128, 520], BF16) for i in range(8)]
                cKT = [mk2("cKT%d" % i, [128, 4, 128], BF16) for i in range(2)]
                cQT = [mk2("cQT%d" % i, [128, 4, 128], BF16) for i in range(2)]
                cV = [mk2("cV%d" % i, [128, 520], BF16) for i in range(2)]
                vsn = mk2("vsn", [128, 512], BF16)
                t1 = mk2("t1", [128, 512], F32)
                osg = [mk2("osg%d" % i, [128, 512], F32) for i in range(4)]
                cosg = [mk2("cosg%d" % i, [128, 512], F32) for i in range(2)]
                rsg = [mk2("rsg%d" % i, [128, 4], F32) for i in range(4)]
                crsg = [mk2("crsg%d" % i, [128, 4], F32) for i in range(2)]
                pT = [mk2("pT%d" % i, [128, 1024], BF16) for i in range(2)]
                ona = mk2("ona", [128, 512], F32)
                ast = mk2("ast", [128, 16], F32)
                yn = mk2("yn", [128, D], BF16)
                yT = mk2("yT", [128, 8, 128], BF16)
                xa = mk2("xa", [128, D], F32)
                xo = [mk2("xo%d" % i, [128, D], F32) for i in range(1)]
                wjobs = []
                if True:
                    wst32 = [mk2("wst32_%d" % i, [128, 1024], F32) for i in range(2)]
                    wst16 = [mk2("wst16_%d" % i, [128, 1024], BF16) for i in range(1)]
                    for le in range(16 * l, 16 * l + 16):
                        for k_ in ("w1", "w3", "w2"):
                            for q_ in range(4):
                                v_ = wf[k_][le].rearrange("(c p) n -> p c n", p=128)
                                src = v_[:, 2 * q_:2 * q_ + 2, :] if k_ != "w2" else v_[:, q_:q_ + 1, :]
                                wjobs.append((src, wb[k_][le * 128:(le + 1) * 128, q_ * 1024:(q_ + 1) * 1024]))
                wcnt = {"i": 0}

                def _wload(j):
                    src, dst = wjobs[j]
                    a = wst32[j % 2]
                    c3 = src.shape[1]
                    S.add("pool", lambda e, a=a, src=src, c3=c3: e.dma_start(out=a[:].rearrange("p (c n) -> p c n", c=c3), in_=src), writes=[a.b], slot=a.b)

                def emit_wjobs(n):
                    for _ in range(n):
                        j = wcnt["i"]
                        if j >= len(wjobs):
                            return
                        if j == 0:
                            _wload(0)
                        if j + 1 < len(wjobs):
                            _wload(j + 1)
                        src, dst = wjobs[j]
                        a, b = wst32[j % 2], wst16[0]
                        wcnt["i"] += 1
                        S.add("pool", lambda e, a=a, b=b: e.tensor_copy(out=b[:], in_=a[:]), reads=[a.b], writes=[b.b])
                        S.add("pool", lambda e, b=b, dst=dst: e.dma_start(out=dst, in_=b[:]), reads=[b.b], writes=[S.buf("wtmp")], slot=b.b)

                P = [mk2("P%d" % i, [128, 512], F32, True) for i in range(1)]
                T0 = mk2("T0", [128, 1024], BF16, True)
                T1 = mk2("T1", [128, 1024], BF16, True)
                Sb = [mk2("S%d" % i, [128, 512], F32, True) for i in range(4)]
                O0 = mk2("O0", [128, 512], F32, True)

                S.add("sp", lambda e: e.dma_start(out=Win[:], in_=wview("w_in", l)), reads=[dbuf("w_in")], writes=[Win.b], slot=Win.b)
                S.add("sp", lambda e: e.dma_start(out=Wout[:], in_=wview("w_out", l)), reads=[dbuf("w_out")], writes=[Wout.b], slot=Wout.b)

                def load_E(dst, sidx):
                    for o in range(6):
                        b_ = bst[0]
                        S.add("sp", lambda e, b_=b_, o=o: e.dma_start(out=b_[:], in_=bias[l, sidx, o]), writes=[b_.b], slot=b_.b)
                        S.add("act", lambda e, b_=b_, o=o: e.activation(out=dst[:, o, :], in_=b_[:], func=AF.Exp), reads=[b_.b], writes=[dst.b])

                load_E(Eint, 0)
                S.add("sp", lambda e: e.dma_start(out=g2[:, 0:1], in_=qg[l]), writes=[g2.b], slot=g2.b)
                S.add("sp", lambda e: e.dma_start(out=g2[:, 1:2], in_=kg[l]), writes=[g2.b], slot=g2.b)
                S.add("dve", lambda e: e.scalar_tensor_tensor(out=g2[:, 2:3], in0=g2[:, 0:1], scalar=0.125, in1=g2[:, 1:2], op0=ALU.mult, op1=ALU.mult),
                      reads=[g2.b], writes=[g2.b])
                S.add("sp", lambda e: e.dma_start(out=slnb[:], in_=sln[l].partition_broadcast(128)), writes=[slnb.b], slot=slnb.b)
                S.add("sp", lambda e: e.dma_start(out=bst[0][:], in_=swT[l]), writes=[bst[0].b], slot=bst[0].b)
                S.add("dve", lambda e: e.tensor_copy(out=swTb[:], in_=bst[0][:]), reads=[bst[0].b], writes=[swTb.b])
                S.add("sp", lambda e: e.dma_start(out=sb[:], in_=sbT[l]), writes=[sb.b], slot=sb.b)
                S.add("sp", lambda e: e.dma_start(out=og[:], in_=ogT[l]), writes=[og.b], slot=og.b)
                for v_ in V + cV:
                    S.add("pool", lambda e, v_=v_: e.memset(v_[:].rearrange("p (h c) -> p h c", c=65)[:, :, 64:65], 1.0), writes=[v_.b])

                cnt = {"p": 0}

                def proj(src_ap, src_b, stream, kt, vt, qt, osg_t, rsg_t, need_q):
                    i = cnt["p"] % 2
                    cnt["p"] += 1
                    x_, xn_, h_, s_ = xt[i], xn[i], hT[i], st[i]
                    S.add("sp", lambda e: e.dma_start(out=x_[:], in_=src_ap), reads=[src_b], writes=[x_.b], slot=x_.b)
                    S.add("act", lambda e: e.activation(out=junk[:], in_=x_[:], func=AF.Square, accum_out=s_[:, 0:1]), reads=[x_.b], writes=[junk.b, s_.b])
                    rstd_ops((s_[:, 0:1], s_.b), (s_[:, 2:3], s_.b), (s_[:, 1:2], s_.b), D)
                    S.add("dve", lambda e: e.tensor_scalar(out=xn_[:], in0=x_[:], scalar1=s_[:, 2:3], scalar2=None, op0=ALU.mult), reads=[x_.b, s_.b], writes=[xn_.b])
                    yield
                    for kc in range(8):
                        S.add("pe", lambda e, kc=kc: e.transpose(out=T0[:, kc * 128:(kc + 1) * 128], in_=xn_[:, kc * 128:(kc + 1) * 128], identity=ident[:]),
                              reads=[xn_.b, ident.b], writes=[T0.b])
                    yield
                    for kc in range(8):
                        if kc % 2 == 0:
                            S.add("act", lambda e, kc=kc: e.activation(out=h_[:, kc, :], in_=T0[:, kc * 128:(kc + 1) * 128], func=AF.Identity,
                                                                      bias=modf[:, 0, kc, stream:stream + 1], scale=modf[:, 1, kc, stream:stream + 1]),
                                  reads=[T0.b, modf.b], writes=[h_.b])
                        else:
                            S.add("dve", lambda e, kc=kc: e.tensor_scalar(out=h_[:, kc, :], in0=T0[:, kc * 128:(kc + 1) * 128], scalar1=modf[:, 1, kc, stream:stream + 1],
                                                                         scalar2=modf[:, 0, kc, stream:stream + 1], op0=ALU.mult, op1=ALU.add),
                                  reads=[T0.b, modf.b], writes=[h_.b])
                    yield
                    plevel = float(_os.environ.get("DBG_P", "9"))
                    if plevel < 2:
                        return
                    chunks = [0, 1, 2, 3, 4] if need_q else [1, 2]
                    for nch in chunks:
                        yield
                        p_ = P[0]
                        for kc in range(8):
                            S.add("pe", lambda e, p_=p_, kc=kc, nch=nch: e.matmul(p_[:], lhsT=h_[:, kc, :], rhs=Win[:, kc, nch * 512:(nch + 1) * 512], start=(kc == 0), stop=(kc == 7)),
                                  reads=[h_.b, Win.b], writes=[p_.b])
                        if nch < 2:
                            S.add("act", lambda e, p_=p_, nch=nch: e.activation(out=qk[:, nch * 512:(nch + 1) * 512], in_=p_[:], func=AF.Copy), reads=[p_.b], writes=[qk.b])
                        elif nch == 2:
                            S.add("dve", lambda e, p_=p_: e.tensor_copy(out=vt[:].rearrange("p (h c) -> p h c", c=65)[:, :, 0:64], in_=p_[:].rearrange("p (h c) -> p h c", c=64)),
                                  reads=[p_.b], writes=[vt.b])
                        elif nch == 3:
                            S.add("act", lambda e, p_=p_: e.activation(out=guv[:, 0:512], in_=p_[:], func=AF.Gelu_apprx_tanh), reads=[p_.b], writes=[guv.b])
                        else:
                            S.add("act", lambda e, p_=p_: e.activation(out=guv[:, 512:1024], in_=p_[:], func=AF.Gelu_apprx_tanh, accum_out=s_[:, 4:5]), reads=[p_.b], writes=[guv.b, s_.b])
                    yield
                    if plevel < 2.2:
                        return
                    c0, nh = (0, 16) if need_q else (512, 8)
                    S.add("dve", lambda e: e.tensor_tensor(out=sqb[:, c0:1024], in0=qk[:, c0:1024], in1=qk[:, c0:1024], op=ALU.mult), reads=[qk.b], writes=[sqb.b])
                    S.add("dve", lambda e: e.tensor_reduce(out=s_[:, 16:16 + nh], in_=sqb[:, c0:1024].rearrange("p (h c) -> p h c", c=64), axis=AX.X, op=ALU.add),
                          reads=[sqb.b], writes=[s_.b])
                    rstd_ops((s_[:, 16:16 + nh], s_.b), (s_[:, 48:48 + nh], s_.b), (s_[:, 32:32 + nh], s_.b), 64)
                    yield
                    if plevel < 2.5:
                        return
                    S.add("dve", lambda e: e.tensor_tensor(out=qkn[:, c0:1024].rearrange("p (h c) -> p h c", c=64), in0=qk[:, c0:1024].rearrange("p (h c) -> p h c", c=64),
                                                          in1=s_[:, 48:48 + nh].unsqueeze(2).to_broadcast([128, nh, 64]), op=ALU.mult), reads=[qk.b, s_.b], writes=[qkn.b])
                    yield
                    for j in range(0 if need_q else 4, 8):
                        S.add("pe", lambda e, j=j: e.transpose(out=T0[:, j * 128:(j + 1) * 128], in_=qkn[:, j * 128:(j + 1) * 128], identity=ident[:]),
                              reads=[qkn.b, ident.b], writes=[T0.b])
                    yield
                    if plevel < 2.8:
                        return
                    ev = _os.environ.get("DBG_EV", "both")
                    if need_q and ev in ("q", "both"):
                        S.add("act", lambda e: e.activation(out=qt[:].rearrange("p a b -> p (a b)"), in_=T0[:, 0:512], func=AF.Identity, scale=g2[:, 2:3]),
                              reads=[T0.b, g2.b], writes=[qt.b])
                    if ev in ("k", "both"):
                        S.add("act", lambda e: e.activation(out=kt[:].rearrange("p a b -> p (a b)"), in_=T0[:, 512:1024], func=AF.Copy), reads=[T0.b], writes=[kt.b])
                    yield
                    if not need_q or plevel < 4:
                        return
                    S.add("dve", lambda e: e.tensor_scalar(out=s_[:, 5:6], in0=s_[:, 4:5], scalar1=-1.0 / 512, scalar2=None, op0=ALU.mult), reads=[s_.b], writes=[s_.b])
                    S.add("act", lambda e: e.activation(out=junk[:, 0:512], in_=guv[:, 512:1024], func=AF.Square, bias=s_[:, 5:6], accum_out=s_[:, 6:7]),
                          reads=[guv.b, s_.b], writes=[junk.b, s_.b])
                    rstd_ops((s_[:, 6:7], s_.b), (s_[:, 8:9], s_.b), (s_[:, 7:8], s_.b), 512)
                    S.add("dve", lambda e: e.tensor_scalar(out=vsn[:], in0=guv[:, 512:1024], scalar1=s_[:, 5:6], scalar2=s_[:, 8:9], op0=ALU.add, op1=ALU.mult),
                          reads=[guv.b, s_.b], writes=[vsn.b])
                    yield
                    p_ = P[0]
                    for h in range(8):
                        S.add("pe", lambda e, h=h: e.matmul(p_[:, h * 64:(h + 1) * 64], lhsT=swTb[:, h * 128:(h + 1) * 128], rhs=vsn[:, h * 64:(h + 1) * 64], start=True, stop=True),
                              reads=[swTb.b, vsn.b], writes=[p_.b])
                    yield
                    S.add("dve", lambda e: e.tensor_tensor(out=t1[:], in0=p_[:], in1=slnb[:], op=ALU.mult), reads=[p_.b, slnb.b], writes=[t1.b])
                    S.add("dve", lambda e: e.tensor_tensor(out=t1[:].rearrange("p (h c) -> p h c", c=64), in0=t1[:].rearrange("p (h c) -> p h c", c=64),
                                                          in1=sb[:].unsqueeze(2).to_broadcast([128, 8, 64]), op=ALU.add), reads=[t1.b, sb.b], writes=[t1.b])
                    S.add("dve", lambda e: e.tensor_tensor(out=osg_t[:], in0=t1[:], in1=guv[:, 0:512], op=ALU.mult), reads=[t1.b, guv.b], writes=[osg_t.b])
                    yield
                    S.add("act", lambda e: e.activation(out=junk[:, 512:1024], in_=osg_t[:], func=AF.Square, accum_out=rsg_t[:, 0:1]), reads=[osg_t.b], writes=[junk.b, rsg_t.b])
                    rstd_ops((rsg_t[:, 0:1], rsg_t.b), (rsg_t[:, 2:3], rsg_t.b), (rsg_t[:, 1:2], rsg_t.b), 512)

                acnt = {"a": 0}

                def attn(src_ap, src_b, dst_ap, dst_b, stream, qt, keys, Eset, osg_t, rsg_t):
                    nk = len(keys)
                    nw = sum(1 for k_ in keys if k_[2] is not None)
                    def emit_qk(h):
                        hp, po = h // 2, (h % 2) * 64
                        SA, SB2 = Sb[(h % 2) * 2], Sb[(h % 2) * 2 + 1]
                        for i, (kt, vt, eo) in enumerate(keys):
                            bank = SA if i < 4 else SB2
                            col = (i % 4) * 128
                            S.add("pe", lambda e, kt=kt, bank=bank, col=col, po=po, hp=hp: e.matmul(bank[:, col:col + 128], lhsT=kt[po:po + 64, hp, :], rhs=qt[po:po + 64, hp, :], start=True, stop=True),
                                  reads=[kt.b, qt.b], writes=[bank.b])

                    emit_qk(0)
                    for h in range(8):
                        SA, SB2 = Sb[(h % 2) * 2], Sb[(h % 2) * 2 + 1]
                        p_ = pT[h % 2]
                        if h + 1 < 8:
                            emit_qk(h + 1)
                        yield
                        na = min(nk, 4)
                        S.add("act", lambda e, SA=SA, p_=p_, na=na: e.activation(out=p_[:, 0:na * 128], in_=SA[:, 0:na * 128], func=AF.Exp), reads=[SA.b], writes=[p_.b])
                        if nk > 4:
                            S.add("act", lambda e, SB2=SB2, p_=p_: e.activation(out=p_[:, 512:nk * 128], in_=SB2[:, 0:(nk - 4) * 128], func=AF.Exp), reads=[SB2.b], writes=[p_.b])
                        yield
                        if nw > 0:
                            S.add("dve", lambda e, p_=p_, h=h: e.tensor_tensor(out=p_[:, 0:nw * 128].rearrange("p (a b) -> p a b", b=128), in0=p_[:, 0:nw * 128].rearrange("p (a b) -> p a b", b=128),
                                                                              in1=Eset[:, 0:nw, h * 128:(h + 1) * 128], op=ALU.mult), reads=[p_.b, Eset.b], writes=[p_.b])
                        yield
                        oc = (h % 4) * 65
                        for i, (kt, vt, eo) in enumerate(keys):
                            S.add("pe", lambda e, vt=vt, i=i, p_=p_, oc=oc, h=h: e.matmul(O0[:, oc:oc + 65], lhsT=p_[:, i * 128:(i + 1) * 128], rhs=vt[:, h * 65:(h + 1) * 65], start=(i == 0), stop=(i == nk - 1)),
                                  reads=[p_.b, vt.b], writes=[O0.b])
                        yield
                        if h % 4 == 3:
                            hb = h - 3
                            S.add("dve", lambda e, hb=hb: e.reciprocal(out=ast[:, hb:hb + 4], in_=O0[:, 0:260].rearrange("p (h c) -> p h c", c=65)[:, :, 64]), reads=[O0.b], writes=[ast.b])
                            S.add("dve", lambda e, hb=hb: e.tensor_tensor(out=ona[:, hb * 64:(hb + 4) * 64].rearrange("p (h c) -> p h c", c=64), in0=O0[:, 0:260].rearrange("p (h c) -> p h c", c=65)[:, :, 0:64],
                                                                         in1=ast[:, hb:hb + 4].unsqueeze(2).to_broadcast([128, 4, 64]), op=ALU.mult), reads=[O0.b, ast.b], writes=[ona.b])
                    yield
                    S.add("act", lambda e: e.activation(out=junk[:, 0:512], in_=ona[:], func=AF.Square, accum_out=ast[:, 8:9]), reads=[ona.b], writes=[junk.b, ast.b])
                    rstd_ops((ast[:, 8:9], ast.b), (ast[:, 10:11], ast.b), (ast[:, 9:10], ast.b), 512)
                    S.add("dve", lambda e: e.tensor_scalar(out=yn[:, 0:512], in0=ona[:], scalar1=ast[:, 10:11], scalar2=None, op0=ALU.mult), reads=[ona.b, ast.b], writes=[yn.b])
                    S.add("dve", lambda e: e.tensor_scalar(out=yn[:, 512:1024], in0=osg_t[:], scalar1=rsg_t[:, 2:3], scalar2=None, op0=ALU.mult), reads=[osg_t.b, rsg_t.b], writes=[yn.b])
                    yield
                    for kc in range(8):
                        S.add("pe", lambda e, kc=kc: e.transpose(out=T1[:, kc * 128:(kc + 1) * 128], in_=yn[:, kc * 128:(kc + 1) * 128], identity=ident[:]), reads=[yn.b, ident.b], writes=[T1.b])
                    for kc in range(8):
                        if kc % 2 == 0:
                            S.add("act", lambda e, kc=kc: e.activation(out=yT[:, kc, :], in_=T1[:, kc * 128:(kc + 1) * 128], func=AF.Identity, scale=og[:, kc:kc + 1]),
                                  reads=[T1.b, og.b], writes=[yT.b])
                        else:
                            S.add("dve", lambda e, kc=kc: e.tensor_scalar(out=yT[:, kc, :], in0=T1[:, kc * 128:(kc + 1) * 128], scalar1=og[:, kc:kc + 1], scalar2=zc[:, 0:1], op0=ALU.mult, op1=ALU.add),
                                  reads=[T1.b, og.b, zc.b], writes=[yT.b])
                    yield
                    S.add("sp", lambda e: e.dma_start(out=xa[:], in_=src_ap), reads=[src_b], writes=[xa.b], slot=xa.b)
                    xo_ = xo[0]
                    acnt["a"] += 1
                    for half in range(2):
                        yield
                        p_ = Sb[half]
                        for kc in range(8):
                            S.add("pe", lambda e, p_=p_, kc=kc, half=half: e.matmul(p_[:], lhsT=yT[:, kc, :], rhs=Wout[:, kc, half * 512:(half + 1) * 512], start=(kc == 0), stop=(kc == 7)),
                                  reads=[yT.b, Wout.b], writes=[p_.b])
                        S.add("dve", lambda e, p_=p_, half=half: e.tensor_tensor(out=xo_[:, half * 512:(half + 1) * 512], in0=p_[:], in1=gates[stream][0][:, half * 512:(half + 1) * 512], op=ALU.mult),
                              reads=[p_.b, gates[stream][0].b], writes=[xo_.b])
                    S.add("dve", lambda e: e.tensor_tensor(out=xo_[:], in0=xo_[:], in1=xa[:], op=ALU.add), reads=[xo_.b, xa.b], writes=[xo_.b])
                    S.add("pool", lambda e: e.dma_start(out=dst_ap, in_=xo_[:]), reads=[xo_.b], writes=[dst_b], slot=xo_.b)

                pa_mode = _os.environ.get("DBG_PA", "full")
                def run(*gens):
                    gens = list(gens)
                    while gens:
                        for g_ in list(gens):
                            try:
                                next(g_)
                            except StopIteration:
                                gens.remove(g_)

                for ci in range(2 if pa_mode != "setup" else 0):
                    run(proj(cin[ci * 128:(ci + 1) * 128, :], cin_b, 1, cKT[ci], cV[ci], cQT[ci], cosg[ci], crsg[ci], l == 0))
                if l == 0 and pa_mode == "full":
                    for ci in range(2):
                        run(attn(cin[ci * 128:(ci + 1) * 128, :], cin_b, ctxm[ci * 128:(ci + 1) * 128, :], dbuf("ctxm"), 1, cQT[ci],
                                 [(cKT[0], cV[0], None), (cKT[1], cV[1], None)], Eint, cosg[ci], crsg[ci]))

                def do_attn(t):
                    if t in (4, 5, 34, 35):
                        sidx = {4: 1, 5: 2, 34: 3, 35: 4}[t]
                        load_E(Esp, sidx)
                        offs = list(range(-2, 4)) if t in (4, 5) else list(range(-3, 3))
                        Eset = Esp
                    else:
                        offs = list(range(-2, 3))
                        Eset = Eint
                    keys = [(KT[(t + o) % 8], V[(t + o) % 8], i) for i, o in enumerate(offs)]
                    keys += [(cKT[0], cV[0], None), (cKT[1], cV[1], None)]
                    return attn(xin[t * 128:(t + 1) * 128, :], xin_b, xm[t * 128:(t + 1) * 128, :], dbuf("xm"), 0, QT[t % 4], keys, Eset, osg[t % 4], rsg[t % 4])

                if pa_mode == "setup":
                    kv_hi = kv_lo
                if pa_mode != "full":
                    q_hi = q_lo
                for t in range(kv_lo, kv_hi):
                    nq = q_lo <= t < q_hi
                    pg_ = proj(xin[t * 128:(t + 1) * 128, :], xin_b, 0, KT[t % 8], V[t % 8], QT[t % 4], osg[t % 4], rsg[t % 4], nq)
                    ta = t - 3
                    if q_lo <= ta < q_hi:
                        if ta in (4, 5):
                            run(pg_)
                            run(do_attn(ta))
                        else:
                            run(pg_, do_attn(ta))
                    else:
                        run(pg_)
                    emit_wjobs(6)
                emit_wjobs(len(wjobs))
                for t in range(kv_hi - 3, q_hi):
                    if t >= q_lo:
                        run(do_attn(t))
                S.barrier()

            if not do_f:
                return
            with ExitStack() as es2:
              if "F" in stages:
                U32 = mybir.dt.uint32
                mk2 = lambda name, shape, dt, psum=False: T(S, es2, nc, name + "_L%d" % l, shape, dt, psum)
                TS = 512
                tiles_all = []
                if l == 0:
                    for ci in range(2):
                        tiles_all.append((ctxm[ci * 128:(ci + 1) * 128, :], dbuf("ctxm"), ctx1[ci * 128:(ci + 1) * 128, :], dbuf("ctx1"), 1, 38 + ci))
                for t in range(q_lo, q_hi):
                    if l == n_layers - 1 and l == 1:
                        dap, db_ = out[(t - 4) * 128:(t - 3) * 128, :], dbuf("out")
                    else:
                        dap, db_ = x1[t * 128:(t + 1) * 128, :], dbuf("x1")
                    tiles_all.append((xm[t * 128:(t + 1) * 128, :], dbuf("xm"), dap, db_, 0, t if t < 38 else t))
                NTOK = len(tiles_all)
                NT = (2 * NTOK * 128 + TS - 1) // TS + 16
                assert NT <= NTMAX
                ew1 = [mk2("ew1_%d" % i, [128, 4096], BF16) for i in range(2)]
                ew3 = [mk2("ew3_%d" % i, [128, 4096], BF16) for i in range(2)]
                ew2 = [mk2("ew2_%d" % i, [128, 4096], BF16) for i in range(2)]
                xg = mk2("xg", [128, 4, D], F32)
                junkf = mk2("junkf", [128, D], BF16)
                xn32 = mk2("xn32", [128, D], F32)
                hf32 = mk2("hf32", [128, 8, 128], F32)
                hs = [mk2("hs%d" % i, [128, D], BF16) for i in range(2)]
                rwt = mk2("rwt", [128, 8, 20], F32)
                rbb = mk2("rbb", [128, 20], F32)
                lg = mk2("lg", [128, 4, 20], F32)
                r = {n_: mk2("r_" + n_, [128, 4, 16], F32) for n_ in ("a", "b", "c", "d", "e", "f", "g", "h", "i", "j", "k")}
                fs = mk2("fs", [128, 16], F32)
                OH = [mk2("OH%d" % i, [128, NTOK, 16], F32) for i in range(2)]
                POS = mk2("POS", [128, NTOK, 16], F32)
                WAB = [mk2("WAB%d" % i, [128, NTOK], F32) for i in range(2)]
                base = mk2("base", [128, 16], F32)
                sel = mk2("sel", [128, 4, 16], BF16)
                cU = mk2("cU", [128, 128], BF16)
                cOnes = mk2("cOnes", [128, 128], BF16)
                itc = mk2("itc", [128, NTMAX, 16], F32)
                cmpt = mk2("cmpt", [128, NTMAX, 16], F32)
                pidx = mk2("pidx", [128, 2], F32)
                offs = mk2("offs", [128, 4, 16], F32)
                eid = mk2("eid", [128, NTMAX], F32)
                widx = mk2("widx", [128, NTMAX], U32)
                slf = [mk2("slf%d" % i, [128, NTOK], F32) for i in range(2)]
                slu = [mk2("slu%d" % i, [128, NTOK], U32) for i in range(2)]
                ptmp = mk2("ptmp", [128, NTOK, 16], F32)
                srt = [mk2("srt%d" % i, [128, 4, D], BF16) for i in range(2)]
                hfT = mk2("hfT", [128, 8, 512], BF16)
                sl = [mk2("sl%d" % i, [128, 512], BF16) for i in range(2)]
                hidT = [mk2("hidT%d" % i, [128, 4, 512], BF16) for i in range(2)]
                ob = [mk2("ob%d" % i, [128, D], F32) for i in range(2)]
                ra = mk2("ra", [128, D], F32)
                rb_ = mk2("rb", [128, D], F32)
                xr = mk2("xr", [128, D], F32)
                xo = [mk2("fxo%d" % i, [128, D], F32) for i in range(2)]
                H1 = [mk2("H1_%d" % i, [128, 512], F32, True) for i in range(2)]
                H3 = [mk2("H3_%d" % i, [128, 512], F32, True) for i in range(2)]
                OUT = [mk2("OUT%d" % i, [128, 512], F32, True) for i in range(2)]
                TB = mk2("TB", [128, 1024], BF16, True)
                TF = mk2("TF", [128, 512], F32, True)
                S.add("sp", lambda e: e.dma_start(out=rwt[:], in_=rw[l].rearrange("(kc p) n -> p kc n", p=128)), writes=[rwt.b], slot=rwt.b)
                S.add("sp", lambda e: e.dma_start(out=rbb[:], in_=rb[l].partition_broadcast(128)), writes=[rbb.b], slot=rbb.b)
                fbc = [[mk2("ffbc%d%d" % (s_, k_), [128, D], F32) for k_ in range(2)] for s_ in range(2)]
                for s_ in range(2):
                    for k_ in range(2):
                        f_ = fbc[s_][k_]
                        S.add("sp", lambda e, f_=f_, s_=s_, k_=k_: e.dma_start(out=f_[:], in_=fbc_d[s_ * 2 + k_]), writes=[f_.b], slot=f_.b)
                S.add("sp", lambda e: e.dma_start(out=cU[:], in_=cU_d[:, :]), writes=[cU.b], slot=cU.b)
                S.add("sp", lambda e: e.dma_start(out=cOnes[:], in_=cOnes_d[:, :]), writes=[cOnes.b], slot=cOnes.b)
                S.add("sp", lambda e: e.dma_start(out=itc[:].rearrange("p a b -> p (a b)"), in_=itc_d[:, :]), writes=[itc.b], slot=itc.b)
                S.add("sp", lambda e: e.dma_start(out=pidx[:], in_=pidx_d[:, :]), writes=[pidx.b], slot=pidx.b)
                S.add("pool", lambda e: e.memset(base[:], 0.0), writes=[base.b])
                S.add("pool", lambda e: e.memset(hfT[:], 0.0), writes=[hfT.b])
                srt_flat = srt_d.rearrange("r n -> (r n)").rearrange("(p f) -> p f", p=128)
                for i in range(NT):
                    S.add("sp", lambda e, i=i: e.dma_start(out=srt_d[i * 512:(i + 1) * 512, :].rearrange("(p a) n -> p (a n)", p=128), in_=hfT[:].rearrange("p a b -> p (a b)")),
                          reads=[hfT.b], writes=[dbuf("srt") if i == NT - 1 else S.buf("zf")], slot=hfT.b)

                def dv(fn, reads, writes):
                    S.add("dve", fn, reads=[x_.b for x_ in reads], writes=[x_.b for x_ in writes])

                hcnt = {"h": 0}

                def route_group(idx0, G):
                    for ti in range(G):
                        sap, sbuf_, dap, dbuf_, stream, hrow = tiles_all[idx0 + ti]
                        S.add("sp", lambda e, ti=ti, sap=sap: e.dma_start(out=xg[:, ti, :], in_=sap), reads=[sbuf_], writes=[xg.b], slot=xg.b)
                    for ti in range(G):
                        sap, sbuf_, dap, dbuf_, stream, hrow = tiles_all[idx0 + ti]
                        S.add("act", lambda e, ti=ti: e.activation(out=junkf[:], in_=xg[:, ti, :], func=AF.Square, accum_out=fs[:, 0:1]), reads=[xg.b], writes=[junkf.b, fs.b])
                        rstd_ops((fs[:, 0:1], fs.b), (fs[:, 2:3], fs.b), (fs[:, 1:2], fs.b), D)
                        S.add("dve", lambda e, ti=ti: e.tensor_scalar(out=xn32[:], in0=xg[:, ti, :], scalar1=fs[:, 2:3], scalar2=None, op0=ALU.mult), reads=[xg.b, fs.b], writes=[xn32.b])
                        h_ = hs[hcnt["h"] % 2]
                        hcnt["h"] += 1
                        S.add("dve", lambda e, stream=stream: e.tensor_tensor(out=ra[:], in0=xn32[:], in1=fbc[stream][1][:], op=ALU.mult), reads=[xn32.b, fbc[stream][1].b], writes=[ra.b])
                        S.add("dve", lambda e, stream=stream, h_=h_: e.tensor_tensor(out=h_[:], in0=ra[:], in1=fbc[stream][0][:], op=ALU.add), reads=[ra.b, fbc[stream][0].b], writes=[h_.b])
                        S.add("pool", lambda e, h_=h_, hrow=hrow: e.dma_start(out=hfd[hrow * 128:(hrow + 1) * 128, :], in_=h_[:]), reads=[h_.b], writes=[dbuf("hfd")], slot=h_.b)
                        for half in range(2):
                            for kc in range(4):
                                S.add("pe", lambda e, kc=kc, half=half: e.transpose(out=TF[:, kc * 128:(kc + 1) * 128], in_=xn32[:, (half * 4 + kc) * 128:(half * 4 + kc + 1) * 128], identity=identf[:]),
                                      reads=[xn32.b, identf.b], writes=[TF.b])
                            for kc in range(4):
                                k8 = half * 4 + kc
                                S.add("dve", lambda e, kc=kc, k8=k8, stream=stream: e.tensor_scalar(out=hf32[:, k8, :], in0=TF[:, kc * 128:(kc + 1) * 128], scalar1=modf[:, 3, k8, stream:stream + 1],
                                                                                                 scalar2=modf[:, 2, k8, stream:stream + 1], op0=ALU.mult, op1=ALU.add), reads=[TF.b, modf.b], writes=[hf32.b])
                        for kc in range(8):
                            S.add("pe", lambda e, kc=kc, ti=ti: e.matmul(H1[0][:, ti * 20:(ti + 1) * 20], lhsT=hf32[:, kc, :], rhs=rwt[:, kc, :], start=(kc == 0), stop=(kc == 7)),
                                  reads=[hf32.b, rwt.b], writes=[H1[0].b])
                        S.add("dve", lambda e, ti=ti: e.tensor_tensor(out=lg[:, ti, :], in0=H1[0][:, ti * 20:(ti + 1) * 20], in1=rbb[:], op=ALU.add), reads=[H1[0].b, rbb.b], writes=[lg.b])
                    lgG = lg[:, 0:G, 0:4]
                    lgE = lg[:, 0:G, 4:20]
                    mg, ohg, dg, eg, sg, pt = r["a"], r["b"], r["c"], r["d"], r["e"], r["f"]
                    dv(lambda e: e.tensor_reduce(out=mg[:, 0:G, 0], in_=lgG, axis=AX.X, op=ALU.max), [lg], [mg])
                    dv(lambda e: e.tensor_tensor(out=ohg[:, 0:G, 0:4], in0=lgG, in1=mg[:, 0:G, 0:1].to_broadcast([128, G, 4]), op=ALU.is_equal), [lg, mg], [ohg])
                    dv(lambda e: e.tensor_tensor(out=dg[:, 0:G, 0:4], in0=lgG, in1=mg[:, 0:G, 0:1].to_broadcast([128, G, 4]), op=ALU.subtract), [lg, mg], [dg])
                    S.add("act", lambda e: e.activation(out=eg[:, 0:G, 0:4], in_=dg[:, 0:G, 0:4], func=AF.Exp), reads=[dg.b], writes=[eg.b])
                    dv(lambda e: e.tensor_reduce(out=sg[:, 0:G, 0], in_=eg[:, 0:G, 0:4], axis=AX.X, op=ALU.add), [eg], [sg])
                    dv(lambda e: e.reciprocal(out=pt[:, 0:G, 0], in_=sg[:, 0:G, 0]), [sg], [pt])
                    tmp, el = r["g"], r["h"]
                    dv(lambda e: e.tensor_tensor(out=tmp[:, 0:G, :].rearrange("p t (g j) -> p t g j", j=4), in0=lgE.rearrange("p t (g j) -> p t g j", j=4),
                                                 in1=ohg[:, 0:G, 0:4].unsqueeze(3).to_broadcast([128, G, 4, 4]), op=ALU.mult), [lg, ohg], [tmp])
                    dv(lambda e: e.tensor_reduce(out=el[:, 0:G, 0:4], in_=tmp[:, 0:G, :].rearrange("p t (g j) -> p t j g", j=4), axis=AX.X, op=ALU.add), [tmp], [el])
                    m1, oh1, el2, m2, oh2 = r["i"], r["j"], r["k"], r["c"], r["d"]
                    dv(lambda e: e.tensor_reduce(out=m1[:, 0:G, 0], in_=el[:, 0:G, 0:4], axis=AX.X, op=ALU.max), [el], [m1])
                    dv(lambda e: e.tensor_tensor(out=oh1[:, 0:G, 0:4], in0=el[:, 0:G, 0:4], in1=m1[:, 0:G, 0:1].to_broadcast([128, G, 4]), op=ALU.is_equal), [el, m1], [oh1])
                    dv(lambda e: e.scalar_tensor_tensor(out=el2[:, 0:G, 0:4], in0=oh1[:, 0:G, 0:4], scalar=-1e30, in1=el[:, 0:G, 0:4], op0=ALU.mult, op1=ALU.add), [oh1, el], [el2])
                    dv(lambda e: e.tensor_reduce(out=m2[:, 0:G, 0], in_=el2[:, 0:G, 0:4], axis=AX.X, op=ALU.max), [el2], [m2])
                    dv(lambda e: e.tensor_tensor(out=oh2[:, 0:G, 0:4], in0=el2[:, 0:G, 0:4], in1=m2[:, 0:G, 0:1].to_broadcast([128, G, 4]), op=ALU.is_equal), [el2, m2], [oh2])
                    dd, ee, w1_, w2_ = r["a"], r["e"], r["g"], r["h"]
                    dv(lambda e: e.tensor_tensor(out=dd[:, 0:G, 0], in0=m2[:, 0:G, 0], in1=m1[:, 0:G, 0], op=ALU.subtract), [m2, m1], [dd])
                    S.add("act", lambda e: e.activation(out=ee[:, 0:G, 0], in_=dd[:, 0:G, 0], func=AF.Exp), reads=[dd.b], writes=[ee.b])
                    dv(lambda e: e.tensor_scalar(out=dd[:, 0:G, 1], in0=ee[:, 0:G, 0], scalar1=1.0, scalar2=None, op0=ALU.add), [ee], [dd])
                    dv(lambda e: e.reciprocal(out=dd[:, 0:G, 2], in_=dd[:, 0:G, 1]), [dd], [dd])
                    dv(lambda e: e.tensor_tensor(out=w1_[:, 0:G, 0], in0=dd[:, 0:G, 2], in1=pt[:, 0:G, 0], op=ALU.mult), [dd, pt], [w1_])
                    dv(lambda e: e.tensor_tensor(out=w2_[:, 0:G, 0], in0=w1_[:, 0:G, 0], in1=ee[:, 0:G, 0], op=ALU.mult), [w1_, ee], [w2_])
                    wa_, wb_ = r["i"], r["k"]
                    dv(lambda e: e.tensor_tensor(out=wa_[:, 0:G, 0:4], in0=oh1[:, 0:G, 0:4], in1=w1_[:, 0:G, 0:1].to_broadcast([128, G, 4]), op=ALU.mult), [oh1, w1_], [wa_])
                    dv(lambda e: e.tensor_tensor(out=wb_[:, 0:G, 0:4], in0=oh2[:, 0:G, 0:4], in1=w2_[:, 0:G, 0:1].to_broadcast([128, G, 4]), op=ALU.mult), [oh2, w2_], [wb_])
                    dv(lambda e: e.tensor_tensor(out=wa_[:, 0:G, 0:4], in0=wa_[:, 0:G, 0:4], in1=wb_[:, 0:G, 0:4], op=ALU.add), [wa_, wb_], [wa_])
                    for k_, oh_ in enumerate((oh1, oh2)):
                        dv(lambda e, k_=k_, oh_=oh_: e.tensor_tensor(out=OH[k_][:, idx0:idx0 + G, :].rearrange("p t (g j) -> p t g j", j=4), in0=ohg[:, 0:G, 0:4].unsqueeze(3).to_broadcast([128, G, 4, 4]),
                                                                    in1=oh_[:, 0:G, 0:4].unsqueeze(2).to_broadcast([128, G, 4, 4]), op=ALU.mult), [ohg, oh_], [OH[k_]])
                    dv(lambda e: e.tensor_copy(out=WAB[0][:, idx0:idx0 + G], in_=w1_[:, 0:G, 0]), [w1_], [WAB[0]])
                    dv(lambda e: e.tensor_copy(out=WAB[1][:, idx0:idx0 + G], in_=w2_[:, 0:G, 0]), [w2_], [WAB[1]])
                    dv(lambda e: e.tensor_tensor(out=sel[:, 0:G, :], in0=OH[0][:, idx0:idx0 + G, :], in1=OH[1][:, idx0:idx0 + G, :], op=ALU.add), [OH[0], OH[1]], [sel])
                    for ti in range(G):
                        S.add("pe", lambda e, ti=ti: e.matmul(H3[0][:, ti * 32:ti * 32 + 16], lhsT=cU[:], rhs=sel[:, ti, :], start=True, stop=True), reads=[cU.b, sel.b], writes=[H3[0].b])
                        S.add("pe", lambda e, ti=ti: e.matmul(H3[0][:, ti * 32 + 16:ti * 32 + 32], lhsT=cOnes[:], rhs=sel[:, ti, :], start=True, stop=True), reads=[cOnes.b, sel.b], writes=[H3[0].b])
                        dv(lambda e, ti=ti: e.tensor_tensor(out=POS[:, idx0 + ti, :], in0=H3[0][:, ti * 32:ti * 32 + 16], in1=base[:], op=ALU.add), [H3[0], base], [POS])
                        dv(lambda e, ti=ti: e.tensor_tensor(out=base[:], in0=H3[0][:, ti * 32 + 16:ti * 32 + 32], in1=base[:], op=ALU.add), [H3[0], base], [base])

                for g0 in range(0, NTOK, 4):
                    route_group(g0, min(4, NTOK - g0))
                o_tmp, o_pad, o_off, o_end = offs[:, 0, :], offs[:, 1, :], offs[:, 2, :], offs[:, 3, :]
                dv(lambda e: e.tensor_tensor(out=cmpt[:, 0:19, :].rearrange("p j e -> p e j"), in0=itc[:, 0:19, :].rearrange("p j e -> p e j"),
                                             in1=base[:].unsqueeze(2).to_broadcast([128, 16, 19]), op=ALU.is_lt), [itc, base], [cmpt])
                dv(lambda e: e.tensor_reduce(out=o_tmp, in_=cmpt[:, 0:19, :].rearrange("p j e -> p e j"), axis=AX.X, op=ALU.add), [cmpt], [offs])
                dv(lambda e: e.tensor_scalar(out=o_pad, in0=o_tmp, scalar1=float(TS), scalar2=None, op0=ALU.mult), [offs], [offs])
                S.add("pool", lambda e: e.memset(offs[:, 2, 0:1], 0.0), reads=[offs.b], writes=[offs.b])
                for ex in range(1, 16):
                    dv(lambda e, ex=ex: e.tensor_tensor(out=offs[:, 2, ex:ex + 1], in0=offs[:, 2, ex - 1:ex], in1=offs[:, 1, ex - 1:ex], op=ALU.add), [offs], [offs])
                dv(lambda e: e.tensor_tensor(out=o_end, in0=o_off, in1=o_pad, op=ALU.add), [offs], [offs])
                dv(lambda e: e.tensor_tensor(out=cmpt[:, 0:NT, :], in0=itc[:, 0:NT, :], in1=offs[:, 3:4, :].to_broadcast([128, NT, 16]), op=ALU.is_ge), [itc, offs], [cmpt])
                dv(lambda e: e.tensor_reduce(out=eid[:, 0:NT], in_=cmpt[:, 0:NT, :], axis=AX.X, op=ALU.add), [cmpt], [eid])
                dv(lambda e: e.tensor_scalar(out=eid[:, 0:NT], in0=eid[:, 0:NT], scalar1=15.0, scalar2=128.0, op0=ALU.min, op1=ALU.mult), [eid], [eid])
                dv(lambda e: e.tensor_scalar(out=eid[:, 0:NT], in0=eid[:, 0:NT], scalar1=pidx[:, l:l + 1], scalar2=None, op0=ALU.add), [eid, pidx], [eid])
                dv(lambda e: e.tensor_copy(out=widx[:, 0:NT], in_=eid[:, 0:NT]), [eid], [widx])
                dv(lambda e: e.tensor_tensor(out=POS[:], in0=POS[:], in1=offs[:, 2:3, :].to_broadcast([128, NTOK, 16]), op=ALU.add), [POS, offs], [POS])
                for k_ in range(2):
                    dv(lambda e, k_=k_: e.tensor_tensor(out=ptmp[:], in0=POS[:], in1=OH[k_][:], op=ALU.mult), [POS, OH[k_]], [ptmp])
                    dv(lambda e, k_=k_: e.tensor_reduce(out=slf[k_][:], in_=ptmp[:], axis=AX.X, op=ALU.add), [ptmp], [slf[k_]])
                    dv(lambda e, k_=k_: e.tensor_copy(out=slu[k_][:], in_=slf[k_][:]), [slf[k_]], [slu[k_]])
                for ti in range(NTOK):
                    hrow = tiles_all[ti][5]
                    h_ = hs[ti % 2]
                    S.add("sp", lambda e, h_=h_, hrow=hrow: e.dma_start(out=h_[:], in_=hfd[hrow * 128:(hrow + 1) * 128, :]), reads=[dbuf("hfd")], writes=[h_.b], slot=h_.b)
                    for k_ in range(2):
                        S.add("pool", lambda e, h_=h_, k_=k_, ti=ti: e.indirect_dma_start(out=srt_d[0:NT * 512, :], out_offset=bass.IndirectOffsetOnAxis(ap=slu[k_][:, ti:ti + 1], axis=0), in_=h_[:], in_offset=None),
                              reads=[h_.b, slu[k_].b], writes=[dbuf("srt")], slot=h_.b)
                for i in range(NT):
                    a1, a3, a2 = ew1[i % 2], ew3[i % 2], ew2[i % 2]
                    for a_, k_ in ((a1, "w1"), (a3, "w3"), (a2, "w2")):
                        S.add("pool", lambda e, a_=a_, k_=k_, i=i: e.indirect_dma_start(out=a_[:], out_offset=None, in_=wb[k_][:, :], in_offset=bass.IndirectOffsetOnAxis(ap=widx[:, i:i + 1], axis=0)),
                              reads=[widx.b], writes=[a_.b], slot=a_.b)
                    sr = srt[i % 2]
                    for ii in ([0, 1] if i == 0 else [i + 1]):
                        if ii < NT:
                            sr2 = srt[ii % 2]
                            S.add("sp", lambda e, sr2=sr2, ii=ii: e.dma_start(out=sr2[:], in_=srt_d[ii * 512:(ii + 1) * 512, :].rearrange("(a p) n -> p a n", p=128)), reads=[dbuf("srt")], writes=[sr2.b], slot=sr2.b)
                    for a in range(4):
                        for kc in range(8):
                            S.add("pe", lambda e, sr=sr, a=a, kc=kc: e.transpose(out=TB[:, kc * 128:(kc + 1) * 128], in_=sr[:, a, kc * 128:(kc + 1) * 128], identity=ident[:]), reads=[sr.b, ident.b], writes=[TB.b])
                        S.add("act", lambda e, a=a: e.activation(out=hfT[:, :, a * 128:(a + 1) * 128], in_=TB[:].rearrange("p (c t) -> p c t", t=128), func=AF.Copy), reads=[TB.b], writes=[hfT.b])
                    hd = hidT[i % 2]
                    a1v = a1[:].rearrange("p (c n) -> p c n", c=8)
                    a3v = a3[:].rearrange("p (c n) -> p c n", c=8)
                    a2v = a2[:].rearrange("p (c n) -> p c n", c=4)
                    for hc in range(4):
                        h1, h3, s_ = H1[hc % 2], H3[hc % 2], sl[hc % 2]
                        for kc in range(8):
                            S.add("pe", lambda e, h1=h1, a1v=a1v, kc=kc, hc=hc: e.matmul(h1[:], lhsT=a1v[:, kc, hc * 128:(hc + 1) * 128], rhs=hfT[:, kc, :], start=(kc == 0), stop=(kc == 7)),
                                  reads=[a1.b, hfT.b], writes=[h1.b])
                        for kc in range(8):
                            S.add("pe", lambda e, h3=h3, a3v=a3v, kc=kc, hc=hc: e.matmul(h3[:], lhsT=a3v[:, kc, hc * 128:(hc + 1) * 128], rhs=hfT[:, kc, :], start=(kc == 0), stop=(kc == 7)),
                                  reads=[a3.b, hfT.b], writes=[h3.b])
                        S.add("act", lambda e, h1=h1, s_=s_: e.activation(out=s_[:], in_=h1[:], func=AF.Silu), reads=[h1.b], writes=[s_.b])
                        S.add("dve", lambda e, h3=h3, s_=s_, hd=hd, hc=hc: e.tensor_tensor(out=hd[:, hc, :], in0=s_[:], in1=h3[:], op=ALU.mult), reads=[s_.b, h3.b], writes=[hd.b])
                    for ti in range(4):
                        o_b = ob[ti % 2]
                        for half in range(2):
                            o_ = OUT[half]
                            for hc in range(4):
                                S.add("pe", lambda e, o_=o_, hd=hd, a2v=a2v, hc=hc, ti=ti, half=half: e.matmul(o_[:], lhsT=hd[:, hc, ti * 128:(ti + 1) * 128], rhs=a2v[:, hc, half * 512:(half + 1) * 512],
                                                                                                        start=(hc == 0), stop=(hc == 3)), reads=[hd.b, a2.b], writes=[o_.b])
                            if half == 0:
                                S.add("act", lambda e, o_=o_, o_b=o_b: e.activation(out=o_b[:, 0:512], in_=o_[:], func=AF.Copy), reads=[o_.b], writes=[o_b.b])
                            else:
                                S.add("dve", lambda e, o_=o_, o_b=o_b: e.tensor_copy(out=o_b[:, 512:1024], in_=o_[:]), reads=[o_.b], writes=[o_b.b])
                        S.add("sp", lambda e, o_b=o_b, i=i, ti=ti: e.dma_start(out=sout[i * 512 + ti * 128:i * 512 + (ti + 1) * 128, :], in_=o_b[:]), reads=[o_b.b], writes=[(dbuf("sout") if ti == 3 else dbuf("sout2")) if i == NT - 1 and ti >= 2 else S.buf("so")], slot=o_b.b)
                for ti in range(NTOK):
                    sap, sbuf_, dap, dbuf_, stream, hrow = tiles_all[ti]
                    S.add("pool", lambda e, ti=ti: e.indirect_dma_start(out=ra[:], out_offset=None, in_=sout[0:NT * 512, :], in_offset=bass.IndirectOffsetOnAxis(ap=slu[0][:, ti:ti + 1], axis=0)),
                          reads=[dbuf("sout"), dbuf("sout2"), slu[0].b], writes=[ra.b], slot=ra.b)
                    S.add("pool", lambda e, ti=ti: e.indirect_dma_start(out=rb_[:], out_offset=None, in_=sout[0:NT * 512, :], in_offset=bass.IndirectOffsetOnAxis(ap=slu[1][:, ti:ti + 1], axis=0)),
                          reads=[dbuf("sout"), dbuf("sout2"), slu[1].b], writes=[rb_.b], slot=rb_.b)
                    S.add("sp", lambda e, sap=sap: e.dma_start(out=xr[:], in_=sap), reads=[sbuf_], writes=[xr.b], slot=xr.b)
                    xo_ = xo[ti % len(xo)]
                    dv(lambda e, ti=ti, xo_=xo_: e.tensor_scalar(out=xo_[:], in0=ra[:], scalar1=WAB[0][:, ti:ti + 1], scalar2=None, op0=ALU.mult), [ra, WAB[0]], [xo_])
                    dv(lambda e, ti=ti, xo_=xo_: e.scalar_tensor_tensor(out=xo_[:], in0=rb_[:], scalar=WAB[1][:, ti:ti + 1], in1=xo_[:], op0=ALU.mult, op1=ALU.add), [rb_, WAB[1], xo_], [xo_])
                    dv(lambda e, stream=stream, xo_=xo_: e.tensor_tensor(out=xo_[:], in0=xo_[:], in1=gates[stream][1][:], op=ALU.mult), [xo_, gates[stream][1]], [xo_])
                    dv(lambda e, xo_=xo_: e.tensor_tensor(out=xo_[:], in0=xo_[:], in1=xr[:], op=ALU.add), [xo_, xr], [xo_])
                    S.add("sp", lambda e, dap=dap, xo_=xo_: e.dma_start(out=dap, in_=xo_[:]), reads=[xo_.b], writes=[dbuf_], slot=xo_.b)
                S.barrier()
        for l_ in range(n_layers):
            layer(l_)
        nsem = S.emit(es)
    return nc, nsem, len(S.ops)


def _bias_sets(rpb_l, j):
    sets = [(64, list(range(-2, 4))), (32 * j, list(range(-2, 4))), (32 * j + 1, list(range(-2, 4))),
            (32 * j + 30, list(range(-3, 3))), (32 * j + 31, list(range(-3, 3)))]
    outp = np.full((5, 6, 128, 8, 128), NEG, np.float32)
    idx = np.arange(128)
    qc = idx % 64
    kc = idx % 64
    cs = np.clip(qc - 8, 0, 48)
    for si, (m, offs) in enumerate(sets):
        qr = 2 * m + idx // 64
        rs = np.clip(qr - 4, 0, 248)
        for oi, o in enumerate(offs):
            kr = 2 * (m + o) + idx // 64
            valid = ((kr[:, None] >= 0) & (kr[:, None] < 256) & (kr[:, None] >= rs[None, :]) & (kr[:, None] < rs[None, :] + 8)
                     & (kc[:, None] >= cs[None, :]) & (kc[:, None] < cs[None, :] + 16))
            dr = np.clip(kr[:, None] - qr[None, :] + 7, 0, 14)
            dc = np.clip(kc[:, None] - qc[None, :] + 15, 0, 30)
            vals = rpb_l[:, dr, dc]
            vals = np.where(valid[None], vals, NEG)
            outp[si, oi] = np.transpose(vals, (1, 0, 2))
    return outp.reshape(5, 6, 128, 1024)


_CACHE = {}


def kernel(x, c, ctx, c_ctx, w_ada, b_ada, w_in, q_gain, k_gain, rpb, sgu_ln, sgu_w, sgu_b, out_gain, w_out,
           rg_w, rg_b, re_w, re_b, w1, w3, w2):
    f = lambda a: np.ascontiguousarray(np.asarray(a, dtype=np.float32))
    x, c, ctx, c_ctx, w_ada, b_ada, w_in, q_gain, k_gain, rpb = map(f, (x, c, ctx, c_ctx, w_ada, b_ada, w_in, q_gain, k_gain, rpb))
    sgu_ln, sgu_w, sgu_b, out_gain, w_out, rg_w, rg_b, re_w, re_b, w1, w3, w2 = map(
        f, (sgu_ln, sgu_w, sgu_b, out_gain, w_out, rg_w, rg_b, re_w, re_b, w1, w3, w2))
    if "nc" not in _CACHE:
        _CACHE["nc"] = build()[0]
    nc = _CACHE["nc"]
    shared = {
        "ident": np.eye(128, dtype=np.float32).astype(ml_dtypes.bfloat16),
        "identf": np.eye(128, dtype=np.float32),
        "w_ada": w_ada, "b_ada": b_ada,
        "b_adaT": np.ascontiguousarray(b_ada.reshape(2, 48, 128).transpose(0, 2, 1)),
        "w_in": w_in.reshape(128, -1), "w_out": w_out.reshape(128, -1),
        "w1": w1.reshape(32, 1024, 512), "w3": w3.reshape(32, 1024, 512), "w2": w2.reshape(32, 512, 1024),
        "cU": np.triu(np.ones((128, 128), np.float32), 1).astype(ml_dtypes.bfloat16),
        "cOnes": np.ones((128, 128), np.float32).astype(ml_dtypes.bfloat16),
        "itc": np.ascontiguousarray(np.broadcast_to((np.arange(35, dtype=np.float32) * 512.0)[None, :, None], (128, 35, 16)).reshape(128, 35 * 16)),
        "pidx": np.ascontiguousarray(np.stack([np.arange(128, dtype=np.float32), np.arange(128, dtype=np.float32) + 2048.0], axis=1)),
        "rw": np.ascontiguousarray(np.concatenate([rg_w, re_w.transpose(0, 2, 1, 3).reshape(2, 1024, 16)], axis=2)),
        "rb": np.ascontiguousarray(np.concatenate([rg_b, re_b.reshape(2, 16)], axis=1)),
        "qg": np.ascontiguousarray(np.tile(q_gain, (1, 2))[:, :, None]),
        "kg": np.ascontiguousarray(np.tile(k_gain, (1, 2))[:, :, None]),
        "sln": sgu_ln,
        "swT": np.ascontiguousarray(sgu_w.transpose(0, 3, 1, 2).reshape(2, 128, 1024)),
        "sbT": np.ascontiguousarray(sgu_b.transpose(0, 2, 1)),
        "ogT": np.ascontiguousarray(out_gain.reshape(2, 8, 128).transpose(0, 2, 1)),
    }
    xpad = np.zeros((2, 272, 64, D), np.float32)
    xpad[:, 8:264] = x.reshape(2, 256, 64, D)
    in_maps = []
    for core in range(8):
        b, j = core // 4, core % 4
        m = dict(shared)
        m["xw"] = np.ascontiguousarray(xpad[b, 64 * j:64 * j + 80].reshape(5120, D))
        m["ctx"] = ctx[b]
        cv = np.stack([c[b].reshape(8, 128).T, c_ctx.reshape(8, 128).T], axis=2)
        m["cvec"] = np.ascontiguousarray(cv.reshape(128, 16))
        m["bias"] = np.stack([_bias_sets(rpb[0], j), _bias_sets(rpb[1], j)], axis=0)
        in_maps.append(m)
    res = run_bass_kernel_spmd(nc, in_maps, core_ids=list(range(8)))
    outp = np.empty((2, 16384, D), np.float32)
    for core in range(8):
        b, j = core // 4, core % 4
        outp[b, 4096 * j:4096 * (j + 1)] = res.results[core]["out"]
    return outp
```
